# Optimizing a Trainium2 kernel written in Bass

```python
import math
import jax, jax.numpy as jnp
from jax import lax
import numpy as np

D_MODEL = 1024
BATCH = 8
SEQ = 2048
DEPTH = 4

N_MIXERS = 2
N_CONV_LAYERS = (DEPTH + N_MIXERS - 1) // N_MIXERS
N_ATTN_LAYERS = DEPTH // N_MIXERS
CONV_WIDTH = 3
N_HEADS = 16
KV_LATENT = 128
N_IDX_HEADS = 8
IDX_DIM = 64
TOPK_MAX = 256
Q_BLOCK = 128
ATTN_IN_WIDTH = N_HEADS * KV_LATENT + KV_LATENT + N_IDX_HEADS * IDX_DIM + IDX_DIM + N_IDX_HEADS
N_BUCKETS = 32
MAX_DISTANCE = 128
N_GROUPS = 8
EXPERTS_PER_GROUP = 8
N_EXPERTS = N_GROUPS * EXPERTS_PER_GROUP
TOPK_IN_GROUP = 2
D_EXPERT = 256
MOE_BLOCK = 128
LN_EPS = 1e-5
RMS_EPS = 1e-6
DEEPNORM_ALPHA = (2.0 * DEPTH) ** 0.25
DEEPNORM_BETA = (8.0 * DEPTH) ** -0.25

kernel_name = "hybrid_conv_dsa_hmoe_deepnorm"


def layer_norm(x, g, b):
    xf = x.astype(jnp.float32)
    mu = jnp.mean(xf, axis=-1, keepdims=True)
    var = jnp.mean(jnp.square(xf - mu), axis=-1, keepdims=True)
    y = (xf - mu) * lax.rsqrt(var + LN_EPS) * g.astype(jnp.float32) + b.astype(jnp.float32)
    return y.astype(x.dtype)


def rms_norm(x, g):
    xf = x.astype(jnp.float32)
    y = xf * lax.rsqrt(jnp.mean(jnp.square(xf), axis=-1, keepdims=True) + RMS_EPS) * g.astype(jnp.float32)
    return y.astype(x.dtype)


def t5_bucket(dist):
    max_exact = N_BUCKETS // 2
    d_f = jnp.maximum(dist, 1).astype(jnp.float32)
    large = max_exact + (jnp.log(d_f / max_exact) / math.log(MAX_DISTANCE / max_exact)
                         * (N_BUCKETS - max_exact)).astype(jnp.int32)
    large = jnp.minimum(large, N_BUCKETS - 1)
    return jnp.where(dist < max_exact, dist, large)


def short_conv_mixer(x, w_in, conv_k, w_out):
    S = x.shape[1]
    b_gate, c_gate, h = jnp.split(x @ w_in, 3, axis=-1)
    u = c_gate * h
    up = jnp.pad(u, ((0, 0), (CONV_WIDTH - 1, 0), (0, 0)))
    conv = up[:, 0:S] * conv_k[0]
    for j in range(1, CONV_WIDTH):
        conv = conv + up[:, j:j + S] * conv_k[j]
    return (b_gate * conv) @ w_out


def dsa_mixer(x, w_in, kv_norm_g, kidx_ln_g, kidx_ln_b, w_out, rel_bias):
    Bsz, S, _ = x.shape
    topk = min(TOPK_MAX, S // 4)
    hq = N_HEADS * KV_LATENT
    cuts = [hq, hq + KV_LATENT, hq + KV_LATENT + N_IDX_HEADS * IDX_DIM,
            hq + KV_LATENT + N_IDX_HEADS * IDX_DIM + IDX_DIM]
    q, c_kv, q_idx, k_idx, w_idx = jnp.split(x @ w_in, cuts, axis=-1)
    q = q.reshape(Bsz, S, N_HEADS, KV_LATENT)
    c_kv = rms_norm(c_kv, kv_norm_g)
    q_idx = q_idx.reshape(Bsz, S, N_IDX_HEADS, IDX_DIM)
    k_idx = layer_norm(k_idx, kidx_ln_g, kidx_ln_b)
    w_idx = w_idx * (N_IDX_HEADS ** -0.5)
    nb = S // Q_BLOCK
    key_pos = jnp.arange(S)

    def to_blocks(a):
        return jnp.moveaxis(a.reshape(Bsz, nb, Q_BLOCK, *a.shape[2:]), 1, 0)

    def block_fn(args):
        qb, qib, wb, blk = args
        t = blk * Q_BLOCK + jnp.arange(Q_BLOCK)
        s_idx = jnp.einsum('bthd,bsd->bths', qib, k_idx,
                           preferred_element_type=jnp.float32) * (IDX_DIM ** -0.5)
        score = jnp.einsum('bths,bth->bts', jax.nn.relu(s_idx), wb.astype(jnp.float32))
        causal = key_pos[None, :] <= t[:, None]
        score = jnp.where(causal[None], score, -jnp.inf)
        _, sel = lax.top_k(score, topk)
        valid = sel <= t[None, :, None]
        kv_sel = jax.vmap(lambda c, i: c[i])(c_kv, sel)
        logits = jnp.einsum('bthc,btjc->bthj', qb, kv_sel,
                            preferred_element_type=jnp.float32) * (KV_LATENT ** -0.5)
        bucket = t5_bucket(jnp.maximum(t[None, :, None] - sel, 0))
        bias = jnp.moveaxis(rel_bias[bucket], -1, 2)
        logits = logits + bias.astype(jnp.float32)
        logits = jnp.where(valid[:, :, None, :], logits, -jnp.inf)
        p = jax.nn.softmax(logits, axis=-1).astype(x.dtype)
        o = jnp.einsum('bthj,btjc->bthc', p, kv_sel)
        return o.reshape(Bsz, Q_BLOCK, hq)

    o = lax.map(block_fn, (to_blocks(q), to_blocks(q_idx), to_blocks(w_idx), jnp.arange(nb)))
    o = jnp.moveaxis(o, 0, 1).reshape(Bsz, S, hq)
    return o @ w_out


def hier_moe(x, wg, bg, we, be, w1, w3, w2):
    Bsz, S, D = x.shape
    xt = x.reshape(-1, D)
    T = xt.shape[0]
    g_logits = (xt @ wg + bg).astype(jnp.float32)
    g_prob = jax.nn.softmax(g_logits, axis=-1)
    g_top = jnp.argmax(g_logits, axis=-1)
    g_gate = jnp.take_along_axis(g_prob, g_top[:, None], axis=-1)
    e_logits = (xt @ we + be).astype(jnp.float32).reshape(T, N_GROUPS, EXPERTS_PER_GROUP)
    e_logits = jnp.take_along_axis(e_logits, g_top[:, None, None], axis=1)[:, 0]
    e_val, e_loc = lax.top_k(e_logits, TOPK_IN_GROUP)
    gate = jax.nn.softmax(e_val, axis=-1) * g_gate
    e_id = g_top[:, None] * EXPERTS_PER_GROUP + e_loc
    N = T * TOPK_IN_GROUP
    flat_e = e_id.reshape(-1).astype(jnp.int32)
    flat_tok = jnp.repeat(jnp.arange(T, dtype=jnp.int32), TOPK_IN_GROUP)
    flat_w = gate.reshape(-1)
    order = jnp.argsort(flat_e)
    e_s, tok_s, w_s = flat_e[order], flat_tok[order], flat_w[order]
    counts = jax.ops.segment_sum(jnp.ones_like(flat_e), flat_e, num_segments=N_EXPERTS)
    padded = (counts + MOE_BLOCK - 1) // MOE_BLOCK * MOE_BLOCK
    start = jnp.cumsum(counts) - counts
    pend = jnp.cumsum(padded)
    pstart = pend - padded
    dest = pstart[e_s] + jnp.arange(N, dtype=jnp.int32) - start[e_s]
    R = (-(-N // MOE_BLOCK) + N_EXPERTS) * MOE_BLOCK
    row_tok = jnp.zeros((R,), jnp.int32).at[dest].set(tok_s)
    row_w = jnp.zeros((R,), jnp.float32).at[dest].set(w_s)
    nblk = R // MOE_BLOCK
    blk_e = jnp.minimum(jnp.searchsorted(pend, jnp.arange(nblk, dtype=jnp.int32) * MOE_BLOCK,
                                         side='right'), N_EXPERTS - 1)
    xs = xt[row_tok].reshape(nblk, MOE_BLOCK, D)

    def expert_block(args):
        xb, e = args
        h = jax.nn.silu(xb @ w1[e]) * (xb @ w3[e])
        return h @ w2[e]

    ys = lax.map(expert_block, (xs, blk_e)).reshape(R, D)
    out = jnp.zeros_like(xt).at[row_tok].add(ys * row_w[:, None].astype(ys.dtype))
    return out.reshape(Bsz, S, D)


def setup_inputs(seed: int = 0) -> dict:
    key = jax.random.key(seed)
    ks = jax.random.split(key, 24)
    D = D_MODEL
    f32 = jnp.float32

    def nrm(k, shape, scale):
        return jax.random.normal(k, shape, f32) * scale

    return {
        "x": nrm(ks[0], (BATCH, SEQ, D), 1.0),
        "conv_w_in": nrm(ks[1], (N_CONV_LAYERS, D, 3 * D), D ** -0.5),
        "conv_k": nrm(ks[2], (N_CONV_LAYERS, CONV_WIDTH, D), CONV_WIDTH ** -0.5),
        "conv_w_out": nrm(ks[3], (N_CONV_LAYERS, D, D), DEEPNORM_BETA * D ** -0.5),
        "attn_w_in": nrm(ks[4], (N_ATTN_LAYERS, D, ATTN_IN_WIDTH), D ** -0.5),
        "kv_norm_g": 1.0 + nrm(ks[5], (N_ATTN_LAYERS, KV_LATENT), 0.02),
        "kidx_ln_g": 1.0 + nrm(ks[6], (N_ATTN_LAYERS, IDX_DIM), 0.02),
        "kidx_ln_b": nrm(ks[7], (N_ATTN_LAYERS, IDX_DIM), 0.02),
        "attn_w_out": nrm(ks[8], (N_ATTN_LAYERS, N_HEADS * KV_LATENT, D),
                          DEEPNORM_BETA * (N_HEADS * KV_LATENT) ** -0.5),
        "rel_bias": nrm(ks[9], (N_BUCKETS, N_HEADS), 0.5),
        "router_wg": nrm(ks[10], (DEPTH, D, N_GROUPS), D ** -0.5),
        "router_bg": nrm(ks[11], (DEPTH, N_GROUPS), 0.01),
        "router_we": nrm(ks[12], (DEPTH, D, N_EXPERTS), D ** -0.5),
        "router_be": nrm(ks[13], (DEPTH, N_EXPERTS), 0.01),
        "exp_w1": nrm(ks[14], (DEPTH, N_EXPERTS, D, D_EXPERT), D ** -0.5),
        "exp_w3": nrm(ks[15], (DEPTH, N_EXPERTS, D, D_EXPERT), D ** -0.5),
        "exp_w2": nrm(ks[16], (DEPTH, N_EXPERTS, D_EXPERT, D), DEEPNORM_BETA * D_EXPERT ** -0.5),
        "ln1_g": 1.0 + nrm(ks[17], (DEPTH, D), 0.02),
        "ln1_b": nrm(ks[18], (DEPTH, D), 0.02),
        "ln2_g": 1.0 + nrm(ks[19], (DEPTH, D), 0.02),
        "ln2_b": nrm(ks[20], (DEPTH, D), 0.02),
    }


def reference(x, conv_w_in, conv_k, conv_w_out, attn_w_in, kv_norm_g, kidx_ln_g, kidx_ln_b,
              attn_w_out, rel_bias, router_wg, router_bg, router_we, router_be,
              exp_w1, exp_w3, exp_w2, ln1_g, ln1_b, ln2_g, ln2_b):
    for i in range(DEPTH):
        j = i // N_MIXERS
        if i % N_MIXERS == 0:
            m = short_conv_mixer(x, conv_w_in[j], conv_k[j], conv_w_out[j])
        else:
            m = dsa_mixer(x, attn_w_in[j], kv_norm_g[j], kidx_ln_g[j], kidx_ln_b[j],
                          attn_w_out[j], rel_bias)
        x = layer_norm(DEEPNORM_ALPHA * x + m, ln1_g[i], ln1_b[i])
        f = hier_moe(x, router_wg[i], router_bg[i], router_we[i], router_be[i],
                     exp_w1[i], exp_w3[i], exp_w2[i])
        x = layer_norm(DEEPNORM_ALPHA * x + f, ln2_g[i], ln2_b[i])
    return x
```

```python
import numpy as np
from contextlib import ExitStack
import concourse.bass as bass
import concourse.mybir as mybir
from concourse.bass_utils import run_bass_kernel_spmd

F32 = mybir.dt.float32
BF16 = mybir.dt.bfloat16
I32 = mybir.dt.int32
AF = mybir.ActivationFunctionType
ALU = mybir.AluOpType
AX = mybir.AxisListType

SEQ = 2048
D = 1024
NT = 16
KC = 8
DEPTH = 4
NH = 16
NIH = 8
TOPK = 256
NBLK = 96
ALPHA = (2.0 * DEPTH) ** 0.25
LN_EPS = 1e-5
RMS_EPS = 1e-6
NEG = -1.0e30
MASK_NEG = -30000.0
VW = 383

COMPUTE = ("pe", "act", "dve", "pool")
ALL_STREAMS = ("pe", "act", "dve", "pool", "sp")


class Buf:
    __slots__ = ("name", "lw", "rd")

    def __init__(self, name=""):
        self.name = name
        self.lw = None
        self.rd = {}


class Sched:
    def __init__(self, nc, es, n_dma_sems=40):
        self.nc = nc
        self.streams = {e: [] for e in ALL_STREAMS}
        self.count = {e: 0 for e in COMPUTE}
        self.waited = {e: {} for e in ALL_STREAMS}
        self.n_dma_sems = n_dma_sems
        self.dma_val = [0] * n_dma_sems
        self.dma_rr = 0
        self.sems = {}
        for e in COMPUTE:
            self.sems[e] = es.enter_context(nc.semaphore("sem_" + e))
        for s in range(n_dma_sems):
            self.sems[("dma", s)] = es.enter_context(nc.semaphore("semd%d" % s))
        self.n_ops = 0

    def _need(self, eng, deps):
        st = self.streams[eng]
        w = self.waited[eng]
        for key, val in deps.items():
            if key == eng:
                if eng == "pe" or self.count[eng] - val >= 3:
                    continue
            if w.get(key, 0) >= val:
                continue
            w[key] = val
            st.append(("wait", key, val))

    def _collect(self, reads, writes):
        deps = {}
        for b in reads:
            if b.lw is not None and deps.get(b.lw[0], 0) < b.lw[1]:
                deps[b.lw[0]] = b.lw[1]
        for b in writes:
            if b.lw is not None and deps.get(b.lw[0], 0) < b.lw[1]:
                deps[b.lw[0]] = b.lw[1]
            for k, v in b.rd.items():
                if deps.get(k, 0) < v:
                    deps[k] = v
        return deps

    def _commit(self, tok, reads, writes):
        k, v = tok
        for b in writes:
            b.lw = tok
            b.rd = {}
        for b in reads:
            if b.rd.get(k, 0) < v:
                b.rd[k] = v

    def op(self, eng, fn, reads=(), writes=()):
        deps = self._collect(reads, writes)
        self._need(eng, deps)
        self.count[eng] += 1
        tok = (eng, self.count[eng])
        self.streams[eng].append(("op", fn, eng))
        self._commit(tok, reads, writes)
        self.n_ops += 1
        return tok

    def dma(self, q, fn, reads=(), writes=()):
        deps = self._collect(reads, writes)
        s = self.dma_rr
        self.dma_rr = (self.dma_rr + 1) % self.n_dma_sems
        key = ("dma", s)
        if self.dma_val[s] > 0:
            deps[key] = max(deps.get(key, 0), self.dma_val[s])
        self._need(q, deps)
        self.dma_val[s] += 16
        tok = (key, self.dma_val[s])
        self.streams[q].append(("dma", fn, key))
        self._commit(tok, reads, writes)
        self.n_ops += 1
        return tok

    def flush(self):
        deps = {("dma", s): self.dma_val[s] for s in range(self.n_dma_sems) if self.dma_val[s] > 0}
        self._need("sp", deps)
        for e in ALL_STREAMS:
            self._need(e, {f: self.count[f] for f in COMPUTE if f != e and self.count[f] > 0})
        sems = self.sems
        streams = self.streams

        def run(stream):
            def body(e):
                for it in stream:
                    if it[0] == "wait":
                        e.wait_ge(sems[it[1]], it[2])
                    elif it[0] == "raw":
                        it[1](e)
                    elif it[0] == "op":
                        it[1](e).then_inc(sems[it[2]], 1)
                    else:
                        it[1](e).then_inc(sems[it[2]], 16)
            return body
        with self.nc.Block() as block:
            block.tensor(run(streams["pe"]))
            block.scalar(run(streams["act"]))
            block.vector(run(streams["dve"]))
            block.gpsimd(run(streams["pool"]))
            block.sync(run(streams["sp"]))
        self.streams = {e: [] for e in ALL_STREAMS}
        for e in ALL_STREAMS:
            for f in COMPUTE:
                self.waited[e][f] = self.count[f]
            for s in range(self.n_dma_sems):
                self.waited[e][("dma", s)] = self.dma_val[s]


class Prog:
    def __init__(self, n_layers=DEPTH, stop_after_mixer=False):
        self.n_layers = n_layers
        self.stop_after_mixer = stop_after_mixer
        self.nc = bass.Bass("TRN2", target_bir_lowering=False)

    def MM(self, out, lhsT, rhs, start, stop, r, w):
        self.S.op("pe", lambda e: e.matmul(out, lhsT=lhsT, rhs=rhs, start=start, stop=stop), r, w)

    def TR(self, out, in_, ident, r, w):
        self.S.op("pe", lambda e: e.transpose(out=out, in_=in_, identity=ident), r, w)

    def ACT(self, out, in_, func, r, w, scale=None, bias=None, accum=None):
        kw = {}
        if scale is not None:
            kw["scale"] = scale
        if bias is not None:
            kw["bias"] = bias
        if accum is not None:
            kw["accum_out"] = accum
        self.S.op("act", lambda e: e.activation(out=out, in_=in_, func=func, **kw), r, w)

    def TS(self, eng, out, in0, s1, op0, r, w, s2=None, op1=None, accum=None):
        kw = {}
        if op1 is not None:
            kw["op1"] = op1
        if accum is not None:
            kw["accum_out"] = accum
        self.S.op(eng, lambda e: e.tensor_scalar(out=out, in0=in0, scalar1=s1, scalar2=s2, op0=op0, **kw), r, w)

    def TT(self, eng, out, in0, in1, op, r, w):
        self.S.op(eng, lambda e: e.tensor_tensor(out=out, in0=in0, in1=in1, op=op), r, w)

    def STT(self, eng, out, in0, scalar, in1, op0, op1, r, w):
        self.S.op(eng, lambda e: e.scalar_tensor_tensor(out=out, in0=in0, scalar=scalar, in1=in1, op0=op0, op1=op1), r, w)

    def CP(self, eng, out, in_, r, w):
        if eng == "act":
            self.S.op("act", lambda e: e.copy(out=out, in_=in_), r, w)
        else:
            self.S.op(eng, lambda e: e.tensor_copy(out=out, in_=in_), r, w)

    def RED(self, out, in_, op, r, w, axis=AX.X):
        self.S.op("dve", lambda e: e.tensor_reduce(out=out, in_=in_, axis=axis, op=op), r, w)

    def MEMSET(self, eng, ap, val, w):
        self.S.op(eng, lambda e: e.memset(ap, val), (), w)

    def DMA(self, q, out, in_, r, w, **kw):
        self.S.dma(q, lambda e: e.dma_start(out=out, in_=in_, **kw), r, w)

    def GATHER(self, out, table, idx, r, w, bound=None):
        if bound is None:
            self.S.dma("pool", lambda e: e.indirect_dma_start(
                out=out, out_offset=None, in_=table, in_offset=bass.IndirectOffsetOnAxis(ap=idx, axis=0)), r, w)
        else:
            self.S.dma("pool", lambda e: e.indirect_dma_start(
                out=out, out_offset=None, in_=table, in_offset=bass.IndirectOffsetOnAxis(ap=idx, axis=0),
                bounds_check=self.bound_reg, oob_is_err=False), r, w)

    def SCATTER(self, table, idx, in_, r, w):
        self.S.dma("pool", lambda e: e.indirect_dma_start(
            out=table, out_offset=bass.IndirectOffsetOnAxis(ap=idx, axis=0), in_=in_, in_offset=None), r, w)

    def sb(self, es, name, shape, dt):
        self.uid = getattr(self, "uid", 0) + 1
        return es.enter_context(self.nc.sbuf_tensor("%s_u%d" % (name, self.uid), shape, dt))

    def ps(self, es, name, shape, dt):
        self.uid = getattr(self, "uid", 0) + 1
        return es.enter_context(self.nc.psum_tensor("%s_u%d" % (name, self.uid), shape, dt))

    def declare_dram(self):
        nc = self.nc

        def inp(name, shape, dt=F32):
            return nc.dram_tensor(name, shape, dt, kind="ExternalInput").ap()

        def internal(name, shape, dt):
            return nc.dram_tensor(name, shape, dt, kind="Internal").ap()
        d = {}
        d["x"] = inp("x", [SEQ, D])
        for j in range(2):
            d["cwin%d" % j] = inp("cwin%d" % j, [D, 3 * D])
            d["cwout%d" % j] = inp("cwout%d" % j, [D, D])
            d["awin%d" % j] = inp("awin%d" % j, [D, 2760])
            d["awout%d" % j] = inp("awout%d" % j, [2048, D])
        d["conv_k"] = inp("conv_k", [2, 3, D])
        d["kv_g"] = inp("kv_g", [2, 128])
        d["ki_g"] = inp("ki_g", [2, 64])
        d["ki_b"] = inp("ki_b", [2, 64])
        d["rel_bias"] = inp("rel_bias", [32, 16])
        d["ohs"] = inp("ohs", [32, VW])
        d["rw"] = inp("rw", [DEPTH, D, 72])
        d["rb"] = inp("rb", [DEPTH, 72])
        for l in range(DEPTH):
            d["w1r%d" % l] = inp("w1r%d" % l, [64 * 128, 2048])
            d["w3r%d" % l] = inp("w3r%d" % l, [64 * 128, 2048])
            d["w2r%d" % l] = inp("w2r%d" % l, [64 * 128, 2048])
        d["ln1_g"] = inp("ln1_g", [DEPTH, D])
        d["ln1_b"] = inp("ln1_b", [DEPTH, D])
        d["ln2_g"] = inp("ln2_g", [DEPTH, D])
        d["ln2_b"] = inp("ln2_b", [DEPTH, D])
        d["out"] = nc.dram_tensor("out", [SEQ, D], F32, kind="ExternalOutput").ap()
        d["xres"] = internal("xres_d", [SEQ, D], F32)
        d["xs"] = internal("xs_d", [NBLK * 128, D], BF16)
        d["ys"] = internal("ys_d", [NBLK * 128, D], F32)
        d["maskT"] = internal("maskT_d", [SEQ, SEQ], BF16)
        d["t5"] = internal("t5_d", [16 * 128 * (VW + 1)], F32)
        d["qT"] = internal("qT_d", [NH, 128, SEQ], BF16)
        self.d = d

    @staticmethod
    def bcast_rows(vec_ap, n):
        return bass.AP(tensor=vec_ap.tensor, offset=vec_ap.offset, ap=[[0, 128], [1, n]])

    def build(self):
        nc = self.nc
        self.declare_dram()
        with ExitStack() as es:
            self.S = Sched(nc, es)
            S = self.S
            self.xa = self.sb(es, "xa", [128, 16384], BF16)
            self.xT = self.xa[:, :].rearrange("p (k t) -> p k t", k=KC)
            self.xb = self.xa[:, :].rearrange("p (i d) -> p i d", i=NT)
            self.BxT = [Buf("xT%d" % i) for i in range(NT)]
            self.Bxb = [Buf("xb%d" % i) for i in range(NT)]
            self.lg = self.sb(es, "lg", [128, NT, 72], F32)
            self.Blg = Buf("lg")
            self.ident_f = self.sb(es, "ident_f", [128, 128], F32)
            self.ident_b = self.sb(es, "ident_b", [128, 128], BF16)
            self.ones_b = self.sb(es, "ones_b", [128, 128], BF16)
            self.tri_b = self.sb(es, "tri_b", [128, 128], BF16)
            self.ones_row = self.sb(es, "ones_row", [1, 128], F32)
            self.eps_ln = self.sb(es, "eps_ln", [128, 1], F32)
            self.eps_rms = self.sb(es, "eps_rms", [128, 1], F32)
            self.pidx = self.sb(es, "pidx", [128, 1], F32)
            self.bvals = self.sb(es, "bvals", [128, NBLK], F32)
            self.BtT = self.sb(es, "BtT", [128, NH, 2, 128], BF16)
            self.Bconst = Buf("const")
            self.phase_init()
            self.phase_load_x()
            import os
            skip0 = int(os.environ.get("SKIP0", "0"))
            for l in range(self.n_layers):
                if l < skip0:
                    continue
                last_mixer = self.stop_after_mixer and l == self.n_layers - 1
                if l % 2 == 0:
                    self.phase_conv(l, to_out=last_mixer)
                else:
                    self.phase_attn(l, to_out=last_mixer)
                if last_mixer:
                    break
                self.phase_moe(l, to_out=(l == self.n_layers - 1))
        return nc

    def phase_init(self):
        S = self.S
        Bc = self.Bconst
        with ExitStack() as es:
            tmpf = self.sb(es, "init_tmpf", [128, 128], F32)
            tmpi = self.sb(es, "init_tmpi", [128, NBLK], I32)
            Bt = Buf()
            Bi = Buf()
            self.MEMSET("pool", self.ident_f[:], 1.0, [Bc])
            S.op("pool", lambda e: e.affine_select(out=self.ident_f[:], in_=self.ident_f[:], pattern=[[-1, 128]],
                                                   compare_op=ALU.is_equal, fill=0.0, base=0, channel_multiplier=1),
                 [Bc], [Bc])
            self.CP("dve", self.ident_b[:], self.ident_f[:], [Bc], [Bc])
            self.MEMSET("dve", self.ones_b[:], 1.0, [Bc])
            self.MEMSET("pool", tmpf[:], 1.0, [Bt])
            S.op("pool", lambda e: e.affine_select(out=tmpf[:], in_=tmpf[:], pattern=[[1, 128]],
                                                   compare_op=ALU.is_ge, fill=0.0, base=-1, channel_multiplier=-1),
                 [Bt], [Bt])
            self.CP("dve", self.tri_b[:], tmpf[:], [Bt], [Bc])
            self.MEMSET("dve", self.ones_row[:], 1.0, [Bc])
            self.MEMSET("dve", self.eps_ln[:], LN_EPS, [Bc])
            self.MEMSET("dve", self.eps_rms[:], RMS_EPS, [Bc])
            S.op("pool", lambda e: e.iota(tmpi[:, 0:1], pattern=[[0, 1]], base=0, channel_multiplier=1), (), [Bi])
            self.CP("dve", self.pidx[:], tmpi[:, 0:1], [Bi], [Bc])
            S.op("pool", lambda e: e.iota(tmpi[:, :], pattern=[[128, NBLK]], base=0, channel_multiplier=0), [Bi], [Bi])
            self.CP("dve", self.bvals[:], tmpi[:, :], [Bi], [Bc])
            if self.n_layers >= 2:
                self.init_t5(es)
            S.flush()

    def init_t5(self, es):
        S = self.S
        d = self.d
        rel = self.sb(es, "t5_rel", [32, 16], F32)
        ohs = self.sb(es, "t5_ohs", [32, VW], F32)
        rhs = self.sb(es, "t5_rhs", [32, NH * VW], F32)
        ones32 = self.sb(es, "t5_ones", [32, 128], F32)
        vec = self.sb(es, "t5_vec", [128, NH, VW], F32)
        btf = self.sb(es, "t5_btf", [128, NH, 2, 128], F32)
        Br, Bo, Brhs, B1, Bv = Buf(), Buf(), Buf(), Buf(), Buf()
        pp = [self.ps(es, "t5_ps%d" % i, [128, 512], F32) for i in range(2)]
        Bpp = [Buf(), Buf()]
        self.DMA("sp", rel[:], d["rel_bias"], (), [Br])
        self.DMA("sp", ohs[:], d["ohs"], (), [Bo])
        self.MEMSET("dve", ones32[:], 1.0, [B1])
        for h in range(NH):
            self.TS("dve", rhs[:, h * VW:(h + 1) * VW], ohs[:], rel[:, h:h + 1], ALU.mult, [Br, Bo], [Brhs])
        tot = NH * VW
        vflat = vec[:, :, :].rearrange("p h v -> p (h v)")
        c = 0
        k = 0
        while c < tot:
            n = min(512, tot - c)
            self.MM(pp[k % 2][:, 0:n], ones32[:, :], rhs[:, c:c + n], True, True, [B1, Brhs], [Bpp[k % 2]])
            self.CP("act" if k % 2 else "dve", vflat[:, c:c + n], pp[k % 2][:, 0:n], [Bpp[k % 2]], [Bv])
            c += n
            k += 1
        t5 = d["t5"]
        dst = bass.AP(tensor=t5.tensor, offset=0, ap=[[VW + 1, 128], [128 * (VW + 1), NH], [1, VW]])
        Bd = Buf()
        self.DMA("sp", dst, vec[:, :, :], [Bv], [Bd])
        src = bass.AP(tensor=t5.tensor, offset=127, ap=[[VW, 128], [128 * (VW + 1), NH], [128, 2], [1, 128]])
        Bbt = Buf()
        self.DMA("sp", btf[:, :, :, :], src, [Bd], [Bbt])
        self.CP("dve", self.BtT[:, :, :, :], btf[:, :, :, :], [Bbt], [self.Bconst])

    def to_xT(self, src, Bsrc, i, pts, Bpts):
        for half in range(2):
            pt, Bp = pts[half], Bpts[half]
            for q in range(4):
                kc = half * 4 + q
                self.TR(pt[:, q * 128:(q + 1) * 128], src[:, kc * 128:(kc + 1) * 128], self.ident_f[:],
                        [Bsrc, self.Bconst], [Bp])
            dst = self.xT[:, half * 4:(half + 1) * 4, i * 128:(i + 1) * 128]
            self.CP("act" if half else "dve", dst, pt[:, :].rearrange("p (q t) -> p q t", q=4), [Bp],
                    [self.BxT[i]] + self.Bxb)

    def phase_load_x(self):
        S = self.S
        d = self.d
        with ExitStack() as es:
            xt = [self.sb(es, "lx_xt%d" % k, [128, D], F32) for k in range(2)]
            Bx = [Buf(), Buf()]
            pts = [self.ps(es, "lx_pt%d" % k, [128, 512], F32) for k in range(4)]
            Bp = [Buf() for _ in range(4)]
            Bxr = Buf()
            self.DMA("sp", d["xres"], d["x"], (), [Bxr])
            for i in range(NT):
                k = i % 2
                self.DMA("sp", xt[k][:], d["x"][i * 128:(i + 1) * 128, :], (), [Bx[k]])
                self.to_xT(xt[k], Bx[k], i, pts[2 * k:2 * k + 2], Bp[2 * k:2 * k + 2])
            S.flush()

    def ln_setup(self, es, tag, g_ap, b_ap, lidx, router, nbuf=3):
        c = {}
        c["g"] = self.sb(es, tag + "_g", [128, D], F32)
        c["b"] = self.sb(es, tag + "_b", [128, D], F32)
        c["Bgb"] = Buf()
        self.DMA("sp", c["g"][:], self.bcast_rows(g_ap, D), (), [c["Bgb"]])
        self.DMA("sp", c["b"][:], self.bcast_rows(b_ap, D), (), [c["Bgb"]])
        for nm in ("xr", "v", "xo"):
            c[nm] = [self.sb(es, "%s_%s%d" % (tag, nm, k), [128, D], F32) for k in range(nbuf)]
            c["B" + nm] = [Buf() for _ in range(nbuf)]
        c["nbuf"] = nbuf
        c["st"] = [self.sb(es, "%s_st%d" % (tag, k), [128, 16], F32) for k in range(nbuf)]
        c["Bst"] = [Buf() for _ in range(nbuf)]
        c["router"] = router
        if router:
            c["rw"] = self.sb(es, tag + "_rw", [128, KC, 72], F32)
            c["rb"] = self.sb(es, tag + "_rbias", [1, 72], F32)
            c["Brw"] = Buf()
            self.DMA("sp", c["rw"][:, :, :], self.d["rw"][lidx].rearrange("(k p) c -> p k c", p=128), (), [c["Brw"]])
            self.DMA("sp", c["rb"][:, :], self.d["rb"][lidx:lidx + 1, :], (), [c["Brw"]])
            c["x32"] = [self.sb(es, "%s_x32%d" % (tag, k), [128, KC, 128], F32) for k in range(2)]
            c["Bx32"] = [Buf(), Buf()]
        return c

    def _ln_bufs(self, c, i):
        k3 = i % c["nbuf"]
        return (c["xr"][k3], c["v"][k3], c["xo"][k3], c["st"][k3], c["Bxr"][k3], c["Bv"][k3], c["Bxo"][k3], c["Bst"][k3])

    def ln_A(self, c, i, y_halves, y_bufs):
        d = self.d
        xr, v, xo, st, Bxr, Bv, Bxo, Bst = self._ln_bufs(c, i)
        self.DMA("sp", xr[:], d["xres"][i * 128:(i + 1) * 128, :], (), [Bxr])
        for h in range(2):
            self.STT("dve", v[:, h * 512:(h + 1) * 512], xr[:, h * 512:(h + 1) * 512], ALPHA, y_halves[h],
                     ALU.mult, ALU.add, [Bxr, y_bufs[h]], [Bv])
        for h in range(2):
            self.S.op("dve", (lambda hh, st=st, v=v: (lambda e: e.bn_stats(out=st[:, hh * 6:(hh + 1) * 6],
                                                                          in_=v[:, hh * 512:(hh + 1) * 512])))(h), [Bv], [Bst])
        self.S.op("dve", (lambda st: (lambda e: e.bn_aggr(out=st[:, 12:14], in_=st[:, 0:12])))(st), [Bst], [Bst])
        self.ACT(st[:, 14:15], st[:, 13:14], AF.Sqrt, [Bst, self.Bconst], [Bst], bias=self.eps_ln[:, 0:1], scale=1.0)
        self.S.op("dve", (lambda st: (lambda e: e.reciprocal(out=st[:, 14:15], in_=st[:, 14:15])))(st), [Bst], [Bst])
        self.STT("dve", st[:, 15:16], st[:, 12:13], -1.0, st[:, 14:15], ALU.mult, ALU.mult, [Bst], [Bst])

    def ln_B(self, c, i, dst_dram):
        xr, v, xo, st, Bxr, Bv, Bxo, Bst = self._ln_bufs(c, i)
        self.ACT(v[:, :], v[:, :], AF.Identity, [Bv, Bst], [Bv], scale=st[:, 14:15], bias=st[:, 15:16])
        self.TT("dve", v[:, :], v[:, :], c["g"][:, :], ALU.mult, [Bv, c["Bgb"]], [Bv])
        self.TT("dve", xo[:, :], v[:, :], c["b"][:, :], ALU.add, [Bv, c["Bgb"]], [Bxo])
        self.DMA("sp", dst_dram[i * 128:(i + 1) * 128, :], xo[:], [Bxo], ())
        if c["router"]:
            self.CP("act", self.xb[:, i, :], xo[:, :], [Bxo], [self.Bxb[i]] + self.BxT)

    def ln_C(self, c, i, pts, Bpts, prl, Bprl):
        xr, v, xo, st, Bxr, Bv, Bxo, Bst = self._ln_bufs(c, i)
        k = i % 2
        if not c["router"]:
            self.to_xT(xo, Bxo, i, pts, Bpts)
        else:
            x32, Bx32 = c["x32"][k], c["Bx32"][k]
            for half in range(2):
                pt, Bp = pts[half], Bpts[half]
                for q in range(4):
                    kc = half * 4 + q
                    self.TR(pt[:, q * 128:(q + 1) * 128], xo[:, kc * 128:(kc + 1) * 128], self.ident_f[:],
                            [Bxo, self.Bconst], [Bp])
                self.CP("act" if half else "dve", x32[:, half * 4:(half + 1) * 4, :],
                        pt[:, :].rearrange("p (q t) -> p q t", q=4), [Bp], [Bx32])

    def ln_D(self, c, i, prl, Bprl):
        if not c["router"]:
            return
        k = i % 2
        x32, Bx32 = c["x32"][k], c["Bx32"][k]
        for kc in range(KC):
            self.MM(prl[:, 0:72], x32[:, kc, :], c["rw"][:, kc, :], kc == 0, False, [Bx32, c["Brw"]], [Bprl])
        self.MM(prl[:, 0:72], self.ones_row[0:1, :], c["rb"][0:1, :], False, True, [self.Bconst, c["Brw"]], [Bprl])
        self.CP("act", self.lg[:, i, :], prl[:, 0:72], [Bprl], [self.Blg])

    def ln_run(self, c, tiles, yfn, dst, ptsfn):
        n = len(tiles)
        for s in range(n + 3):
            if s < n:
                yh, yb = yfn(tiles[s])
                self.ln_A(c, tiles[s], yh, yb)
            if 0 <= s - 1 < n:
                self.ln_B(c, tiles[s - 1], dst)
            if 0 <= s - 2 < n:
                pts, Bpts, prl, Bprl = ptsfn(tiles[s - 2])
                self.ln_C(c, tiles[s - 2], pts, Bpts, prl, Bprl)
            if 0 <= s - 3 < n:
                pts, Bpts, prl, Bprl = ptsfn(tiles[s - 3])
                self.ln_D(c, tiles[s - 3], prl, Bprl)

    def phase_conv(self, l, to_out):
        S = self.S
        d = self.d
        j = l // 2
        win = d["cwin%d" % j].rearrange("(k p) f -> p k f", p=128)
        with ExitStack() as es:
            c = self.ln_setup(es, "cv", d["ln1_g"][l], d["ln1_b"][l], l, router=not to_out)
            wb = [self.sb(es, "cv_w%d" % k, [128, 3, KC, 128], BF16) for k in range(2)]
            Bw = [Buf(), Buf()]
            wout = self.sb(es, "cv_wout", [128, KC, D], BF16)
            Bwout = Buf()
            zT = self.sb(es, "cv_zT", [128, KC, SEQ], BF16)
            Bz = [Buf() for _ in range(KC)]
            ub = self.sb(es, "cv_u", [128, SEQ + 2], F32)
            Bu = Buf()
            cs = [self.sb(es, "cv_cs%d" % k, [128, 512], F32) for k in range(2)]
            Bcs = [Buf(), Buf()]
            cvt = [self.sb(es, "cv_cv%d" % k, [128, 512], F32) for k in range(2)]
            Bcv = [Buf(), Buf()]
            ck = self.sb(es, "cv_ck", [128, 3, KC], F32)
            Bck = Buf()
            pp = [self.ps(es, "cv_ps%d" % k, [128, 512], F32) for k in range(8)]
            Bpp = [Buf() for _ in range(8)]
            self.DMA("sp", ck[:, :, :], d["conv_k"][j].rearrange("j (f p) -> p j f", p=128), (), [Bck],
                     allow_slow_non_contiguous=True)
            self.DMA("pool", wout[:, :, :], d["cwout%d" % j].rearrange("(k p) f -> p k f", p=128), (), [Bwout])
            self.MEMSET("dve", ub[:, 0:2], 0.0, [Bu])
            for f in range(KC):
                k = f % 2
                for g in range(3):
                    self.DMA("pool", wb[k][:, g, :, :], win[:, :, g * D + f * 128: g * D + (f + 1) * 128], (), [Bw[k]])
                for tc in range(4):
                    base = (tc % 2) * 3
                    pB, pC, pH = pp[base], pp[base + 1], pp[base + 2]
                    BpB, BpC, BpH = Bpp[base], Bpp[base + 1], Bpp[base + 2]
                    rT = [self.BxT[4 * tc + q] for q in range(4)]
                    for g, (po, Bpo) in enumerate(((pB, BpB), (pC, BpC), (pH, BpH))):
                        for kc in range(KC):
                            self.MM(po[:, :], wb[k][:, g, kc, :], self.xT[:, kc, tc * 512:(tc + 1) * 512],
                                    kc == 0, kc == KC - 1, [Bw[k]] + rT, [Bpo])
                    kk = tc % 2
                    self.CP("act", cs[kk][:, :], pC[:, :], [BpC], [Bcs[kk]])
                    self.TT("dve", ub[:, 2 + tc * 512: 2 + (tc + 1) * 512], pH[:, :], cs[kk][:, :], ALU.mult,
                            [BpH, Bcs[kk]], [Bu])
                    o = tc * 512
                    self.TS("dve", cvt[kk][:, :], ub[:, o:o + 512], ck[:, 0, f:f + 1], ALU.mult, [Bu, Bck], [Bcv[kk]])
                    self.STT("dve", cvt[kk][:, :], ub[:, o + 1:o + 513], ck[:, 1, f:f + 1], cvt[kk][:, :],
                             ALU.mult, ALU.add, [Bu, Bck, Bcv[kk]], [Bcv[kk]])
                    self.STT("dve", cvt[kk][:, :], ub[:, o + 2:o + 514], ck[:, 2, f:f + 1], cvt[kk][:, :],
                             ALU.mult, ALU.add, [Bu, Bck, Bcv[kk]], [Bcv[kk]])
                    self.TT("dve", zT[:, f, o:o + 512], pB[:, :], cvt[kk][:, :], ALU.mult, [BpB, Bcv[kk]], [Bz[f]])
            dst = d["out"] if to_out else d["xres"]
            py = [pp[6], pp[7]]
            Bpy = [Bpp[6], Bpp[7]]

            def yfn(i):
                for h in range(2):
                    for kc in range(KC):
                        self.MM(py[h][:, :], zT[:, kc, i * 128:(i + 1) * 128], wout[:, kc, h * 512:(h + 1) * 512],
                                kc == 0, kc == KC - 1, [Bz[kc], Bwout], [Bpy[h]])
                return [py[0][:, :], py[1][:, :]], Bpy

            def ptsfn(i):
                k = i % 2
                return [pp[0 + 2 * k], pp[1 + 2 * k]], [Bpp[0 + 2 * k], Bpp[1 + 2 * k]], pp[4 + k], Bpp[4 + k]
            self.ln_run(c, list(range(NT)), yfn, dst, ptsfn)
            S.flush()

    def phase_moe(self, l, to_out):
        S = self.S
        d = self.d
        with ExitStack() as es:
            cur_es = [es]
            sm = lambda name, shape, dt=F32: self.sb(cur_es[0], "mo_" + name, shape, dt)
            gates = sm("gates", [128, NT, 2]); desti = sm("desti", [128, NT, 2], I32); widx = sm("widx", [128, NBLK], I32)
            esA = ExitStack()
            cur_es[0] = esA
            lg = self.lg
            Blg = self.Blg
            lgg = lg[:, :, 0:8]
            lge = lg[:, :, 8:72].rearrange("p i (g e) -> p i g e", g=8)
            gmax = sm("gmax", [128, NT]); goh = sm("goh", [128, NT, 8]); gsh = sm("gsh", [128, NT, 8])
            gsum = sm("gsum", [128, NT]); ggate = sm("ggate", [128, NT])
            tmp4 = sm("tmp4", [128, NT, 8, 8]); sel = sm("sel", [128, NT, 8]); sel2 = sm("sel2", [128, NT, 8])
            v1 = sm("v1", [128, NT]); v2 = sm("v2", [128, NT]); oh1 = sm("oh1", [128, NT, 8]); oh2 = sm("oh2", [128, NT, 8])
            e2 = sm("e2", [128, NT]); p1 = sm("p1", [128, NT])
            A1 = sm("A1", [128, NT, 64]); A2 = sm("A2", [128, NT, 64]); Ab = sm("Ab", [128, NT, 64], BF16)
            B = Buf("route")
            bc8 = lambda ap: ap.unsqueeze(2).to_broadcast([128, NT, 8])
            self.RED(gmax[:, :], lgg, ALU.max, [Blg], [B])
            self.TT("dve", goh[:, :, :], lgg, bc8(gmax[:, :]), ALU.is_equal, [Blg, B], [B])
            self.TT("dve", gsh[:, :, :], lgg, bc8(gmax[:, :]), ALU.subtract, [Blg, B], [B])
            self.ACT(gsh[:, :, :], gsh[:, :, :], AF.Exp, [B], [B])
            self.RED(gsum[:, :], gsh[:, :, :], ALU.add, [B], [B])
            S.op("dve", lambda e: e.reciprocal(out=ggate[:, :], in_=gsum[:, :]), [B], [B])
            self.TT("dve", tmp4[:, :, :, :], lge, goh[:, :, :].unsqueeze(3).to_broadcast([128, NT, 8, 8]), ALU.mult,
                    [Blg, B], [B])
            self.RED(sel[:, :, :], tmp4[:, :, :, :].rearrange("p i g e -> p i e g"), ALU.add, [B], [B])
            self.RED(v1[:, :], sel[:, :, :], ALU.max, [B], [B])
            self.TT("dve", oh1[:, :, :], sel[:, :, :], bc8(v1[:, :]), ALU.is_equal, [B], [B])
            self.STT("dve", sel2[:, :, :], oh1[:, :, :], NEG, sel[:, :, :], ALU.mult, ALU.add, [B], [B])
            self.RED(v2[:, :], sel2[:, :, :], ALU.max, [B], [B])
            self.TT("dve", oh2[:, :, :], sel2[:, :, :], bc8(v2[:, :]), ALU.is_equal, [B], [B])
            self.TT("dve", e2[:, :], v2[:, :], v1[:, :], ALU.subtract, [B], [B])
            self.ACT(e2[:, :], e2[:, :], AF.Exp, [B], [B])
            self.TS("dve", p1[:, :], e2[:, :], 1.0, ALU.add, [B], [B])
            S.op("dve", lambda e: e.reciprocal(out=p1[:, :], in_=p1[:, :]), [B], [B])
            self.TT("dve", gates[:, :, 0], p1[:, :], ggate[:, :], ALU.mult, [B], [B])
            self.TT("dve", e2[:, :], e2[:, :], p1[:, :], ALU.mult, [B], [B])
            self.TT("dve", gates[:, :, 1], e2[:, :], ggate[:, :], ALU.mult, [B], [B])
            gb = goh[:, :, :].unsqueeze(3).to_broadcast([128, NT, 8, 8])
            for Ak, ohk in ((A1, oh1), (A2, oh2)):
                self.TT("dve", Ak[:, :, :].rearrange("p i (g e) -> p i g e", g=8), gb,
                        ohk[:, :, :].unsqueeze(2).to_broadcast([128, NT, 8, 8]), ALU.mult, [B], [B])
            self.TT("dve", Ab[:, :, :], A1[:, :, :], A2[:, :, :], ALU.add, [B], [B])
            pp = [self.ps(es, "mo_pp%d" % k, [128, 512], F32) for k in range(6)]
            Bpp = [Buf() for _ in range(6)]
            pcs, ppf = pp[0:2], pp[2:4]
            Bpcs, Bppf = Bpp[0:2], Bpp[2:4]
            Abf = Ab[:, :, :].rearrange("p i e -> p (i e)")
            for h in range(2):
                self.MM(pcs[h][:, :], self.ones_b[:, :], Abf[:, h * 512:(h + 1) * 512], True, True, [B, self.Bconst], [Bpcs[h]])
                self.MM(ppf[h][:, :], self.tri_b[:, :], Abf[:, h * 512:(h + 1) * 512], True, True, [B, self.Bconst], [Bppf[h]])
            cs = sm("cs", [128, NT, 64]); base = sm("base", [128, NT, 64]); carry = sm("carry", [128, NT, 64])
            csf = cs[:, :, :].rearrange("p i e -> p (i e)")
            bsf = base[:, :, :].rearrange("p i e -> p (i e)")
            for h in range(2):
                self.CP("dve", csf[:, h * 512:(h + 1) * 512], pcs[h][:, :], [Bpcs[h]], [B])
                self.CP("act", bsf[:, h * 512:(h + 1) * 512], ppf[h][:, :], [Bppf[h]], [B])
            self.MEMSET("dve", carry[:, 0, :], 0.0, [B])
            for i in range(1, NT):
                self.TT("dve", carry[:, i, :], carry[:, i - 1, :], cs[:, i - 1, :], ALU.add, [B], [B])
            cnt = sm("cnt", [128, 64]); cnti = sm("cnti", [128, 64], I32); padf = sm("padf", [128, 64])
            sc = [sm("scan%d" % k, [128, 64]) for k in range(2)]
            self.TT("dve", cnt[:, :], carry[:, NT - 1, :], cs[:, NT - 1, :], ALU.add, [B], [B])
            self.TS("dve", cnti[:, :], cnt[:, :], 127.0, ALU.add, [B], [B])
            self.TS("dve", cnti[:, :], cnti[:, :], 7, ALU.arith_shift_right, [B], [B])
            self.TS("dve", cnti[:, :], cnti[:, :], 7, ALU.logical_shift_left, [B], [B])
            self.CP("dve", padf[:, :], cnti[:, :], [B], [B])
            self.CP("dve", sc[0][:, :], padf[:, :], [B], [B])
            cur = 0
            s = 1
            while s < 64:
                a, b = sc[cur], sc[1 - cur]
                self.CP("dve", b[:, 0:s], a[:, 0:s], [B], [B])
                self.TT("dve", b[:, s:64], a[:, s:64], a[:, 0:64 - s], ALU.add, [B], [B])
                cur = 1 - cur
                s *= 2
            pend = sc[cur]
            pstart = sm("pstart", [128, 64])
            self.TT("dve", pstart[:, :], pend[:, :], padf[:, :], ALU.subtract, [B], [B])
            self.TT("dve", base[:, :, :], base[:, :, :], carry[:, :, :], ALU.add, [B], [B])
            self.TT("dve", base[:, :, :], base[:, :, :], pstart[:, :].unsqueeze(1).to_broadcast([128, NT, 64]), ALU.add,
                    [B], [B])
            destf = sm("destf", [128, NT, 2])
            for kk, Ak in enumerate((A1, A2)):
                self.TT("dve", cs[:, :, :], Ak[:, :, :], base[:, :, :], ALU.mult, [B], [B])
                self.RED(destf[:, :, kk], cs[:, :, :], ALU.add, [B], [B])
            Bdest = Buf("dest")
            self.CP("dve", desti[:, :, :], destf[:, :, :], [B], [Bdest])
            cmp = sm("cmp", [128, NBLK, 64], BF16); blke = sm("blke", [128, NBLK])
            self.TT("dve", cmp[:, :, :], pend[:, :].unsqueeze(1).to_broadcast([128, NBLK, 64]),
                    self.bvals[:, :].unsqueeze(2).to_broadcast([128, NBLK, 64]), ALU.is_le, [B, self.Bconst], [B])
            self.RED(blke[:, :], cmp[:, :, :], ALU.add, [B], [B])
            self.TS("dve", blke[:, :], blke[:, :], 128.0, ALU.mult, [B], [B])
            Bwidx = Buf("widx")
            self.TS("dve", widx[:, :], blke[:, :], self.pidx[:, 0:1], ALU.add, [B, self.Bconst], [Bwidx])
            Bxs = Buf("xs")
            for i in range(NT):
                for kk in range(2):
                    self.SCATTER(d["xs"], desti[:, i, kk:kk + 1], self.xb[:, i, :], [Bdest, self.Bxb[i]], [Bxs])
            S.flush()
            esA.close()
            esB = ExitStack()
            cur_es[0] = esB
            NB = 5
            xsb = [sm("xsb%d" % k, [128, D], BF16) for k in range(5)]; Bxsb = [Buf() for _ in range(5)]
            xsT = [sm("xsT%d" % k, [128, KC, 128], BF16) for k in range(2)]; BxsT = [Buf(), Buf()]
            w1b = [sm("w1b%d" % k, [128, KC, 256], BF16) for k in range(NB)]; Bw1 = [Buf() for _ in range(NB)]
            w3b = [sm("w3b%d" % k, [128, KC, 256], BF16) for k in range(NB)]; Bw3 = [Buf() for _ in range(NB)]
            w2b = [sm("w2b%d" % k, [128, 2, D], BF16) for k in range(NB)]; Bw2 = [Buf() for _ in range(NB)]
            sa = [sm("sa%d" % k, [128, 256]) for k in range(2)]; Bsa = [Buf(), Buf()]
            hT = [sm("hT%d" % k, [128, 2, 128], BF16) for k in range(2)]; BhT = [Buf(), Buf()]
            ysb = [sm("ysb%d" % k, [128, D]) for k in range(2)]; Bysb = [Buf(), Buf()]
            ptr = [self.ps(es, "mo_ptr%d" % k, [128, 1024], BF16) for k in range(2)]; Bptr = [Buf(), Buf()]
            ph = pp[0:2]; Bph = Bpp[0:2]
            pys = pp[2:4]; Bpys = Bpp[2:4]
            Bys = Buf("ys")
            w1t, w3t, w2t = d["w1r%d" % l], d["w3r%d" % l], d["w2r%d" % l]
            PF = 4
            S.streams["pool"].append(("raw", lambda e: setattr(self, "bound_reg", e.to_reg(64 * 128 - 1))))

            def issue_loads(b):
                kx = b % 5
                kw = b % NB
                self.DMA("act", xsb[kx][:, :], d["xs"][b * 128:(b + 1) * 128, :], [Bxs], [Bxsb[kx]])
                self.GATHER(w1b[kw][:, :, :].rearrange("p k f -> p (k f)"), w1t, widx[:, b:b + 1], [Bwidx], [Bw1[kw]], bound=64 * 128 - 1)
                self.GATHER(w3b[kw][:, :, :].rearrange("p k f -> p (k f)"), w3t, widx[:, b:b + 1], [Bwidx], [Bw3[kw]], bound=64 * 128 - 1)
                self.GATHER(w2b[kw][:, :, :].rearrange("p k f -> p (k f)"), w2t, widx[:, b:b + 1], [Bwidx], [Bw2[kw]], bound=64 * 128 - 1)
            for b in range(PF):
                issue_loads(b)
            for b in range(NBLK):
                k = b % 2
                kw = b % NB
                kx = b % 5
                if b + PF < NBLK:
                    issue_loads(b + PF)
                for kc in range(KC):
                    self.TR(ptr[k][:, kc * 128:(kc + 1) * 128], xsb[kx][:, kc * 128:(kc + 1) * 128], self.ident_b[:],
                            [Bxsb[kx], self.Bconst], [Bptr[k]])
                self.CP("dve", xsT[k][:, :, :].rearrange("p k r -> p (k r)"), ptr[k][:, :], [Bptr[k]], [BxsT[k]])
                for gi, (wt, Bwt) in enumerate(((w1b[kw], Bw1[kw]), (w3b[kw], Bw3[kw]))):
                    for fc in range(2):
                        col = (gi * 2 + fc) * 128
                        for kc in range(KC):
                            self.MM(ph[k][:, col:col + 128], wt[:, kc, fc * 128:(fc + 1) * 128], xsT[k][:, kc, :],
                                    kc == 0, kc == KC - 1, [Bwt, BxsT[k]], [Bph[k]])
                self.ACT(sa[k][:, :], ph[k][:, 0:256], AF.Silu, [Bph[k]], [Bsa[k]])
                self.TT("dve", hT[k][:, :, :].rearrange("p c r -> p (c r)"), sa[k][:, :], ph[k][:, 256:512], ALU.mult,
                        [Bsa[k], Bph[k]], [BhT[k]])
                for h in range(2):
                    py = pys[h]
                    for fc in range(2):
                        self.MM(py[:, :], hT[k][:, fc, :], w2b[kw][:, fc, h * 512:(h + 1) * 512], fc == 0, fc == 1,
                                [BhT[k], Bw2[kw]], [Bpys[h]])
                    self.CP("act" if h == 0 else "dve", ysb[k][:, h * 512:(h + 1) * 512], py[:, :],
                            [Bpys[h]], [Bysb[k]])
                self.DMA("sp", d["ys"][b * 128:(b + 1) * 128, :], ysb[k][:, :], [Bysb[k]], [Bys])
            esC = ExitStack()
            cur_es[0] = esC
            c = self.ln_setup(esC, "m2", d["ln2_g"][l], d["ln2_b"][l], l, router=False)
            y0 = [sm("y0_%d" % k, [128, D]) for k in range(2)]; By0 = [Buf(), Buf()]
            y1 = [sm("y1_%d" % k, [128, D]) for k in range(2)]; By1 = [Buf(), Buf()]
            dst = d["out"] if to_out else d["xres"]
            def issue_gather(i):
                k = i % 2
                self.GATHER(y0[k][:, :], d["ys"], desti[:, i, 0:1], [Bdest, Bys], [By0[k]])
                self.GATHER(y1[k][:, :], d["ys"], desti[:, i, 1:2], [Bdest, Bys], [By1[k]])
            issue_gather(0)

            def yfn(i):
                k = i % 2
                self.TS("dve", y0[k][:, :], y0[k][:, :], gates[:, i, 0:1], ALU.mult, [By0[k], B], [By0[k]])
                self.STT("dve", y0[k][:, :], y1[k][:, :], gates[:, i, 1:2], y0[k][:, :], ALU.mult, ALU.add,
                         [By1[k], By0[k], B], [By0[k]])
                if i + 1 < NT:
                    issue_gather(i + 1)
                return [y0[k][:, 0:512], y0[k][:, 512:1024]], [By0[k], By0[k]]

            def ptsfn(i):
                k = i % 2
                return [pp[2 * k], pp[2 * k + 1]], [Bpp[2 * k], Bpp[2 * k + 1]], None, None
            self.ln_run(c, list(range(NT)), yfn, dst, ptsfn)
            S.flush()
            esC.close()
            esB.close()

    def phase_attn(self, l, to_out):
        S = self.S
        d = self.d
        j = l // 2
        awin = d["awin%d" % j].rearrange("(k p) f -> p k f", p=128)
        import os
        att_stop = int(os.environ.get("ATT_STOP", "9"))
        if att_stop <= 0:
            return
        qd = d["qT"]
        with ExitStack() as es0:
            sm0 = lambda name, shape, dt=F32: self.sb(es0, "at_" + name, shape, dt)
            ckv_tm = sm0("ckv_tm", [128, NT, 128], BF16); Bckv = Buf("ckv_tm")
            ckvT = sm0("ckvT", [128, SEQ], BF16); BckvT = Buf("ckvT")
            with ExitStack() as es:
                sm = lambda name, shape, dt=F32: self.sb(es, "a1_" + name, shape, dt)
                pp = [self.ps(es, "a1_pp%d" % k, [128, 512], F32) for k in range(6)]
                Bpp = [Buf() for _ in range(6)]
                ptb = [self.ps(es, "a1_ptb%d" % k, [128, 1024], BF16) for k in range(2)]
                Bptb = [Buf(), Buf()]
                kiT = sm("kiT", [128, SEQ], BF16); BkiT = Buf("kiT")
                qiT = sm("qiT", [64, NIH, SEQ], BF16); BqiT = [Buf() for _ in range(NIH)]
                wi = sm("wi", [128, NT, 8]); Bwi = Buf("wi")
                wckv = sm("wckv", [128, KC, 128], BF16); wki = sm("wki", [128, KC, 72], BF16); Bwa = Buf()
                kvg = sm("kvg", [128, 128]); kig = sm("kig", [128, 64]); kib = sm("kib", [128, 64]); Bg = Buf()
                self.DMA("pool", wckv[:, :, :], awin[:, :, 2048:2176], (), [Bwa])
                self.DMA("pool", wki[:, :, :], awin[:, :, 2688:2760], (), [Bwa])
                self.DMA("sp", kvg[:, :], self.bcast_rows(d["kv_g"][j], 128), (), [Bg])
                self.DMA("sp", kig[:, :], self.bcast_rows(d["ki_g"][j], 64), (), [Bg])
                self.DMA("sp", kib[:, :], self.bcast_rows(d["ki_b"][j], 64), (), [Bg])
                st1 = [sm("st%d" % k, [128, 24]) for k in range(2)]; Bst1 = [Buf(), Buf()]
                junk = [sm("junk%d" % k, [128, 128]) for k in range(2)]
                kin = [sm("kin%d" % k, [128, 128], BF16) for k in range(2)]; Bkin = [Buf(), Buf()]
                ktmp = [sm("ktmp%d" % k, [128, 64]) for k in range(2)]
                bis = int(os.environ.get("BIS", "9"))
                def ck_A(i):
                    k = i % 2
                    pa, Bpa = pp[k], Bpp[k]
                    for kc in range(KC):
                        self.MM(pa[:, 0:128], self.xT[:, kc, i * 128:(i + 1) * 128], wckv[:, kc, :], kc == 0, kc == KC - 1,
                                [self.BxT[i], Bwa], [Bpa])
                    for kc in range(KC):
                        self.MM(pa[:, 128:200], self.xT[:, kc, i * 128:(i + 1) * 128], wki[:, kc, :], kc == 0, kc == KC - 1,
                                [self.BxT[i], Bwa], [Bpa])

                def ck_B(i):
                    k = i % 2
                    pa, Bpa = pp[k], Bpp[k]
                    st, Bs = st1[k], Bst1[k]
                    self.ACT(junk[k][:, :], pa[:, 0:128], AF.Square, [Bpa], [Bs], accum=st[:, 0:1])
                    self.ACT(st[:, 1:2], st[:, 0:1], AF.Sqrt, [Bs, self.Bconst], [Bs], scale=1.0 / 128.0, bias=self.eps_rms[:, 0:1])
                    S.op("dve", (lambda s_: (lambda e: e.reciprocal(out=s_[:, 1:2], in_=s_[:, 1:2])))(st), [Bs], [Bs])
                    self.STT("dve", ckv_tm[:, i, :], pa[:, 0:128], st[:, 1:2], kvg[:, :], ALU.mult, ALU.mult,
                             [Bpa, Bs, Bg], [Bckv])
                    S.op("dve", (lambda s_, p_: (lambda e: e.bn_stats(out=s_[:, 8:14], in_=p_[:, 128:192])))(st, pa), [Bpa], [Bs])
                    S.op("dve", (lambda s_: (lambda e: e.bn_aggr(out=s_[:, 14:16], in_=s_[:, 8:14])))(st), [Bs], [Bs])
                    self.ACT(st[:, 16:17], st[:, 15:16], AF.Sqrt, [Bs, self.Bconst], [Bs], scale=1.0, bias=self.eps_ln[:, 0:1])
                    S.op("dve", (lambda s_: (lambda e: e.reciprocal(out=s_[:, 16:17], in_=s_[:, 16:17])))(st), [Bs], [Bs])
                    self.TS("dve", ktmp[k][:, :], pa[:, 128:192], st[:, 14:15], ALU.subtract, [Bpa, Bs], [Bkin[k]],
                            s2=st[:, 16:17], op1=ALU.mult)
                    self.TT("dve", ktmp[k][:, :], ktmp[k][:, :], kig[:, :], ALU.mult, [Bkin[k], Bg], [Bkin[k]])
                    self.TT("dve", kin[k][:, 0:64], ktmp[k][:, :], kib[:, :], ALU.add, [Bkin[k], Bg], [Bkin[k]])
                    self.TT("dve", kin[k][:, 64:128], ktmp[k][:, :], kib[:, :], ALU.add, [Bkin[k], Bg], [Bkin[k]])
                    self.CP("act", wi[:, i, :], pa[:, 192:200], [Bpa], [Bwi])

                def ck_C(i):
                    k = i % 2
                    self.TR(ptb[k][:, 0:128], ckv_tm[:, i, :], self.ident_b[:], [Bckv, self.Bconst], [Bptb[k]])
                    self.TR(ptb[k][:, 128:256], kin[k][:, :], self.ident_b[:], [Bkin[k], self.Bconst], [Bptb[k]])
                    self.CP("dve", ckvT[:, i * 128:(i + 1) * 128], ptb[k][:, 0:128], [Bptb[k]], [BckvT])
                    self.CP("dve", kiT[:, i * 128:(i + 1) * 128], ptb[k][:, 128:256], [Bptb[k]], [BkiT])
                for s in range(NT + 2):
                    if s < NT:
                        ck_A(s)
                    if 0 <= s - 1 < NT:
                        ck_B(s - 1)
                    if 0 <= s - 2 < NT:
                        ck_C(s - 2)
                if att_stop <= 1:
                    S.flush()
                    return
                wq = [sm("wq%d" % k, [128, KC, 128], BF16) for k in range(2)]; Bwq = [Buf(), Buf()]
                qs = [sm("qs%d" % k, [128, 512], BF16) for k in range(4)]; Bqs = [Buf() for _ in range(4)]
                Bqd = Buf("qd")
                n = 0
                for h in range(NH):
                    k = h % 2
                    self.DMA("pool", wq[k][:, :, :], awin[:, :, h * 128:(h + 1) * 128], (), [Bwq[k]])
                    for tc in range(4):
                        po, Bpo = pp[2 + n % 4], Bpp[2 + n % 4]
                        for kc in range(KC):
                            self.MM(po[:, :], wq[k][:, kc, :], self.xT[:, kc, tc * 512:(tc + 1) * 512], kc == 0, kc == KC - 1,
                                    [Bwq[k]] + self.BxT[4 * tc:4 * tc + 4], [Bpo])
                        if n % 2:
                            self.ACT(qs[n % 4][:, :], po[:, :], AF.Copy, [Bpo], [Bqs[n % 4]], scale=128.0 ** -0.5)
                        else:
                            self.TS("dve", qs[n % 4][:, :], po[:, :], 128.0 ** -0.5, ALU.mult, [Bpo], [Bqs[n % 4]])
                        self.DMA("sp", qd[h, :, tc * 512:(tc + 1) * 512], qs[n % 4][:, :], [Bqs[n % 4]], [Bqd])
                        n += 1
                wqi = [sm("wqi%d" % k, [128, KC, 64], BF16) for k in range(2)]; Bwqi = [Buf(), Buf()]
                for h in range(NIH):
                    k = h % 2
                    self.DMA("pool", wqi[k][:, :, :], awin[:, :, 2176 + h * 64:2176 + (h + 1) * 64], (), [Bwqi[k]])
                    for tc in range(4):
                        po, Bpo = pp[2 + n % 4], Bpp[2 + n % 4]
                        for kc in range(KC):
                            self.MM(po[0:64, :], wqi[k][:, kc, :], self.xT[:, kc, tc * 512:(tc + 1) * 512], kc == 0, kc == KC - 1,
                                    [Bwqi[k]] + self.BxT[4 * tc:4 * tc + 4], [Bpo])
                        self.CP("act" if n % 2 else "dve", qiT[:, h, tc * 512:(tc + 1) * 512], po[0:64, :], [Bpo], [BqiT[h]])
                        n += 1
                if att_stop <= 2:
                    S.flush()
                    return
                U32 = mybir.dt.uint32
                NIT = 18
                sc4s = [sm("sc4_%d" % k, [128, 4, SEQ]) for k in range(2)]
                Bscs = [[Buf() for _ in range(4)] for _ in range(2)]
                junkb = sm("junkb", [128, SEQ], BF16)
                junka = junkb
                lhs_ = [sm("lohi%d" % k, [128, 5, 4]) for k in range(2)]; Blhs = [Buf("lohi0"), Buf("lohi1")]
                Bcas = [Buf("cnta0"), Buf("cnta1")]; Bcds = [Buf("cntd0"), Buf("cntd1")]; Btvs = [Buf("tv0"), Buf("tv1")]
                tvs = [sm("tv%d" % k, [128, 4]) for k in range(2)]
                pm = sm("predm", [128, 2, 4], U32)
                NRL = 12
                rl = [sm("rl%d" % k, [128, 512]) for k in range(NRL)]; Brl = [Buf() for _ in range(NRL)]
                maskb = [sm("maskb%d" % k, [128, SEQ], BF16) for k in range(1)] * 2; Bmk = [Buf()] * 2
                mT = [sm("mT%d" % k, [128, NT, 128], BF16) for k in range(1)] * 2; BmT = [Buf()] * 2
                Bmd = Buf("maskT_d")
                self.Bmd = Bmd
                cnt_ps = [0]

                def score_steps(g):
                    sc4, Bsc, lh, Blh = sc4s[g % 2], Bscs[g % 2], lhs_[g % 2], Blhs[g % 2]
                    steps = []
                    for q in range(4):
                        qb = 4 * g + q
                        L = (qb + 1) * 128
                        for c0 in range(0, L, 512):
                            nn = min(512, L - c0)
                            for h in range(NIH):
                                def mk_step(q=q, qb=qb, c0=c0, nn=nn, h=h):
                                    stt = {}

                                    def p1():
                                        i_ = cnt_ps[0]
                                        cnt_ps[0] += 1
                                        pS, BpS = pp[i_ % 2], Bpp[i_ % 2]
                                        r_, Br_ = rl[i_ % NRL], Brl[i_ % NRL]
                                        stt["r"] = (r_, Br_)
                                        self.MM(pS[:, 0:nn], qiT[:, h, qb * 128:(qb + 1) * 128], kiT[0:64, c0:c0 + nn], True, True,
                                                [BqiT[h], BkiT], [BpS])
                                        self.ACT(r_[:, 0:nn], pS[:, 0:nn], AF.Relu, [BpS], [Br_])

                                    def p2():
                                        r_, Br_ = stt["r"]
                                        sc_ = sc4[:, q, :]
                                        Bs_ = Bsc[q]
                                        if h == 0:
                                            self.TS("dve", sc_[:, c0:c0 + nn], r_[:, 0:nn], wi[:, qb, 0:1], ALU.mult, [Br_, Bwi], [Bs_])
                                        else:
                                            self.STT("dve", sc_[:, c0:c0 + nn], r_[:, 0:nn], wi[:, qb, h:h + 1], sc_[:, c0:c0 + nn],
                                                     ALU.mult, ALU.add, [Br_, Bwi, Bs_], [Bs_])
                                    return p1, p2
                                steps.append(mk_step())

                        def post(q=q, qb=qb, L=L):
                            sc_ = sc4[:, q, :]
                            Bs_ = Bsc[q]
                            if qb >= 2:
                                self.RED(lh[:, 0, q:q + 1], sc_[:, 0:L], ALU.min, [Bs_], [Blh])
                            dg = sc_[:, qb * 128:(qb + 1) * 128]
                            S.op("pool", (lambda a: (lambda e: e.affine_select(out=a, in_=a, pattern=[[-1, 128]], compare_op=ALU.is_ge,
                                                                              fill=NEG, base=0, channel_multiplier=1)))(dg), [Bs_], [Bs_])
                            if qb >= 2:
                                self.RED(lh[:, 1, q:q + 1], sc_[:, 0:L], ALU.max, [Bs_], [Blh])
                                Lq = L
                                act_q = (q >= (2 if g == 3 else 1)) if g > 0 else (q == 3)
                                self.MEMSET("pool", tvs[g % 2][:, q:q + 1], float(2 * TOPK - Lq) if act_q else float(TOPK), [Btvs[g % 2]])
                        steps.append((lambda: None, post))
                    return steps

                def bisect_first(g):
                    sc4, Bsc, lh, Blh, tv = sc4s[g % 2], Bscs[g % 2], lhs_[g % 2], Blhs[g % 2], tvs[g % 2]
                    Bca, Bcd = Bcas[g % 2], Bcds[g % 2]
                    qs_need = [q for q in range(4) if 4 * g + q >= 2]
                    q0 = qs_need[0]
                    act_qs = [q for q in qs_need if ((q >= (2 if g == 3 else 1)) if g > 0 else (q == 3))]
                    self.TT("dve", lh[:, 2, q0:4], lh[:, 0, q0:4], lh[:, 1, q0:4], ALU.add, [Blh], [Blh])
                    self.TS("dve", lh[:, 2, q0:4], lh[:, 2, q0:4], 0.5, ALU.mult, [Blh], [Blh])
                    self.TS("dve", lh[:, 4, q0:4], lh[:, 2, q0:4], -1.0, ALU.mult, [Blh], [Blh])
                    for q in qs_need:
                        L = (4 * g + q + 1) * 128
                        if q in act_qs:
                            self.ACT(junka[:, 0:L], sc4[:, q, 0:L], AF.Sign, [Bsc[q], Blh], [Bca], bias=lh[:, 4, q:q + 1],
                                     scale=1.0, accum=lh[:, 3, q:q + 1])
                    for q in qs_need:
                        L = (4 * g + q + 1) * 128
                        if q not in act_qs:
                            self.TS("dve", junkb[:, 0:L], sc4[:, q, 0:L], lh[:, 2, q:q + 1], ALU.is_ge, [Bsc[q], Blh], [Bcd],
                                    op1=ALU.add, accum=lh[:, 3, q:q + 1])

                def bisect_second(g):
                    lh, Blh, tv = lhs_[g % 2], Blhs[g % 2], tvs[g % 2]
                    Bca, Bcd, Btv = Bcas[g % 2], Bcds[g % 2], Btvs[g % 2]
                    qs_need = [q for q in range(4) if 4 * g + q >= 2]
                    q0 = qs_need[0]
                    self.TT("dve", pm[:, 0, q0:4], lh[:, 3, q0:4], tv[:, q0:4], ALU.is_ge, [Bca, Bcd, Btv], [Blh])
                    self.TT("dve", pm[:, 1, q0:4], lh[:, 3, q0:4], tv[:, q0:4], ALU.is_lt, [Bca, Bcd, Btv], [Blh])
                    S.op("dve", (lambda a, l_: (lambda e: e.copy_predicated(out=l_[:, 0, a:4], mask=pm[:, 0, a:4], data=l_[:, 2, a:4])))(q0, lh),
                         [Blh], [Blh])
                    S.op("dve", (lambda a, l_: (lambda e: e.copy_predicated(out=l_[:, 1, a:4], mask=pm[:, 1, a:4], data=l_[:, 2, a:4])))(q0, lh),
                         [Blh], [Blh])

                def emit_masks(g):
                    sc4, Bsc, lh, Blh = sc4s[g % 2], Bscs[g % 2], lhs_[g % 2], Blhs[g % 2]
                    for q in range(4):
                        qb = 4 * g + q
                        k = qb % 2
                        L = (qb + 1) * 128
                        if qb >= 2:
                            self.TS("dve", maskb[k][:, 0:L], sc4[:, q, 0:L], lh[:, 0, q:q + 1], ALU.is_lt, [Bsc[q], Blh], [Bmk[k]],
                                    s2=MASK_NEG, op1=ALU.mult)
                        else:
                            self.TS("dve", maskb[k][:, 0:L], sc4[:, q, 0:L], -1.0e29, ALU.is_lt, [Bsc[q]], [Bmk[k]],
                                    s2=MASK_NEG, op1=ALU.mult)
                        for g0 in range(0, qb + 1, 8):
                            gn = min(8, qb + 1 - g0)
                            pt, Bpt = ptb[(g0 // 8) % 2], Bptb[(g0 // 8) % 2]
                            for qq in range(gn):
                                st_ = g0 + qq
                                self.TR(pt[:, qq * 128:(qq + 1) * 128], maskb[k][:, st_ * 128:(st_ + 1) * 128], self.ident_b[:],
                                        [Bmk[k], self.Bconst], [Bpt])
                            self.CP("dve", mT[k][:, g0:g0 + gn, :].rearrange("p s t -> p (s t)"), pt[:, 0:gn * 128], [Bpt], [BmT[k]])
                        self.DMA("sp", d["maskT"][0:L, qb * 128:(qb + 1) * 128].rearrange("(s p) t -> p s t", p=128),
                                 mT[k][:, 0:qb + 1, :], [BmT[k]], [Bmd])

                for p1, p2 in score_steps(0):
                    p1()
                    p2()
                for g in range(4):
                    nxt = score_steps(g + 1) if g < 3 else []
                    per = (len(nxt) + NIT - 1) // NIT if nxt else 0
                    batches = [nxt[it * per:(it + 1) * per] for it in range(NIT)]
                    for p1, _ in batches[0]:
                        p1()
                    for it in range(NIT):
                        bisect_first(g)
                        nb_ = batches[it + 1] if it + 1 < NIT else []
                        hb_ = len(nb_) // 2
                        for p1, _ in nb_[:hb_]:
                            p1()
                        for _, p2 in batches[it]:
                            p2()
                        for p1, _ in nb_[hb_:]:
                            p1()
                        bisect_second(g)
                    emit_masks(g)
                S.flush()
            if att_stop <= 3:
                return
            with ExitStack() as es:
                sm = lambda name, shape, dt=F32: self.sb(es, "a3_" + name, shape, dt)
                pp = [self.ps(es, "a3_pp%d" % k, [128, 512], F32) for k in range(8)]
                Bpp = [Buf() for _ in range(8)]
                c = self.ln_setup(es, "a3", d["ln1_g"][l], d["ln1_b"][l], l, router=not to_out, nbuf=2)
                wout = sm("wout", [128, NH, D], BF16); Bwout = Buf()
                self.DMA("pool", wout[:, :, :], d["awout%d" % j].rearrange("(h c) f -> c h f", c=128), (), [Bwout])
                qtc = [sm("qtc%d" % k, [128, NH, 512], BF16) for k in range(2)]; Bqtc = [Buf(), Buf()]
                mk = sm("mk", [128, NT, 512], BF16); Bmk3 = Buf()
                oT = sm("oT", [128, NH, 512], BF16); BoT = [Buf() for _ in range(NH)]
                pT = [sm("pT%d" % k, [128, 512], BF16) for k in range(3)]; BpT = [Buf() for _ in range(3)]
                rec = sm("rec", [128, 512]); Brec = Buf()
                dst = d["out"] if to_out else d["xres"]
                npt = 0
                npl = 0
                for tc in range(4):
                    kq = tc % 2
                    nst = 4 * (tc + 1)
                    self.DMA("sp", qtc[kq][:, :, :], qd[:, :, tc * 512:(tc + 1) * 512].rearrange("h c t -> c h t"),
                             [Bqd], [Bqtc[kq]])
                    self.DMA("sp", mk[:, 0:nst, :],
                             d["maskT"][0:nst * 128, tc * 512:(tc + 1) * 512].rearrange("(s p) t -> p s t", p=128),
                             [Bmd], [Bmk3])
                    for h in range(NH):
                        po, Bpo = pp[2 + h % 2], Bpp[2 + h % 2]
                        pd, Bpd = pp[4 + h % 2], Bpp[4 + h % 2]

                        def emit_qk(st_, h=h):
                            nonlocal npl
                            c0 = max(0, st_ - 4 * tc) * 128
                            pl, Bpl = pp[npl % 2], Bpp[npl % 2]
                            npl += 1
                            extra = []
                            for dl in (0, 1):
                                tt = st_ + dl
                                if 4 * tc <= tt <= 4 * tc + 3:
                                    extra.append((dl, (tt - 4 * tc) * 128))
                            self.MM(pl[:, c0:512], ckvT[:, st_ * 128:(st_ + 1) * 128], qtc[kq][:, h, c0:512], True, False,
                                    [BckvT, Bqtc[kq]], [Bpl])
                            for ei, (dl, cc) in enumerate(extra):
                                self.MM(pl[:, cc:cc + 128], self.ident_b[:, :], self.BtT[:, h, dl, :], False, False,
                                        [self.Bconst], [Bpl])
                            self.MM(pl[:, c0:512], self.ident_b[:, :], mk[:, st_, c0:512], False, True, [self.Bconst, Bmk3], [Bpl])
                            return pl, Bpl, c0
                        nxt = emit_qk(0)
                        for st_ in range(nst):
                            pl, Bpl, c0 = nxt
                            if st_ + 1 < nst:
                                nxt = emit_qk(st_ + 1)
                            p_, Bp_ = pT[npt % 3], BpT[npt % 3]
                            npt += 1
                            self.ACT(p_[:, c0:512], pl[:, c0:512], AF.Exp, [Bpl], [Bp_])
                            self.MM(po[:, c0:512], ckv_tm[:, st_, :], p_[:, c0:512], st_ == 0, st_ == nst - 1, [Bckv, Bp_], [Bpo])
                            self.MM(pd[:, c0:512], self.ones_b[:, :], p_[:, c0:512], st_ == 0, st_ == nst - 1, [self.Bconst, Bp_], [Bpd])
                        S.op("dve", (lambda pd_: (lambda e: e.reciprocal(out=rec[:, :], in_=pd_[:, :])))(pd), [Bpd], [Brec])
                        self.TT("dve", oT[:, h, :], po[:, :], rec[:, :], ALU.mult, [Bpo, Brec], [BoT[h]])
                    py = [pp[6], pp[7]]
                    Bpy = [Bpp[6], Bpp[7]]

                    def yfn(i, tc=tc):
                        q = i - 4 * tc
                        for hf in range(2):
                            for h in range(NH):
                                self.MM(py[hf][:, :], oT[:, h, q * 128:(q + 1) * 128], wout[:, h, hf * 512:(hf + 1) * 512],
                                        h == 0, h == NH - 1, [BoT[h], Bwout], [Bpy[hf]])
                        return [py[0][:, :], py[1][:, :]], Bpy

                    def ptsfn(i):
                        return [pp[0], pp[1]], [Bpp[0], Bpp[1]], pp[2], Bpp[2]
                    self.ln_run(c, [4 * tc + q for q in range(4)], yfn, dst, ptsfn)
                S.flush()


def _t5_onehot():
    n_buckets, max_distance = 32, 128
    max_exact = n_buckets // 2
    oh = np.zeros((32, VW), np.float32)
    for jj in range(VW):
        dd = jj - 127
        if dd < 0:
            continue
        d_f = np.float32(max(dd, 1))
        large = max_exact + int(np.float32(np.log(d_f / np.float32(max_exact), dtype=np.float32)
                                           / np.float32(np.log(max_distance / max_exact))
                                           * np.float32(n_buckets - max_exact)))
        large = min(large, n_buckets - 1)
        bkt = dd if dd < max_exact else large
        oh[bkt, jj] += 1.0
        oh[n_buckets - 1, jj] -= 1.0
    return oh


def _prep_shared(inputs):
    f = lambda a: np.ascontiguousarray(np.asarray(a, dtype=np.float32))
    m = {}
    for j in range(2):
        m["cwin%d" % j] = f(inputs["conv_w_in"][j])
        m["cwout%d" % j] = f(inputs["conv_w_out"][j])
        m["awin%d" % j] = f(inputs["attn_w_in"][j])
        m["awout%d" % j] = f(inputs["attn_w_out"][j])
    m["conv_k"] = f(inputs["conv_k"])
    m["kv_g"] = f(inputs["kv_norm_g"])
    m["ki_g"] = f(inputs["kidx_ln_g"])
    m["ki_b"] = f(inputs["kidx_ln_b"])
    m["rel_bias"] = f(inputs["rel_bias"])
    m["ohs"] = _t5_onehot()
    m["rw"] = f(np.concatenate([np.asarray(inputs["router_wg"]), np.asarray(inputs["router_we"])], axis=2))
    m["rb"] = f(np.concatenate([np.asarray(inputs["router_bg"]), np.asarray(inputs["router_be"])], axis=1))
    for l in range(DEPTH):
        w1 = np.asarray(inputs["exp_w1"][l], dtype=np.float32).reshape(64, KC, 128, 256)
        m["w1r%d" % l] = np.ascontiguousarray(w1.transpose(0, 2, 1, 3)).reshape(64 * 128, 2048)
        w3 = np.asarray(inputs["exp_w3"][l], dtype=np.float32).reshape(64, KC, 128, 256)
        m["w3r%d" % l] = np.ascontiguousarray(w3.transpose(0, 2, 1, 3)).reshape(64 * 128, 2048)
        w2 = np.asarray(inputs["exp_w2"][l], dtype=np.float32).reshape(64, 2, 128, D)
        m["w2r%d" % l] = np.ascontiguousarray(w2.transpose(0, 2, 1, 3)).reshape(64 * 128, 2048)
    for nm in ("ln1_g", "ln1_b", "ln2_g", "ln2_b"):
        m[nm] = f(inputs[nm])
    return m


_PROG_CACHE = {}


def _get_prog(n_layers=DEPTH, stop_after_mixer=False):
    key = (n_layers, stop_after_mixer)
    if key not in _PROG_CACHE:
        _PROG_CACHE[key] = Prog(n_layers, stop_after_mixer).build()
    return _PROG_CACHE[key]


def kernel(**inputs):
    x = np.asarray(inputs["x"], dtype=np.float32)
    n = x.shape[0]
    shared = _prep_shared(inputs)
    nc = _get_prog()
    in_maps = []
    for b in range(n):
        m = dict(shared)
        m["x"] = np.ascontiguousarray(x[b])
        in_maps.append(m)
    res = run_bass_kernel_spmd(nc, in_maps, core_ids=list(range(n)))
    return np.stack([np.asarray(r["out"]) for r in res.results], axis=0).astype(np.float32)
```

```python
import numpy as np
from contextlib import ExitStack
import concourse.bass as bass
import concourse.mybir as mybir
from concourse.bass_utils import run_bass_kernel_spmd

F32 = mybir.dt.float32
BF16 = mybir.dt.bfloat16
I32 = mybir.dt.int32
AF = mybir.ActivationFunctionType
ALU = mybir.AluOpType
AX = mybir.AxisListType

SEQ = 2048
D = 1024
NT = 16
KC = 8
DEPTH = 4
NH = 16
NIH = 8
TOPK = 256
NBLK = 96
ALPHA = (2.0 * DEPTH) ** 0.25
LN_EPS = 1e-5
RMS_EPS = 1e-6
NEG = -1.0e30
MASK_NEG = -30000.0
VW = 383

COMPUTE = ("pe", "act", "dve", "pool")
ALL_STREAMS = ("pe", "act", "dve", "pool", "sp")


class Buf:
    __slots__ = ("name", "lw", "rd")

    def __init__(self, name=""):
        self.name = name
        self.lw = None
        self.rd = {}


class Sched:
    def __init__(self, nc, es, n_dma_sems=40):
        self.nc = nc
        self.streams = {e: [] for e in ALL_STREAMS}
        self.count = {e: 0 for e in COMPUTE}
        self.waited = {e: {} for e in ALL_STREAMS}
        self.n_dma_sems = n_dma_sems
        self.dma_val = [0] * n_dma_sems
        self.dma_rr = 0
        self.sems = {}
        for e in COMPUTE:
            self.sems[e] = es.enter_context(nc.semaphore("sem_" + e))
        for s in range(n_dma_sems):
            self.sems[("dma", s)] = es.enter_context(nc.semaphore("semd%d" % s))
        self.n_ops = 0

    def _need(self, eng, deps):
        st = self.streams[eng]
        w = self.waited[eng]
        for key, val in deps.items():
            if key == eng:
                if eng == "pe" or self.count[eng] - val >= 3:
                    continue
            if w.get(key, 0) >= val:
                continue
            w[key] = val
            st.append(("wait", key, val))

    def _collect(self, reads, writes):
        deps = {}
        for b in reads:
            if b.lw is not None and deps.get(b.lw[0], 0) < b.lw[1]:
                deps[b.lw[0]] = b.lw[1]
        for b in writes:
            if b.lw is not None and deps.get(b.lw[0], 0) < b.lw[1]:
                deps[b.lw[0]] = b.lw[1]
            for k, v in b.rd.items():
                if deps.get(k, 0) < v:
                    deps[k] = v
        return deps

    def _commit(self, tok, reads, writes):
        k, v = tok
        for b in writes:
            b.lw = tok
            b.rd = {}
        for b in reads:
            if b.rd.get(k, 0) < v:
                b.rd[k] = v

    def op(self, eng, fn, reads=(), writes=()):
        deps = self._collect(reads, writes)
        self._need(eng, deps)
        self.count[eng] += 1
        tok = (eng, self.count[eng])
        self.streams[eng].append(("op", fn, eng))
        self._commit(tok, reads, writes)
        self.n_ops += 1
        return tok

    def dma(self, q, fn, reads=(), writes=()):
        deps = self._collect(reads, writes)
        s = self.dma_rr
        self.dma_rr = (self.dma_rr + 1) % self.n_dma_sems
        key = ("dma", s)
        if self.dma_val[s] > 0:
            deps[key] = max(deps.get(key, 0), self.dma_val[s])
        self._need(q, deps)
        self.dma_val[s] += 16
        tok = (key, self.dma_val[s])
        self.streams[q].append(("dma", fn, key))
        self._commit(tok, reads, writes)
        self.n_ops += 1
        return tok

    def flush(self):
        deps = {("dma", s): self.dma_val[s] for s in range(self.n_dma_sems) if self.dma_val[s] > 0}
        self._need("sp", deps)
        for e in ALL_STREAMS:
            self._need(e, {f: self.count[f] for f in COMPUTE if f != e and self.count[f] > 0})
        sems = self.sems
        streams = self.streams

        def run(stream):
            def body(e):
                for it in stream:
                    if it[0] == "wait":
                        e.wait_ge(sems[it[1]], it[2])
                    elif it[0] == "raw":
                        it[1](e)
                    elif it[0] == "op":
                        it[1](e).then_inc(sems[it[2]], 1)
                    else:
                        it[1](e).then_inc(sems[it[2]], 16)
            return body
        with self.nc.Block() as block:
            block.tensor(run(streams["pe"]))
            block.scalar(run(streams["act"]))
            block.vector(run(streams["dve"]))
            block.gpsimd(run(streams["pool"]))
            block.sync(run(streams["sp"]))
        self.streams = {e: [] for e in ALL_STREAMS}
        for e in ALL_STREAMS:
            for f in COMPUTE:
                self.waited[e][f] = self.count[f]
            for s in range(self.n_dma_sems):
                self.waited[e][("dma", s)] = self.dma_val[s]


class Prog:
    def __init__(self, n_layers=DEPTH, stop_after_mixer=False):
        self.n_layers = n_layers
        self.stop_after_mixer = stop_after_mixer
        self.nc = bass.Bass("TRN2", target_bir_lowering=False)

    def MM(self, out, lhsT, rhs, start, stop, r, w):
        self.S.op("pe", lambda e: e.matmul(out, lhsT=lhsT, rhs=rhs, start=start, stop=stop), r, w)

    def TR(self, out, in_, ident, r, w):
        self.S.op("pe", lambda e: e.transpose(out=out, in_=in_, identity=ident), r, w)

    def ACT(self, out, in_, func, r, w, scale=None, bias=None, accum=None):
        kw = {}
        if scale is not None:
            kw["scale"] = scale
        if bias is not None:
            kw["bias"] = bias
        if accum is not None:
            kw["accum_out"] = accum
        self.S.op("act", lambda e: e.activation(out=out, in_=in_, func=func, **kw), r, w)

    def TS(self, eng, out, in0, s1, op0, r, w, s2=None, op1=None, accum=None):
        kw = {}
        if op1 is not None:
            kw["op1"] = op1
        if accum is not None:
            kw["accum_out"] = accum
        self.S.op(eng, lambda e: e.tensor_scalar(out=out, in0=in0, scalar1=s1, scalar2=s2, op0=op0, **kw), r, w)

    def TT(self, eng, out, in0, in1, op, r, w):
        self.S.op(eng, lambda e: e.tensor_tensor(out=out, in0=in0, in1=in1, op=op), r, w)

    def STT(self, eng, out, in0, scalar, in1, op0, op1, r, w):
        self.S.op(eng, lambda e: e.scalar_tensor_tensor(out=out, in0=in0, scalar=scalar, in1=in1, op0=op0, op1=op1), r, w)

    def CP(self, eng, out, in_, r, w):
        if eng == "act":
            self.S.op("act", lambda e: e.copy(out=out, in_=in_), r, w)
        else:
            self.S.op(eng, lambda e: e.tensor_copy(out=out, in_=in_), r, w)

    def RED(self, out, in_, op, r, w, axis=AX.X):
        self.S.op("dve", lambda e: e.tensor_reduce(out=out, in_=in_, axis=axis, op=op), r, w)

    def MEMSET(self, eng, ap, val, w):
        self.S.op(eng, lambda e: e.memset(ap, val), (), w)

    def DMA(self, q, out, in_, r, w, **kw):
        self.S.dma(q, lambda e: e.dma_start(out=out, in_=in_, **kw), r, w)

    def GATHER(self, out, table, idx, r, w, bound=None):
        if bound is None:
            self.S.dma("pool", lambda e: e.indirect_dma_start(
                out=out, out_offset=None, in_=table, in_offset=bass.IndirectOffsetOnAxis(ap=idx, axis=0)), r, w)
        else:
            self.S.dma("pool", lambda e: e.indirect_dma_start(
                out=out, out_offset=None, in_=table, in_offset=bass.IndirectOffsetOnAxis(ap=idx, axis=0),
                bounds_check=self.bound_reg, oob_is_err=False), r, w)

    def SCATTER(self, table, idx, in_, r, w):
        self.S.dma("pool", lambda e: e.indirect_dma_start(
            out=table, out_offset=bass.IndirectOffsetOnAxis(ap=idx, axis=0), in_=in_, in_offset=None), r, w)

    def sb(self, es, name, shape, dt):
        self.uid = getattr(self, "uid", 0) + 1
        return es.enter_context(self.nc.sbuf_tensor("%s_u%d" % (name, self.uid), shape, dt))

    def ps(self, es, name, shape, dt):
        self.uid = getattr(self, "uid", 0) + 1
        return es.enter_context(self.nc.psum_tensor("%s_u%d" % (name, self.uid), shape, dt))

    def declare_dram(self):
        nc = self.nc

        def inp(name, shape, dt=F32):
            return nc.dram_tensor(name, shape, dt, kind="ExternalInput").ap()

        def internal(name, shape, dt):
            return nc.dram_tensor(name, shape, dt, kind="Internal").ap()
        d = {}
        d["x"] = inp("x", [SEQ, D])
        for j in range(2):
            d["cwin%d" % j] = inp("cwin%d" % j, [D, 3 * D])
            d["cwout%d" % j] = inp("cwout%d" % j, [D, D])
            d["awin%d" % j] = inp("awin%d" % j, [D, 2760])
            d["awout%d" % j] = inp("awout%d" % j, [2048, D])
        d["conv_k"] = inp("conv_k", [2, 3, D])
        d["kv_g"] = inp("kv_g", [2, 128])
        d["ki_g"] = inp("ki_g", [2, 64])
        d["ki_b"] = inp("ki_b", [2, 64])
        d["rel_bias"] = inp("rel_bias", [32, 16])
        d["ohs"] = inp("ohs", [32, VW])
        d["rw"] = inp("rw", [DEPTH, D, 72])
        d["rb"] = inp("rb", [DEPTH, 72])
        for l in range(DEPTH):
            d["w1r%d" % l] = inp("w1r%d" % l, [64 * 128, 2048])
            d["w3r%d" % l] = inp("w3r%d" % l, [64 * 128, 2048])
            d["w2r%d" % l] = inp("w2r%d" % l, [64 * 128, 2048])
        d["ln1_g"] = inp("ln1_g", [DEPTH, D])
        d["ln1_b"] = inp("ln1_b", [DEPTH, D])
        d["ln2_g"] = inp("ln2_g", [DEPTH, D])
        d["ln2_b"] = inp("ln2_b", [DEPTH, D])
        d["out"] = nc.dram_tensor("out", [SEQ, D], F32, kind="ExternalOutput").ap()
        d["xres"] = internal("xres_d", [SEQ, D], F32)
        d["xs"] = internal("xs_d", [NBLK * 128, D], BF16)
        d["ys"] = internal("ys_d", [NBLK * 128, D], F32)
        d["maskT"] = internal("maskT_d", [SEQ, SEQ], BF16)
        d["t5"] = internal("t5_d", [16 * 128 * (VW + 1)], F32)
        d["qT"] = internal("qT_d", [NH, 128, SEQ], BF16)
        self.d = d

    @staticmethod
    def bcast_rows(vec_ap, n):
        return bass.AP(tensor=vec_ap.tensor, offset=vec_ap.offset, ap=[[0, 128], [1, n]])

    def build(self):
        nc = self.nc
        self.declare_dram()
        with ExitStack() as es:
            self.S = Sched(nc, es)
            S = self.S
            self.xa = self.sb(es, "xa", [128, 16384], BF16)
            self.xT = self.xa[:, :].rearrange("p (k t) -> p k t", k=KC)
            self.xb = self.xa[:, :].rearrange("p (i d) -> p i d", i=NT)
            self.BxT = [Buf("xT%d" % i) for i in range(NT)]
            self.Bxb = [Buf("xb%d" % i) for i in range(NT)]
            self.lg = self.sb(es, "lg", [128, NT, 72], F32)
            self.Blg = Buf("lg")
            self.ident_f = self.sb(es, "ident_f", [128, 128], F32)
            self.ident_b = self.sb(es, "ident_b", [128, 128], BF16)
            self.ones_b = self.sb(es, "ones_b", [128, 128], BF16)
            self.tri_b = self.sb(es, "tri_b", [128, 128], BF16)
            self.ones_row = self.sb(es, "ones_row", [1, 128], F32)
            self.eps_ln = self.sb(es, "eps_ln", [128, 1], F32)
            self.eps_rms = self.sb(es, "eps_rms", [128, 1], F32)
            self.pidx = self.sb(es, "pidx", [128, 1], F32)
            self.bvals = self.sb(es, "bvals", [128, NBLK], F32)
            self.BtT = self.sb(es, "BtT", [128, NH, 2, 128], BF16)
            self.Bconst = Buf("const")
            self.phase_init()
            self.phase_load_x()
            import os
            skip0 = int(os.environ.get("SKIP0", "0"))
            for l in range(self.n_layers):
                if l < skip0:
                    continue
                last_mixer = self.stop_after_mixer and l == self.n_layers - 1
                if l % 2 == 0:
                    self.phase_conv(l, to_out=last_mixer)
                else:
                    self.phase_attn(l, to_out=last_mixer)
                if last_mixer:
                    break
                self.phase_moe(l, to_out=(l == self.n_layers - 1))
        return nc

    def phase_init(self):
        S = self.S
        Bc = self.Bconst
        with ExitStack() as es:
            tmpf = self.sb(es, "init_tmpf", [128, 128], F32)
            tmpi = self.sb(es, "init_tmpi", [128, NBLK], I32)
            Bt = Buf()
            Bi = Buf()
            self.MEMSET("pool", self.ident_f[:], 1.0, [Bc])
            S.op("pool", lambda e: e.affine_select(out=self.ident_f[:], in_=self.ident_f[:], pattern=[[-1, 128]],
                                                   compare_op=ALU.is_equal, fill=0.0, base=0, channel_multiplier=1),
                 [Bc], [Bc])
            self.CP("dve", self.ident_b[:], self.ident_f[:], [Bc], [Bc])
            self.MEMSET("dve", self.ones_b[:], 1.0, [Bc])
            self.MEMSET("pool", tmpf[:], 1.0, [Bt])
            S.op("pool", lambda e: e.affine_select(out=tmpf[:], in_=tmpf[:], pattern=[[1, 128]],
                                                   compare_op=ALU.is_ge, fill=0.0, base=-1, channel_multiplier=-1),
                 [Bt], [Bt])
            self.CP("dve", self.tri_b[:], tmpf[:], [Bt], [Bc])
            self.MEMSET("dve", self.ones_row[:], 1.0, [Bc])
            self.MEMSET("dve", self.eps_ln[:], LN_EPS, [Bc])
            self.MEMSET("dve", self.eps_rms[:], RMS_EPS, [Bc])
            S.op("pool", lambda e: e.iota(tmpi[:, 0:1], pattern=[[0, 1]], base=0, channel_multiplier=1), (), [Bi])
            self.CP("dve", self.pidx[:], tmpi[:, 0:1], [Bi], [Bc])
            S.op("pool", lambda e: e.iota(tmpi[:, :], pattern=[[128, NBLK]], base=0, channel_multiplier=0), [Bi], [Bi])
            self.CP("dve", self.bvals[:], tmpi[:, :], [Bi], [Bc])
            if self.n_layers >= 2:
                self.init_t5(es)
            S.flush()

    def init_t5(self, es):
        S = self.S
        d = self.d
        rel = self.sb(es, "t5_rel", [32, 16], F32)
        ohs = self.sb(es, "t5_ohs", [32, VW], F32)
        rhs = self.sb(es, "t5_rhs", [32, NH * VW], F32)
        ones32 = self.sb(es, "t5_ones", [32, 128], F32)
        vec = self.sb(es, "t5_vec", [128, NH, VW], F32)
        btf = self.sb(es, "t5_btf", [128, NH, 2, 128], F32)
        Br, Bo, Brhs, B1, Bv = Buf(), Buf(), Buf(), Buf(), Buf()
        pp = [self.ps(es, "t5_ps%d" % i, [128, 512], F32) for i in range(2)]
        Bpp = [Buf(), Buf()]
        self.DMA("sp", rel[:], d["rel_bias"], (), [Br])
        self.DMA("sp", ohs[:], d["ohs"], (), [Bo])
        self.MEMSET("dve", ones32[:], 1.0, [B1])
        for h in range(NH):
            self.TS("dve", rhs[:, h * VW:(h + 1) * VW], ohs[:], rel[:, h:h + 1], ALU.mult, [Br, Bo], [Brhs])
        tot = NH * VW
        vflat = vec[:, :, :].rearrange("p h v -> p (h v)")
        c = 0
        k = 0
        while c < tot:
            n = min(512, tot - c)
            self.MM(pp[k % 2][:, 0:n], ones32[:, :], rhs[:, c:c + n], True, True, [B1, Brhs], [Bpp[k % 2]])
            self.CP("act" if k % 2 else "dve", vflat[:, c:c + n], pp[k % 2][:, 0:n], [Bpp[k % 2]], [Bv])
            c += n
            k += 1
        t5 = d["t5"]
        dst = bass.AP(tensor=t5.tensor, offset=0, ap=[[VW + 1, 128], [128 * (VW + 1), NH], [1, VW]])
        Bd = Buf()
        self.DMA("sp", dst, vec[:, :, :], [Bv], [Bd])
        src = bass.AP(tensor=t5.tensor, offset=127, ap=[[VW, 128], [128 * (VW + 1), NH], [128, 2], [1, 128]])
        Bbt = Buf()
        self.DMA("sp", btf[:, :, :, :], src, [Bd], [Bbt])
        self.CP("dve", self.BtT[:, :, :, :], btf[:, :, :, :], [Bbt], [self.Bconst])

    def to_xT(self, src, Bsrc, i, pts, Bpts):
        for half in range(2):
            pt, Bp = pts[half], Bpts[half]
            for q in range(4):
                kc = half * 4 + q
                self.TR(pt[:, q * 128:(q + 1) * 128], src[:, kc * 128:(kc + 1) * 128], self.ident_f[:],
                        [Bsrc, self.Bconst], [Bp])
            dst = self.xT[:, half * 4:(half + 1) * 4, i * 128:(i + 1) * 128]
            self.CP("act" if half else "dve", dst, pt[:, :].rearrange("p (q t) -> p q t", q=4), [Bp],
                    [self.BxT[i]] + self.Bxb)

    def phase_load_x(self):
        S = self.S
        d = self.d
        with ExitStack() as es:
            xt = [self.sb(es, "lx_xt%d" % k, [128, D], F32) for k in range(2)]
            Bx = [Buf(), Buf()]
            pts = [self.ps(es, "lx_pt%d" % k, [128, 512], F32) for k in range(4)]
            Bp = [Buf() for _ in range(4)]
            Bxr = Buf()
            self.DMA("sp", d["xres"], d["x"], (), [Bxr])
            for i in range(NT):
                k = i % 2
                self.DMA("sp", xt[k][:], d["x"][i * 128:(i + 1) * 128, :], (), [Bx[k]])
                self.to_xT(xt[k], Bx[k], i, pts[2 * k:2 * k + 2], Bp[2 * k:2 * k + 2])
            S.flush()

    def ln_setup(self, es, tag, g_ap, b_ap, lidx, router, nbuf=3):
        c = {}
        c["g"] = self.sb(es, tag + "_g", [128, D], F32)
        c["b"] = self.sb(es, tag + "_b", [128, D], F32)
        c["Bgb"] = Buf()
        self.DMA("sp", c["g"][:], self.bcast_rows(g_ap, D), (), [c["Bgb"]])
        self.DMA("sp", c["b"][:], self.bcast_rows(b_ap, D), (), [c["Bgb"]])
        for nm in ("xr", "v", "xo"):
            c[nm] = [self.sb(es, "%s_%s%d" % (tag, nm, k), [128, D], F32) for k in range(nbuf)]
            c["B" + nm] = [Buf() for _ in range(nbuf)]
        c["nbuf"] = nbuf
        c["st"] = [self.sb(es, "%s_st%d" % (tag, k), [128, 16], F32) for k in range(nbuf)]
        c["Bst"] = [Buf() for _ in range(nbuf)]
        c["router"] = router
        if router:
            c["rw"] = self.sb(es, tag + "_rw", [128, KC, 72], F32)
            c["rb"] = self.sb(es, tag + "_rbias", [1, 72], F32)
            c["Brw"] = Buf()
            self.DMA("sp", c["rw"][:, :, :], self.d["rw"][lidx].rearrange("(k p) c -> p k c", p=128), (), [c["Brw"]])
            self.DMA("sp", c["rb"][:, :], self.d["rb"][lidx:lidx + 1, :], (), [c["Brw"]])
            c["x32"] = [self.sb(es, "%s_x32%d" % (tag, k), [128, KC, 128], F32) for k in range(2)]
            c["Bx32"] = [Buf(), Buf()]
        return c

    def _ln_bufs(self, c, i):
        k3 = i % c["nbuf"]
        return (c["xr"][k3], c["v"][k3], c["xo"][k3], c["st"][k3], c["Bxr"][k3], c["Bv"][k3], c["Bxo"][k3], c["Bst"][k3])

    def ln_A(self, c, i, y_halves, y_bufs):
        d = self.d
        xr, v, xo, st, Bxr, Bv, Bxo, Bst = self._ln_bufs(c, i)
        self.DMA("sp", xr[:], d["xres"][i * 128:(i + 1) * 128, :], (), [Bxr])
        for h in range(2):
            self.STT("dve", v[:, h * 512:(h + 1) * 512], xr[:, h * 512:(h + 1) * 512], ALPHA, y_halves[h],
                     ALU.mult, ALU.add, [Bxr, y_bufs[h]], [Bv])
        for h in range(2):
            self.S.op("dve", (lambda hh, st=st, v=v: (lambda e: e.bn_stats(out=st[:, hh * 6:(hh + 1) * 6],
                                                                          in_=v[:, hh * 512:(hh + 1) * 512])))(h), [Bv], [Bst])
        self.S.op("dve", (lambda st: (lambda e: e.bn_aggr(out=st[:, 12:14], in_=st[:, 0:12])))(st), [Bst], [Bst])
        self.ACT(st[:, 14:15], st[:, 13:14], AF.Sqrt, [Bst, self.Bconst], [Bst], bias=self.eps_ln[:, 0:1], scale=1.0)
        self.S.op("dve", (lambda st: (lambda e: e.reciprocal(out=st[:, 14:15], in_=st[:, 14:15])))(st), [Bst], [Bst])
        self.STT("dve", st[:, 15:16], st[:, 12:13], -1.0, st[:, 14:15], ALU.mult, ALU.mult, [Bst], [Bst])

    def ln_B(self, c, i, dst_dram):
        xr, v, xo, st, Bxr, Bv, Bxo, Bst = self._ln_bufs(c, i)
        self.ACT(v[:, :], v[:, :], AF.Identity, [Bv, Bst], [Bv], scale=st[:, 14:15], bias=st[:, 15:16])
        self.TT("dve", v[:, :], v[:, :], c["g"][:, :], ALU.mult, [Bv, c["Bgb"]], [Bv])
        self.TT("dve", xo[:, :], v[:, :], c["b"][:, :], ALU.add, [Bv, c["Bgb"]], [Bxo])
        self.DMA("sp", dst_dram[i * 128:(i + 1) * 128, :], xo[:], [Bxo], ())
        if c["router"]:
            self.CP("act", self.xb[:, i, :], xo[:, :], [Bxo], [self.Bxb[i]] + self.BxT)

    def ln_C(self, c, i, pts, Bpts, prl, Bprl):
        xr, v, xo, st, Bxr, Bv, Bxo, Bst = self._ln_bufs(c, i)
        k = i % 2
        if not c["router"]:
            self.to_xT(xo, Bxo, i, pts, Bpts)
        else:
            x32, Bx32 = c["x32"][k], c["Bx32"][k]
            for half in range(2):
                pt, Bp = pts[half], Bpts[half]
                for q in range(4):
                    kc = half * 4 + q
                    self.TR(pt[:, q * 128:(q + 1) * 128], xo[:, kc * 128:(kc + 1) * 128], self.ident_f[:],
                            [Bxo, self.Bconst], [Bp])
                self.CP("act" if half else "dve", x32[:, half * 4:(half + 1) * 4, :],
                        pt[:, :].rearrange("p (q t) -> p q t", q=4), [Bp], [Bx32])

    def ln_D(self, c, i, prl, Bprl):
        if not c["router"]:
            return
        k = i % 2
        x32, Bx32 = c["x32"][k], c["Bx32"][k]
        for kc in range(KC):
            self.MM(prl[:, 0:72], x32[:, kc, :], c["rw"][:, kc, :], kc == 0, False, [Bx32, c["Brw"]], [Bprl])
        self.MM(prl[:, 0:72], self.ones_row[0:1, :], c["rb"][0:1, :], False, True, [self.Bconst, c["Brw"]], [Bprl])
        self.CP("act", self.lg[:, i, :], prl[:, 0:72], [Bprl], [self.Blg])

    def ln_run(self, c, tiles, yfn, dst, ptsfn):
        n = len(tiles)
        for s in range(n + 3):
            if s < n:
                yh, yb = yfn(tiles[s])
                self.ln_A(c, tiles[s], yh, yb)
            if 0 <= s - 1 < n:
                self.ln_B(c, tiles[s - 1], dst)
            if 0 <= s - 2 < n:
                pts, Bpts, prl, Bprl = ptsfn(tiles[s - 2])
                self.ln_C(c, tiles[s - 2], pts, Bpts, prl, Bprl)
            if 0 <= s - 3 < n:
                pts, Bpts, prl, Bprl = ptsfn(tiles[s - 3])
                self.ln_D(c, tiles[s - 3], prl, Bprl)

    def phase_conv(self, l, to_out):
        S = self.S
        d = self.d
        j = l // 2
        win = d["cwin%d" % j].rearrange("(k p) f -> p k f", p=128)
        with ExitStack() as es:
            c = self.ln_setup(es, "cv", d["ln1_g"][l], d["ln1_b"][l], l, router=not to_out)
            wb = [self.sb(es, "cv_w%d" % k, [128, 3, KC, 128], BF16) for k in range(2)]
            Bw = [Buf(), Buf()]
            wout = self.sb(es, "cv_wout", [128, KC, D], BF16)
            Bwout = Buf()
            zT = self.sb(es, "cv_zT", [128, KC, SEQ], BF16)
            Bz = [Buf() for _ in range(KC)]
            ub = self.sb(es, "cv_u", [128, SEQ + 2], F32)
            Bu = Buf()
            cs = [self.sb(es, "cv_cs%d" % k, [128, 512], F32) for k in range(2)]
            Bcs = [Buf(), Buf()]
            cvt = [self.sb(es, "cv_cv%d" % k, [128, 512], F32) for k in range(2)]
            Bcv = [Buf(), Buf()]
            ck = self.sb(es, "cv_ck", [128, 3, KC], F32)
            Bck = Buf()
            pp = [self.ps(es, "cv_ps%d" % k, [128, 512], F32) for k in range(8)]
            Bpp = [Buf() for _ in range(8)]
            self.DMA("sp", ck[:, :, :], d["conv_k"][j].rearrange("j (f p) -> p j f", p=128), (), [Bck],
                     allow_slow_non_contiguous=True)
            self.DMA("pool", wout[:, :, :], d["cwout%d" % j].rearrange("(k p) f -> p k f", p=128), (), [Bwout])
            self.MEMSET("dve", ub[:, 0:2], 0.0, [Bu])
            for f in range(KC):
                k = f % 2
                for g in range(3):
                    self.DMA("pool", wb[k][:, g, :, :], win[:, :, g * D + f * 128: g * D + (f + 1) * 128], (), [Bw[k]])
                for tc in range(4):
                    base = (tc % 2) * 3
                    pB, pC, pH = pp[base], pp[base + 1], pp[base + 2]
                    BpB, BpC, BpH = Bpp[base], Bpp[base + 1], Bpp[base + 2]
                    rT = [self.BxT[4 * tc + q] for q in range(4)]
                    for g, (po, Bpo) in enumerate(((pB, BpB), (pC, BpC), (pH, BpH))):
                        for kc in range(KC):
                            self.MM(po[:, :], wb[k][:, g, kc, :], self.xT[:, kc, tc * 512:(tc + 1) * 512],
                                    kc == 0, kc == KC - 1, [Bw[k]] + rT, [Bpo])
                    kk = tc % 2
                    self.CP("act", cs[kk][:, :], pC[:, :], [BpC], [Bcs[kk]])
                    self.TT("dve", ub[:, 2 + tc * 512: 2 + (tc + 1) * 512], pH[:, :], cs[kk][:, :], ALU.mult,
                            [BpH, Bcs[kk]], [Bu])
                    o = tc * 512
                    self.TS("dve", cvt[kk][:, :], ub[:, o:o + 512], ck[:, 0, f:f + 1], ALU.mult, [Bu, Bck], [Bcv[kk]])
                    self.STT("dve", cvt[kk][:, :], ub[:, o + 1:o + 513], ck[:, 1, f:f + 1], cvt[kk][:, :],
                             ALU.mult, ALU.add, [Bu, Bck, Bcv[kk]], [Bcv[kk]])
                    self.STT("dve", cvt[kk][:, :], ub[:, o + 2:o + 514], ck[:, 2, f:f + 1], cvt[kk][:, :],
                             ALU.mult, ALU.add, [Bu, Bck, Bcv[kk]], [Bcv[kk]])
                    self.TT("dve", zT[:, f, o:o + 512], pB[:, :], cvt[kk][:, :], ALU.mult, [BpB, Bcv[kk]], [Bz[f]])
            dst = d["out"] if to_out else d["xres"]
            py = [pp[6], pp[7]]
            Bpy = [Bpp[6], Bpp[7]]

            def yfn(i):
                for h in range(2):
                    for kc in range(KC):
                        self.MM(py[h][:, :], zT[:, kc, i * 128:(i + 1) * 128], wout[:, kc, h * 512:(h + 1) * 512],
                                kc == 0, kc == KC - 1, [Bz[kc], Bwout], [Bpy[h]])
                return [py[0][:, :], py[1][:, :]], Bpy

            def ptsfn(i):
                k = i % 2
                return [pp[0 + 2 * k], pp[1 + 2 * k]], [Bpp[0 + 2 * k], Bpp[1 + 2 * k]], pp[4 + k], Bpp[4 + k]
            self.ln_run(c, list(range(NT)), yfn, dst, ptsfn)
            S.flush()

    def phase_moe(self, l, to_out):
        S = self.S
        d = self.d
        with ExitStack() as es:
            cur_es = [es]
            sm = lambda name, shape, dt=F32: self.sb(cur_es[0], "mo_" + name, shape, dt)
            gates = sm("gates", [128, NT, 2]); desti = sm("desti", [128, NT, 2], I32); widx = sm("widx", [128, NBLK], I32)
            esA = ExitStack()
            cur_es[0] = esA
            lg = self.lg
            Blg = self.Blg
            lgg = lg[:, :, 0:8]
            lge = lg[:, :, 8:72].rearrange("p i (g e) -> p i g e", g=8)
            gmax = sm("gmax", [128, NT]); goh = sm("goh", [128, NT, 8]); gsh = sm("gsh", [128, NT, 8])
            gsum = sm("gsum", [128, NT]); ggate = sm("ggate", [128, NT])
            tmp4 = sm("tmp4", [128, NT, 8, 8]); sel = sm("sel", [128, NT, 8]); sel2 = sm("sel2", [128, NT, 8])
            v1 = sm("v1", [128, NT]); v2 = sm("v2", [128, NT]); oh1 = sm("oh1", [128, NT, 8]); oh2 = sm("oh2", [128, NT, 8])
            e2 = sm("e2", [128, NT]); p1 = sm("p1", [128, NT])
            A1 = sm("A1", [128, NT, 64]); A2 = sm("A2", [128, NT, 64]); Ab = sm("Ab", [128, NT, 64], BF16)
            B = Buf("route")
            bc8 = lambda ap: ap.unsqueeze(2).to_broadcast([128, NT, 8])
            self.RED(gmax[:, :], lgg, ALU.max, [Blg], [B])
            self.TT("dve", goh[:, :, :], lgg, bc8(gmax[:, :]), ALU.is_equal, [Blg, B], [B])
            self.TT("dve", gsh[:, :, :], lgg, bc8(gmax[:, :]), ALU.subtract, [Blg, B], [B])
            self.ACT(gsh[:, :, :], gsh[:, :, :], AF.Exp, [B], [B])
            self.RED(gsum[:, :], gsh[:, :, :], ALU.add, [B], [B])
            S.op("dve", lambda e: e.reciprocal(out=ggate[:, :], in_=gsum[:, :]), [B], [B])
            self.TT("dve", tmp4[:, :, :, :], lge, goh[:, :, :].unsqueeze(3).to_broadcast([128, NT, 8, 8]), ALU.mult,
                    [Blg, B], [B])
            self.RED(sel[:, :, :], tmp4[:, :, :, :].rearrange("p i g e -> p i e g"), ALU.add, [B], [B])
            self.RED(v1[:, :], sel[:, :, :], ALU.max, [B], [B])
            self.TT("dve", oh1[:, :, :], sel[:, :, :], bc8(v1[:, :]), ALU.is_equal, [B], [B])
            self.STT("dve", sel2[:, :, :], oh1[:, :, :], NEG, sel[:, :, :], ALU.mult, ALU.add, [B], [B])
            self.RED(v2[:, :], sel2[:, :, :], ALU.max, [B], [B])
            self.TT("dve", oh2[:, :, :], sel2[:, :, :], bc8(v2[:, :]), ALU.is_equal, [B], [B])
            self.TT("dve", e2[:, :], v2[:, :], v1[:, :], ALU.subtract, [B], [B])
            self.ACT(e2[:, :], e2[:, :], AF.Exp, [B], [B])
            self.TS("dve", p1[:, :], e2[:, :], 1.0, ALU.add, [B], [B])
            S.op("dve", lambda e: e.reciprocal(out=p1[:, :], in_=p1[:, :]), [B], [B])
            self.TT("dve", gates[:, :, 0], p1[:, :], ggate[:, :], ALU.mult, [B], [B])
            self.TT("dve", e2[:, :], e2[:, :], p1[:, :], ALU.mult, [B], [B])
            self.TT("dve", gates[:, :, 1], e2[:, :], ggate[:, :], ALU.mult, [B], [B])
            gb = goh[:, :, :].unsqueeze(3).to_broadcast([128, NT, 8, 8])
            for Ak, ohk in ((A1, oh1), (A2, oh2)):
                self.TT("dve", Ak[:, :, :].rearrange("p i (g e) -> p i g e", g=8), gb,
                        ohk[:, :, :].unsqueeze(2).to_broadcast([128, NT, 8, 8]), ALU.mult, [B], [B])
            self.TT("dve", Ab[:, :, :], A1[:, :, :], A2[:, :, :], ALU.add, [B], [B])
            pp = [self.ps(es, "mo_pp%d" % k, [128, 512], F32) for k in range(6)]
            Bpp = [Buf() for _ in range(6)]
            pcs, ppf = pp[0:2], pp[2:4]
            Bpcs, Bppf = Bpp[0:2], Bpp[2:4]
            Abf = Ab[:, :, :].rearrange("p i e -> p (i e)")
            for h in range(2):
                self.MM(pcs[h][:, :], self.ones_b[:, :], Abf[:, h * 512:(h + 1) * 512], True, True, [B, self.Bconst], [Bpcs[h]])
                self.MM(ppf[h][:, :], self.tri_b[:, :], Abf[:, h * 512:(h + 1) * 512], True, True, [B, self.Bconst], [Bppf[h]])
            cs = sm("cs", [128, NT, 64]); base = sm("base", [128, NT, 64]); carry = sm("carry", [128, NT, 64])
            csf = cs[:, :, :].rearrange("p i e -> p (i e)")
            bsf = base[:, :, :].rearrange("p i e -> p (i e)")
            for h in range(2):
                self.CP("dve", csf[:, h * 512:(h + 1) * 512], pcs[h][:, :], [Bpcs[h]], [B])
                self.CP("act", bsf[:, h * 512:(h + 1) * 512], ppf[h][:, :], [Bppf[h]], [B])
            self.MEMSET("dve", carry[:, 0, :], 0.0, [B])
            for i in range(1, NT):
                self.TT("dve", carry[:, i, :], carry[:, i - 1, :], cs[:, i - 1, :], ALU.add, [B], [B])
            cnt = sm("cnt", [128, 64]); cnti = sm("cnti", [128, 64], I32); padf = sm("padf", [128, 64])
            sc = [sm("scan%d" % k, [128, 64]) for k in range(2)]
            self.TT("dve", cnt[:, :], carry[:, NT - 1, :], cs[:, NT - 1, :], ALU.add, [B], [B])
            self.TS("dve", cnti[:, :], cnt[:, :], 127.0, ALU.add, [B], [B])
            self.TS("dve", cnti[:, :], cnti[:, :], 7, ALU.arith_shift_right, [B], [B])
            self.TS("dve", cnti[:, :], cnti[:, :], 7, ALU.logical_shift_left, [B], [B])
            self.CP("dve", padf[:, :], cnti[:, :], [B], [B])
            self.CP("dve", sc[0][:, :], padf[:, :], [B], [B])
            cur = 0
            s = 1
            while s < 64:
                a, b = sc[cur], sc[1 - cur]
                self.CP("dve", b[:, 0:s], a[:, 0:s], [B], [B])
                self.TT("dve", b[:, s:64], a[:, s:64], a[:, 0:64 - s], ALU.add, [B], [B])
                cur = 1 - cur
                s *= 2
            pend = sc[cur]
            pstart = sm("pstart", [128, 64])
            self.TT("dve", pstart[:, :], pend[:, :], padf[:, :], ALU.subtract, [B], [B])
            self.TT("dve", base[:, :, :], base[:, :, :], carry[:, :, :], ALU.add, [B], [B])
            self.TT("dve", base[:, :, :], base[:, :, :], pstart[:, :].unsqueeze(1).to_broadcast([128, NT, 64]), ALU.add,
                    [B], [B])
            destf = sm("destf", [128, NT, 2])
            for kk, Ak in enumerate((A1, A2)):
                self.TT("dve", cs[:, :, :], Ak[:, :, :], base[:, :, :], ALU.mult, [B], [B])
                self.RED(destf[:, :, kk], cs[:, :, :], ALU.add, [B], [B])
            Bdest = Buf("dest")
            self.CP("dve", desti[:, :, :], destf[:, :, :], [B], [Bdest])
            cmp = sm("cmp", [128, NBLK, 64], BF16); blke = sm("blke", [128, NBLK])
            self.TT("dve", cmp[:, :, :], pend[:, :].unsqueeze(1).to_broadcast([128, NBLK, 64]),
                    self.bvals[:, :].unsqueeze(2).to_broadcast([128, NBLK, 64]), ALU.is_le, [B, self.Bconst], [B])
            self.RED(blke[:, :], cmp[:, :, :], ALU.add, [B], [B])
            self.TS("dve", blke[:, :], blke[:, :], 128.0, ALU.mult, [B], [B])
            Bwidx = Buf("widx")
            self.TS("dve", widx[:, :], blke[:, :], self.pidx[:, 0:1], ALU.add, [B, self.Bconst], [Bwidx])
            Bxs = Buf("xs")
            for i in range(NT):
                for kk in range(2):
                    self.SCATTER(d["xs"], desti[:, i, kk:kk + 1], self.xb[:, i, :], [Bdest, self.Bxb[i]], [Bxs])
            S.flush()
            esA.close()
            esB = ExitStack()
            cur_es[0] = esB
            NB = 5
            xsb = [sm("xsb%d" % k, [128, D], BF16) for k in range(5)]; Bxsb = [Buf() for _ in range(5)]
            xsT = [sm("xsT%d" % k, [128, KC, 128], BF16) for k in range(2)]; BxsT = [Buf(), Buf()]
            w1b = [sm("w1b%d" % k, [128, KC, 256], BF16) for k in range(NB)]; Bw1 = [Buf() for _ in range(NB)]
            w3b = [sm("w3b%d" % k, [128, KC, 256], BF16) for k in range(NB)]; Bw3 = [Buf() for _ in range(NB)]
            w2b = [sm("w2b%d" % k, [128, 2, D], BF16) for k in range(NB)]; Bw2 = [Buf() for _ in range(NB)]
            sa = [sm("sa%d" % k, [128, 256]) for k in range(2)]; Bsa = [Buf(), Buf()]
            hT = [sm("hT%d" % k, [128, 2, 128], BF16) for k in range(2)]; BhT = [Buf(), Buf()]
            ysb = [sm("ysb%d" % k, [128, D]) for k in range(2)]; Bysb = [Buf(), Buf()]
            ptr = [self.ps(es, "mo_ptr%d" % k, [128, 1024], BF16) for k in range(2)]; Bptr = [Buf(), Buf()]
            ph = pp[0:2]; Bph = Bpp[0:2]
            pys = pp[2:4]; Bpys = Bpp[2:4]
            Bys = Buf("ys")
            w1t, w3t, w2t = d["w1r%d" % l], d["w3r%d" % l], d["w2r%d" % l]
            PF = 4
            S.streams["pool"].append(("raw", lambda e: setattr(self, "bound_reg", e.to_reg(64 * 128 - 1))))

            def issue_loads(b):
                kx = b % 5
                kw = b % NB
                self.DMA("act", xsb[kx][:, :], d["xs"][b * 128:(b + 1) * 128, :], [Bxs], [Bxsb[kx]])
                self.GATHER(w1b[kw][:, :, :].rearrange("p k f -> p (k f)"), w1t, widx[:, b:b + 1], [Bwidx], [Bw1[kw]], bound=64 * 128 - 1)
                self.GATHER(w3b[kw][:, :, :].rearrange("p k f -> p (k f)"), w3t, widx[:, b:b + 1], [Bwidx], [Bw3[kw]], bound=64 * 128 - 1)
                self.GATHER(w2b[kw][:, :, :].rearrange("p k f -> p (k f)"), w2t, widx[:, b:b + 1], [Bwidx], [Bw2[kw]], bound=64 * 128 - 1)
            for b in range(PF):
                issue_loads(b)
            for b in range(NBLK):
                k = b % 2
                kw = b % NB
                kx = b % 5
                if b + PF < NBLK:
                    issue_loads(b + PF)
                for kc in range(KC):
                    self.TR(ptr[k][:, kc * 128:(kc + 1) * 128], xsb[kx][:, kc * 128:(kc + 1) * 128], self.ident_b[:],
                            [Bxsb[kx], self.Bconst], [Bptr[k]])
                self.CP("dve", xsT[k][:, :, :].rearrange("p k r -> p (k r)"), ptr[k][:, :], [Bptr[k]], [BxsT[k]])
                for gi, (wt, Bwt) in enumerate(((w1b[kw], Bw1[kw]), (w3b[kw], Bw3[kw]))):
                    for fc in range(2):
                        col = (gi * 2 + fc) * 128
                        for kc in range(KC):
                            self.MM(ph[k][:, col:col + 128], wt[:, kc, fc * 128:(fc + 1) * 128], xsT[k][:, kc, :],
                                    kc == 0, kc == KC - 1, [Bwt, BxsT[k]], [Bph[k]])
                self.ACT(sa[k][:, :], ph[k][:, 0:256], AF.Silu, [Bph[k]], [Bsa[k]])
                self.TT("dve", hT[k][:, :, :].rearrange("p c r -> p (c r)"), sa[k][:, :], ph[k][:, 256:512], ALU.mult,
                        [Bsa[k], Bph[k]], [BhT[k]])
                for h in range(2):
                    py = pys[h]
                    for fc in range(2):
                        self.MM(py[:, :], hT[k][:, fc, :], w2b[kw][:, fc, h * 512:(h + 1) * 512], fc == 0, fc == 1,
                                [BhT[k], Bw2[kw]], [Bpys[h]])
                    self.CP("act" if h == 0 else "dve", ysb[k][:, h * 512:(h + 1) * 512], py[:, :],
                            [Bpys[h]], [Bysb[k]])
                self.DMA("sp", d["ys"][b * 128:(b + 1) * 128, :], ysb[k][:, :], [Bysb[k]], [Bys])
            esC = ExitStack()
            cur_es[0] = esC
            c = self.ln_setup(esC, "m2", d["ln2_g"][l], d["ln2_b"][l], l, router=False)
            y0 = [sm("y0_%d" % k, [128, D]) for k in range(2)]; By0 = [Buf(), Buf()]
            y1 = [sm("y1_%d" % k, [128, D]) for k in range(2)]; By1 = [Buf(), Buf()]
            dst = d["out"] if to_out else d["xres"]
            def issue_gather(i):
                k = i % 2
                self.GATHER(y0[k][:, :], d["ys"], desti[:, i, 0:1], [Bdest, Bys], [By0[k]])
                self.GATHER(y1[k][:, :], d["ys"], desti[:, i, 1:2], [Bdest, Bys], [By1[k]])
            issue_gather(0)

            def yfn(i):
                k = i % 2
                self.TS("dve", y0[k][:, :], y0[k][:, :], gates[:, i, 0:1], ALU.mult, [By0[k], B], [By0[k]])
                self.STT("dve", y0[k][:, :], y1[k][:, :], gates[:, i, 1:2], y0[k][:, :], ALU.mult, ALU.add,
                         [By1[k], By0[k], B], [By0[k]])
                if i + 1 < NT:
                    issue_gather(i + 1)
                return [y0[k][:, 0:512], y0[k][:, 512:1024]], [By0[k], By0[k]]

            def ptsfn(i):
                k = i % 2
                return [pp[2 * k], pp[2 * k + 1]], [Bpp[2 * k], Bpp[2 * k + 1]], None, None
            self.ln_run(c, list(range(NT)), yfn, dst, ptsfn)
            S.flush()
            esC.close()
            esB.close()

    def phase_attn(self, l, to_out):
        S = self.S
        d = self.d
        j = l // 2
        awin = d["awin%d" % j].rearrange("(k p) f -> p k f", p=128)
        import os
        att_stop = int(os.environ.get("ATT_STOP", "9"))
        if att_stop <= 0:
            return
        qd = d["qT"]
        with ExitStack() as es0:
            sm0 = lambda name, shape, dt=F32: self.sb(es0, "at_" + name, shape, dt)
            ckv_tm = sm0("ckv_tm", [128, NT, 128], BF16); Bckv = Buf("ckv_tm")
            ckvT = sm0("ckvT", [128, SEQ], BF16); BckvT = Buf("ckvT")
            with ExitStack() as es:
                sm = lambda name, shape, dt=F32: self.sb(es, "a1_" + name, shape, dt)
                pp = [self.ps(es, "a1_pp%d" % k, [128, 512], F32) for k in range(6)]
                Bpp = [Buf() for _ in range(6)]
                ptb = [self.ps(es, "a1_ptb%d" % k, [128, 1024], BF16) for k in range(2)]
                Bptb = [Buf(), Buf()]
                kiT = sm("kiT", [128, SEQ], BF16); BkiT = Buf("kiT")
                qiT = sm("qiT", [64, NIH, SEQ], BF16); BqiT = [Buf() for _ in range(NIH)]
                wi = sm("wi", [128, NT, 8]); Bwi = Buf("wi")
                wckv = sm("wckv", [128, KC, 128], BF16); wki = sm("wki", [128, KC, 72], BF16); Bwa = Buf()
                kvg = sm("kvg", [128, 128]); kig = sm("kig", [128, 64]); kib = sm("kib", [128, 64]); Bg = Buf()
                self.DMA("pool", wckv[:, :, :], awin[:, :, 2048:2176], (), [Bwa])
                self.DMA("pool", wki[:, :, :], awin[:, :, 2688:2760], (), [Bwa])
                self.DMA("sp", kvg[:, :], self.bcast_rows(d["kv_g"][j], 128), (), [Bg])
                self.DMA("sp", kig[:, :], self.bcast_rows(d["ki_g"][j], 64), (), [Bg])
                self.DMA("sp", kib[:, :], self.bcast_rows(d["ki_b"][j], 64), (), [Bg])
                st1 = [sm("st%d" % k, [128, 24]) for k in range(2)]; Bst1 = [Buf(), Buf()]
                junk = [sm("junk%d" % k, [128, 128]) for k in range(2)]
                kin = [sm("kin%d" % k, [128, 128], BF16) for k in range(2)]; Bkin = [Buf(), Buf()]
                ktmp = [sm("ktmp%d" % k, [128, 64]) for k in range(2)]
                bis = int(os.environ.get("BIS", "9"))
                def ck_A(i):
                    k = i % 2
                    pa, Bpa = pp[k], Bpp[k]
                    for kc in range(KC):
                        self.MM(pa[:, 0:128], self.xT[:, kc, i * 128:(i + 1) * 128], wckv[:, kc, :], kc == 0, kc == KC - 1,
                                [self.BxT[i], Bwa], [Bpa])
                    for kc in range(KC):
                        self.MM(pa[:, 128:200], self.xT[:, kc, i * 128:(i + 1) * 128], wki[:, kc, :], kc == 0, kc == KC - 1,
                                [self.BxT[i], Bwa], [Bpa])

                def ck_B(i):
                    k = i % 2
                    pa, Bpa = pp[k], Bpp[k]
                    st, Bs = st1[k], Bst1[k]
                    self.ACT(junk[k][:, :], pa[:, 0:128], AF.Square, [Bpa], [Bs], accum=st[:, 0:1])
                    self.ACT(st[:, 1:2], st[:, 0:1], AF.Sqrt, [Bs, self.Bconst], [Bs], scale=1.0 / 128.0, bias=self.eps_rms[:, 0:1])
                    S.op("dve", (lambda s_: (lambda e: e.reciprocal(out=s_[:, 1:2], in_=s_[:, 1:2])))(st), [Bs], [Bs])
                    self.STT("dve", ckv_tm[:, i, :], pa[:, 0:128], st[:, 1:2], kvg[:, :], ALU.mult, ALU.mult,
                             [Bpa, Bs, Bg], [Bckv])
                    S.op("dve", (lambda s_, p_: (lambda e: e.bn_stats(out=s_[:, 8:14], in_=p_[:, 128:192])))(st, pa), [Bpa], [Bs])
                    S.op("dve", (lambda s_: (lambda e: e.bn_aggr(out=s_[:, 14:16], in_=s_[:, 8:14])))(st), [Bs], [Bs])
                    self.ACT(st[:, 16:17], st[:, 15:16], AF.Sqrt, [Bs, self.Bconst], [Bs], scale=1.0, bias=self.eps_ln[:, 0:1])
                    S.op("dve", (lambda s_: (lambda e: e.reciprocal(out=s_[:, 16:17], in_=s_[:, 16:17])))(st), [Bs], [Bs])
                    self.TS("dve", ktmp[k][:, :], pa[:, 128:192], st[:, 14:15], ALU.subtract, [Bpa, Bs], [Bkin[k]],
                            s2=st[:, 16:17], op1=ALU.mult)
                    self.TT("dve", ktmp[k][:, :], ktmp[k][:, :], kig[:, :], ALU.mult, [Bkin[k], Bg], [Bkin[k]])
                    self.TT("dve", kin[k][:, 0:64], ktmp[k][:, :], kib[:, :], ALU.add, [Bkin[k], Bg], [Bkin[k]])
                    self.TT("dve", kin[k][:, 64:128], ktmp[k][:, :], kib[:, :], ALU.add, [Bkin[k], Bg], [Bkin[k]])
                    self.CP("act", wi[:, i, :], pa[:, 192:200], [Bpa], [Bwi])

                def ck_C(i):
                    k = i % 2
                    self.TR(ptb[k][:, 0:128], ckv_tm[:, i, :], self.ident_b[:], [Bckv, self.Bconst], [Bptb[k]])
                    self.TR(ptb[k][:, 128:256], kin[k][:, :], self.ident_b[:], [Bkin[k], self.Bconst], [Bptb[k]])
                    self.CP("dve", ckvT[:, i * 128:(i + 1) * 128], ptb[k][:, 0:128], [Bptb[k]], [BckvT])
                    self.CP("dve", kiT[:, i * 128:(i + 1) * 128], ptb[k][:, 128:256], [Bptb[k]], [BkiT])
                for s in range(NT + 2):
                    if s < NT:
                        ck_A(s)
                    if 0 <= s - 1 < NT:
                        ck_B(s - 1)
                    if 0 <= s - 2 < NT:
                        ck_C(s - 2)
                if att_stop <= 1:
                    S.flush()
                    return
                wq = [sm("wq%d" % k, [128, KC, 128], BF16) for k in range(2)]; Bwq = [Buf(), Buf()]
                qs = [sm("qs%d" % k, [128, 512], BF16) for k in range(4)]; Bqs = [Buf() for _ in range(4)]
                Bqd = Buf("qd")
                n = 0
                for h in range(NH):
                    k = h % 2
                    self.DMA("pool", wq[k][:, :, :], awin[:, :, h * 128:(h + 1) * 128], (), [Bwq[k]])
                    for tc in range(4):
                        po, Bpo = pp[2 + n % 4], Bpp[2 + n % 4]
                        for kc in range(KC):
                            self.MM(po[:, :], wq[k][:, kc, :], self.xT[:, kc, tc * 512:(tc + 1) * 512], kc == 0, kc == KC - 1,
                                    [Bwq[k]] + self.BxT[4 * tc:4 * tc + 4], [Bpo])
                        if n % 2:
                            self.ACT(qs[n % 4][:, :], po[:, :], AF.Copy, [Bpo], [Bqs[n % 4]], scale=128.0 ** -0.5)
                        else:
                            self.TS("dve", qs[n % 4][:, :], po[:, :], 128.0 ** -0.5, ALU.mult, [Bpo], [Bqs[n % 4]])
                        self.DMA("sp", qd[h, :, tc * 512:(tc + 1) * 512], qs[n % 4][:, :], [Bqs[n % 4]], [Bqd])
                        n += 1
                wqi = [sm("wqi%d" % k, [128, KC, 64], BF16) for k in range(2)]; Bwqi = [Buf(), Buf()]
                for h in range(NIH):
                    k = h % 2
                    self.DMA("pool", wqi[k][:, :, :], awin[:, :, 2176 + h * 64:2176 + (h + 1) * 64], (), [Bwqi[k]])
                    for tc in range(4):
                        po, Bpo = pp[2 + n % 4], Bpp[2 + n % 4]
                        for kc in range(KC):
                            self.MM(po[0:64, :], wqi[k][:, kc, :], self.xT[:, kc, tc * 512:(tc + 1) * 512], kc == 0, kc == KC - 1,
                                    [Bwqi[k]] + self.BxT[4 * tc:4 * tc + 4], [Bpo])
                        self.CP("act" if n % 2 else "dve", qiT[:, h, tc * 512:(tc + 1) * 512], po[0:64, :], [Bpo], [BqiT[h]])
                        n += 1
                if att_stop <= 2:
                    S.flush()
                    return
                U32 = mybir.dt.uint32
                NIT = 18
                sc4s = [sm("sc4_%d" % k, [128, 4, SEQ]) for k in range(2)]
                Bscs = [[Buf() for _ in range(4)] for _ in range(2)]
                junkb = sm("junkb", [128, SEQ], BF16)
                junka = junkb
                lhs_ = [sm("lohi%d" % k, [128, 5, 4]) for k in range(2)]; Blhs = [Buf("lohi0"), Buf("lohi1")]
                Bcas = [Buf("cnta0"), Buf("cnta1")]; Bcds = [Buf("cntd0"), Buf("cntd1")]; Btvs = [Buf("tv0"), Buf("tv1")]
                tvs = [sm("tv%d" % k, [128, 4]) for k in range(2)]
                pm = sm("predm", [128, 2, 4], U32)
                NRL = 12
                rl = [sm("rl%d" % k, [128, 512]) for k in range(NRL)]; Brl = [Buf() for _ in range(NRL)]
                maskb = [sm("maskb%d" % k, [128, SEQ], BF16) for k in range(1)] * 2; Bmk = [Buf()] * 2
                mT = [sm("mT%d" % k, [128, NT, 128], BF16) for k in range(1)] * 2; BmT = [Buf()] * 2
                Bmd = Buf("maskT_d")
                self.Bmd = Bmd
                cnt_ps = [0]

                def score_steps(g):
                    sc4, Bsc, lh, Blh = sc4s[g % 2], Bscs[g % 2], lhs_[g % 2], Blhs[g % 2]
                    steps = []
                    for q in range(4):
                        qb = 4 * g + q
                        L = (qb + 1) * 128
                        for c0 in range(0, L, 512):
                            nn = min(512, L - c0)
                            for h in range(NIH):
                                def mk_step(q=q, qb=qb, c0=c0, nn=nn, h=h):
                                    stt = {}

                                    def p1():
                                        i_ = cnt_ps[0]
                                        cnt_ps[0] += 1
                                        pS, BpS = pp[i_ % 2], Bpp[i_ % 2]
                                        r_, Br_ = rl[i_ % NRL], Brl[i_ % NRL]
                                        stt["r"] = (r_, Br_)
                                        self.MM(pS[:, 0:nn], qiT[:, h, qb * 128:(qb + 1) * 128], kiT[0:64, c0:c0 + nn], True, True,
                                                [BqiT[h], BkiT], [BpS])
                                        self.ACT(r_[:, 0:nn], pS[:, 0:nn], AF.Relu, [BpS], [Br_])

                                    def p2():
                                        r_, Br_ = stt["r"]
                                        sc_ = sc4[:, q, :]
                                        Bs_ = Bsc[q]
                                        if h == 0:
                                            self.TS("dve", sc_[:, c0:c0 + nn], r_[:, 0:nn], wi[:, qb, 0:1], ALU.mult, [Br_, Bwi], [Bs_])
                                        else:
                                            self.STT("dve", sc_[:, c0:c0 + nn], r_[:, 0:nn], wi[:, qb, h:h + 1], sc_[:, c0:c0 + nn],
                                                     ALU.mult, ALU.add, [Br_, Bwi, Bs_], [Bs_])
                                    return p1, p2
                                steps.append(mk_step())

                        def post(q=q, qb=qb, L=L):
                            sc_ = sc4[:, q, :]
                            Bs_ = Bsc[q]
                            if qb >= 2:
                                self.RED(lh[:, 0, q:q + 1], sc_[:, 0:L], ALU.min, [Bs_], [Blh])
                            dg = sc_[:, qb * 128:(qb + 1) * 128]
                            S.op("pool", (lambda a: (lambda e: e.affine_select(out=a, in_=a, pattern=[[-1, 128]], compare_op=ALU.is_ge,
                                                                              fill=NEG, base=0, channel_multiplier=1)))(dg), [Bs_], [Bs_])
                            if qb >= 2:
                                self.RED(lh[:, 1, q:q + 1], sc_[:, 0:L], ALU.max, [Bs_], [Blh])
                                Lq = L
                                act_q = (q >= (2 if g == 3 else 1)) if g > 0 else (q == 3)
                                self.MEMSET("pool", tvs[g % 2][:, q:q + 1], float(Lq - 2 * TOPK) if act_q else float(Lq - TOPK), [Btvs[g % 2]])
                        steps.append((lambda: None, post))
                    return steps

                def bisect_first(g):
                    sc4, Bsc, lh, Blh, tv = sc4s[g % 2], Bscs[g % 2], lhs_[g % 2], Blhs[g % 2], tvs[g % 2]
                    Bca, Bcd = Bcas[g % 2], Bcds[g % 2]
                    qs_need = [q for q in range(4) if 4 * g + q >= 2]
                    q0 = qs_need[0]
                    act_qs = [q for q in qs_need if ((q >= (2 if g == 3 else 1)) if g > 0 else (q == 3))]
                    self.TT("dve", lh[:, 2, q0:4], lh[:, 0, q0:4], lh[:, 1, q0:4], ALU.add, [Blh], [Blh])
                    self.TS("dve", lh[:, 2, q0:4], lh[:, 2, q0:4], 0.5, ALU.mult, [Blh], [Blh])
                    for q in qs_need:
                        L = (4 * g + q + 1) * 128
                        if q in act_qs:
                            self.ACT(junka[:, 0:L], sc4[:, q, 0:L], AF.Sign, [Bsc[q], Blh], [Bca], bias=lh[:, 2, q:q + 1],
                                     scale=-1.0, accum=lh[:, 3, q:q + 1])
                    for q in qs_need:
                        L = (4 * g + q + 1) * 128
                        if q not in act_qs:
                            self.TS("dve", junkb[:, 0:L], sc4[:, q, 0:L], lh[:, 2, q:q + 1], ALU.is_lt, [Bsc[q], Blh], [Bcd],
                                    op1=ALU.add, accum=lh[:, 3, q:q + 1])

                def bisect_second(g):
                    lh, Blh, tv = lhs_[g % 2], Blhs[g % 2], tvs[g % 2]
                    Bca, Bcd, Btv = Bcas[g % 2], Bcds[g % 2], Btvs[g % 2]
                    qs_need = [q for q in range(4) if 4 * g + q >= 2]
                    q0 = qs_need[0]
                    self.TT("dve", pm[:, 0, q0:4], lh[:, 3, q0:4], tv[:, q0:4], ALU.is_le, [Bca, Bcd, Btv], [Blh])
                    self.TT("dve", pm[:, 1, q0:4], lh[:, 3, q0:4], tv[:, q0:4], ALU.is_gt, [Bca, Bcd, Btv], [Blh])
                    S.op("dve", (lambda a, l_: (lambda e: e.copy_predicated(out=l_[:, 0, a:4], mask=pm[:, 0, a:4], data=l_[:, 2, a:4])))(q0, lh),
                         [Blh], [Blh])
                    S.op("dve", (lambda a, l_: (lambda e: e.copy_predicated(out=l_[:, 1, a:4], mask=pm[:, 1, a:4], data=l_[:, 2, a:4])))(q0, lh),
                         [Blh], [Blh])

                def emit_masks(g):
                    sc4, Bsc, lh, Blh = sc4s[g % 2], Bscs[g % 2], lhs_[g % 2], Blhs[g % 2]
                    for q in range(4):
                        qb = 4 * g + q
                        k = qb % 2
                        L = (qb + 1) * 128
                        if qb >= 2:
                            self.TS("dve", maskb[k][:, 0:L], sc4[:, q, 0:L], lh[:, 0, q:q + 1], ALU.is_lt, [Bsc[q], Blh], [Bmk[k]],
                                    s2=MASK_NEG, op1=ALU.mult)
                        else:
                            self.TS("dve", maskb[k][:, 0:L], sc4[:, q, 0:L], -1.0e29, ALU.is_lt, [Bsc[q]], [Bmk[k]],
                                    s2=MASK_NEG, op1=ALU.mult)
                        for g0 in range(0, qb + 1, 8):
                            gn = min(8, qb + 1 - g0)
                            pt, Bpt = ptb[(g0 // 8) % 2], Bptb[(g0 // 8) % 2]
                            for qq in range(gn):
                                st_ = g0 + qq
                                self.TR(pt[:, qq * 128:(qq + 1) * 128], maskb[k][:, st_ * 128:(st_ + 1) * 128], self.ident_b[:],
                                        [Bmk[k], self.Bconst], [Bpt])
                            self.CP("dve", mT[k][:, g0:g0 + gn, :].rearrange("p s t -> p (s t)"), pt[:, 0:gn * 128], [Bpt], [BmT[k]])
                        self.DMA("sp", d["maskT"][0:L, qb * 128:(qb + 1) * 128].rearrange("(s p) t -> p s t", p=128),
                                 mT[k][:, 0:qb + 1, :], [BmT[k]], [Bmd])

                for p1, p2 in score_steps(0):
                    p1()
                    p2()
                for g in range(4):
                    nxt = score_steps(g + 1) if g < 3 else []
                    per = (len(nxt) + NIT - 1) // NIT if nxt else 0
                    batches = [nxt[it * per:(it + 1) * per] for it in range(NIT)]
                    for p1, _ in batches[0]:
                        p1()
                    for it in range(NIT):
                        bisect_first(g)
                        nb_ = batches[it + 1] if it + 1 < NIT else []
                        hb_ = len(nb_) // 2
                        for p1, _ in nb_[:hb_]:
                            p1()
                        for _, p2 in batches[it]:
                            p2()
                        for p1, _ in nb_[hb_:]:
                            p1()
                        bisect_second(g)
                    emit_masks(g)
                S.flush()
            if att_stop <= 3:
                return
            with ExitStack() as es:
                sm = lambda name, shape, dt=F32: self.sb(es, "a3_" + name, shape, dt)
                pp = [self.ps(es, "a3_pp%d" % k, [128, 512], F32) for k in range(8)]
                Bpp = [Buf() for _ in range(8)]
                c = self.ln_setup(es, "a3", d["ln1_g"][l], d["ln1_b"][l], l, router=not to_out, nbuf=2)
                wout = sm("wout", [128, NH, D], BF16); Bwout = Buf()
                self.DMA("pool", wout[:, :, :], d["awout%d" % j].rearrange("(h c) f -> c h f", c=128), (), [Bwout])
                qtc = [sm("qtc%d" % k, [128, NH, 512], BF16) for k in range(2)]; Bqtc = [Buf(), Buf()]
                mk = sm("mk", [128, NT, 512], BF16); Bmk3 = Buf()
                oT = sm("oT", [128, NH, 512], BF16); BoT = [Buf() for _ in range(NH)]
                pT = [sm("pT%d" % k, [128, 512], BF16) for k in range(3)]; BpT = [Buf() for _ in range(3)]
                rec = sm("rec", [128, 512]); Brec = Buf()
                dst = d["out"] if to_out else d["xres"]
                npt = 0
                npl = 0
                for tc in range(4):
                    kq = tc % 2
                    nst = 4 * (tc + 1)
                    self.DMA("sp", qtc[kq][:, :, :], qd[:, :, tc * 512:(tc + 1) * 512].rearrange("h c t -> c h t"),
                             [Bqd], [Bqtc[kq]])
                    self.DMA("sp", mk[:, 0:nst, :],
                             d["maskT"][0:nst * 128, tc * 512:(tc + 1) * 512].rearrange("(s p) t -> p s t", p=128),
                             [Bmd], [Bmk3])
                    for h in range(NH):
                        po, Bpo = pp[2 + h % 2], Bpp[2 + h % 2]
                        pd, Bpd = pp[4 + h % 2], Bpp[4 + h % 2]

                        def emit_qk(st_, h=h):
                            nonlocal npl
                            c0 = max(0, st_ - 4 * tc) * 128
                            pl, Bpl = pp[npl % 2], Bpp[npl % 2]
                            npl += 1
                            extra = []
                            for dl in (0, 1):
                                tt = st_ + dl
                                if 4 * tc <= tt <= 4 * tc + 3:
                                    extra.append((dl, (tt - 4 * tc) * 128))
                            self.MM(pl[:, c0:512], ckvT[:, st_ * 128:(st_ + 1) * 128], qtc[kq][:, h, c0:512], True, False,
                                    [BckvT, Bqtc[kq]], [Bpl])
                            for ei, (dl, cc) in enumerate(extra):
                                self.MM(pl[:, cc:cc + 128], self.ident_b[:, :], self.BtT[:, h, dl, :], False, False,
                                        [self.Bconst], [Bpl])
                            self.MM(pl[:, c0:512], self.ident_b[:, :], mk[:, st_, c0:512], False, True, [self.Bconst, Bmk3], [Bpl])
                            return pl, Bpl, c0
                        nxt = emit_qk(0)
                        for st_ in range(nst):
                            pl, Bpl, c0 = nxt
                            if st_ + 1 < nst:
                                nxt = emit_qk(st_ + 1)
                            p_, Bp_ = pT[npt % 3], BpT[npt % 3]
                            npt += 1
                            self.ACT(p_[:, c0:512], pl[:, c0:512], AF.Exp, [Bpl], [Bp_])
                            self.MM(po[:, c0:512], ckv_tm[:, st_, :], p_[:, c0:512], st_ == 0, st_ == nst - 1, [Bckv, Bp_], [Bpo])
                            self.MM(pd[:, c0:512], self.ones_b[:, :], p_[:, c0:512], st_ == 0, st_ == nst - 1, [self.Bconst, Bp_], [Bpd])
                        S.op("dve", (lambda pd_: (lambda e: e.reciprocal(out=rec[:, :], in_=pd_[:, :])))(pd), [Bpd], [Brec])
                        self.TT("dve", oT[:, h, :], po[:, :], rec[:, :], ALU.mult, [Bpo, Brec], [BoT[h]])
                    py = [pp[6], pp[7]]
                    Bpy = [Bpp[6], Bpp[7]]

                    def yfn(i, tc=tc):
                        q = i - 4 * tc
                        for hf in range(2):
                            for h in range(NH):
                                self.MM(py[hf][:, :], oT[:, h, q * 128:(q + 1) * 128], wout[:, h, hf * 512:(hf + 1) * 512],
                                        h == 0, h == NH - 1, [BoT[h], Bwout], [Bpy[hf]])
                        return [py[0][:, :], py[1][:, :]], Bpy

                    def ptsfn(i):
                        return [pp[0], pp[1]], [Bpp[0], Bpp[1]], pp[2], Bpp[2]
                    self.ln_run(c, [4 * tc + q for q in range(4)], yfn, dst, ptsfn)
                S.flush()


def _t5_onehot():
    n_buckets, max_distance = 32, 128
    max_exact = n_buckets // 2
    oh = np.zeros((32, VW), np.float32)
    for jj in range(VW):
        dd = jj - 127
        if dd < 0:
            continue
        d_f = np.float32(max(dd, 1))
        large = max_exact + int(np.float32(np.log(d_f / np.float32(max_exact), dtype=np.float32)
                                           / np.float32(np.log(max_distance / max_exact))
                                           * np.float32(n_buckets - max_exact)))
        large = min(large, n_buckets - 1)
        bkt = dd if dd < max_exact else large
        oh[bkt, jj] += 1.0
        oh[n_buckets - 1, jj] -= 1.0
    return oh


def _prep_shared(inputs):
    f = lambda a: np.ascontiguousarray(np.asarray(a, dtype=np.float32))
    m = {}
    for j in range(2):
        m["cwin%d" % j] = f(inputs["conv_w_in"][j])
        m["cwout%d" % j] = f(inputs["conv_w_out"][j])
        m["awin%d" % j] = f(inputs["attn_w_in"][j])
        m["awout%d" % j] = f(inputs["attn_w_out"][j])
    m["conv_k"] = f(inputs["conv_k"])
    m["kv_g"] = f(inputs["kv_norm_g"])
    m["ki_g"] = f(inputs["kidx_ln_g"])
    m["ki_b"] = f(inputs["kidx_ln_b"])
    m["rel_bias"] = f(inputs["rel_bias"])
    m["ohs"] = _t5_onehot()
    m["rw"] = f(np.concatenate([np.asarray(inputs["router_wg"]), np.asarray(inputs["router_we"])], axis=2))
    m["rb"] = f(np.concatenate([np.asarray(inputs["router_bg"]), np.asarray(inputs["router_be"])], axis=1))
    for l in range(DEPTH):
        w1 = np.asarray(inputs["exp_w1"][l], dtype=np.float32).reshape(64, KC, 128, 256)
        m["w1r%d" % l] = np.ascontiguousarray(w1.transpose(0, 2, 1, 3)).reshape(64 * 128, 2048)
        w3 = np.asarray(inputs["exp_w3"][l], dtype=np.float32).reshape(64, KC, 128, 256)
        m["w3r%d" % l] = np.ascontiguousarray(w3.transpose(0, 2, 1, 3)).reshape(64 * 128, 2048)
        w2 = np.asarray(inputs["exp_w2"][l], dtype=np.float32).reshape(64, 2, 128, D)
        m["w2r%d" % l] = np.ascontiguousarray(w2.transpose(0, 2, 1, 3)).reshape(64 * 128, 2048)
    for nm in ("ln1_g", "ln1_b", "ln2_g", "ln2_b"):
        m[nm] = f(inputs[nm])
    return m


_PROG_CACHE = {}


def _get_prog(n_layers=DEPTH, stop_after_mixer=False):
    key = (n_layers, stop_after_mixer)
    if key not in _PROG_CACHE:
        _PROG_CACHE[key] = Prog(n_layers, stop_after_mixer).build()
    return _PROG_CACHE[key]


def kernel(**inputs):
    x = np.asarray(inputs["x"], dtype=np.float32)
    n = x.shape[0]
    shared = _prep_shared(inputs)
    nc = _get_prog()
    in_maps = []
    for b in range(n):
        m = dict(shared)
        m["x"] = np.ascontiguousarray(x[b])
        in_maps.append(m)
    res = run_bass_kernel_spmd(nc, in_maps, core_ids=list(range(n)))
    return np.stack([np.asarray(r["out"]) for r in res.results], axis=0).astype(np.float32)
```

```python
import numpy as np
from contextlib import ExitStack
import concourse.bass as bass
import concourse.mybir as mybir
from concourse.bass_utils import run_bass_kernel_spmd

F32 = mybir.dt.float32
BF16 = mybir.dt.bfloat16
I32 = mybir.dt.int32
AF = mybir.ActivationFunctionType
ALU = mybir.AluOpType
AX = mybir.AxisListType

SEQ = 2048
D = 1024
NT = 16
KC = 8
DEPTH = 4
NH = 16
NIH = 8
TOPK = 256
NBLK = 96
ALPHA = (2.0 * DEPTH) ** 0.25
LN_EPS = 1e-5
RMS_EPS = 1e-6
NEG = -1.0e30
MASK_NEG = -30000.0
VW = 383

COMPUTE = ("pe", "act", "dve", "pool")
ALL_STREAMS = ("pe", "act", "dve", "pool", "sp")


class Buf:
    __slots__ = ("name", "lw", "rd")

    def __init__(self, name=""):
        self.name = name
        self.lw = None
        self.rd = {}


class Sched:
    def __init__(self, nc, es, n_dma_sems=40):
        self.nc = nc
        self.streams = {e: [] for e in ALL_STREAMS}
        self.count = {e: 0 for e in COMPUTE}
        self.waited = {e: {} for e in ALL_STREAMS}
        self.n_dma_sems = n_dma_sems
        self.dma_val = [0] * n_dma_sems
        self.dma_rr = 0
        self.sems = {}
        for e in COMPUTE:
            self.sems[e] = es.enter_context(nc.semaphore("sem_" + e))
        for s in range(n_dma_sems):
            self.sems[("dma", s)] = es.enter_context(nc.semaphore("semd%d" % s))
        self.n_ops = 0

    def _need(self, eng, deps):
        st = self.streams[eng]
        w = self.waited[eng]
        for key, val in deps.items():
            if key == eng:
                if eng == "pe" or self.count[eng] - val >= 3:
                    continue
            if w.get(key, 0) >= val:
                continue
            w[key] = val
            st.append(("wait", key, val))

    def _collect(self, reads, writes):
        deps = {}
        for b in reads:
            if b.lw is not None and deps.get(b.lw[0], 0) < b.lw[1]:
                deps[b.lw[0]] = b.lw[1]
        for b in writes:
            if b.lw is not None and deps.get(b.lw[0], 0) < b.lw[1]:
                deps[b.lw[0]] = b.lw[1]
            for k, v in b.rd.items():
                if deps.get(k, 0) < v:
                    deps[k] = v
        return deps

    def _commit(self, tok, reads, writes):
        k, v = tok
        for b in writes:
            b.lw = tok
            b.rd = {}
        for b in reads:
            if b.rd.get(k, 0) < v:
                b.rd[k] = v

    def op(self, eng, fn, reads=(), writes=()):
        deps = self._collect(reads, writes)
        self._need(eng, deps)
        self.count[eng] += 1
        tok = (eng, self.count[eng])
        self.streams[eng].append(("op", fn, eng))
        self._commit(tok, reads, writes)
        self.n_ops += 1
        return tok

    def dma(self, q, fn, reads=(), writes=()):
        deps = self._collect(reads, writes)
        s = self.dma_rr
        self.dma_rr = (self.dma_rr + 1) % self.n_dma_sems
        key = ("dma", s)
        if self.dma_val[s] > 0:
            deps[key] = max(deps.get(key, 0), self.dma_val[s])
        self._need(q, deps)
        self.dma_val[s] += 16
        tok = (key, self.dma_val[s])
        self.streams[q].append(("dma", fn, key))
        self._commit(tok, reads, writes)
        self.n_ops += 1
        return tok

    def flush(self):
        deps = {("dma", s): self.dma_val[s] for s in range(self.n_dma_sems) if self.dma_val[s] > 0}
        self._need("sp", deps)
        for e in ALL_STREAMS:
            self._need(e, {f: self.count[f] for f in COMPUTE if f != e and self.count[f] > 0})
        sems = self.sems
        streams = self.streams

        def run(stream):
            def body(e):
                for it in stream:
                    if it[0] == "wait":
                        e.wait_ge(sems[it[1]], it[2])
                    elif it[0] == "raw":
                        it[1](e)
                    elif it[0] == "op":
                        it[1](e).then_inc(sems[it[2]], 1)
                    else:
                        it[1](e).then_inc(sems[it[2]], 16)
            return body
        with self.nc.Block() as block:
            block.tensor(run(streams["pe"]))
            block.scalar(run(streams["act"]))
            block.vector(run(streams["dve"]))
            block.gpsimd(run(streams["pool"]))
            block.sync(run(streams["sp"]))
        self.streams = {e: [] for e in ALL_STREAMS}
        for e in ALL_STREAMS:
            for f in COMPUTE:
                self.waited[e][f] = self.count[f]
            for s in range(self.n_dma_sems):
                self.waited[e][("dma", s)] = self.dma_val[s]


class Prog:
    def __init__(self, n_layers=DEPTH, stop_after_mixer=False):
        self.n_layers = n_layers
        self.stop_after_mixer = stop_after_mixer
        self.nc = bass.Bass("TRN2", target_bir_lowering=False)

    def MM(self, out, lhsT, rhs, start, stop, r, w):
        self.S.op("pe", lambda e: e.matmul(out, lhsT=lhsT, rhs=rhs, start=start, stop=stop), r, w)

    def TR(self, out, in_, ident, r, w):
        self.S.op("pe", lambda e: e.transpose(out=out, in_=in_, identity=ident), r, w)

    def ACT(self, out, in_, func, r, w, scale=None, bias=None, accum=None):
        kw = {}
        if scale is not None:
            kw["scale"] = scale
        if bias is not None:
            kw["bias"] = bias
        if accum is not None:
            kw["accum_out"] = accum
        self.S.op("act", lambda e: e.activation(out=out, in_=in_, func=func, **kw), r, w)

    def TS(self, eng, out, in0, s1, op0, r, w, s2=None, op1=None, accum=None):
        kw = {}
        if op1 is not None:
            kw["op1"] = op1
        if accum is not None:
            kw["accum_out"] = accum
        self.S.op(eng, lambda e: e.tensor_scalar(out=out, in0=in0, scalar1=s1, scalar2=s2, op0=op0, **kw), r, w)

    def TT(self, eng, out, in0, in1, op, r, w):
        self.S.op(eng, lambda e: e.tensor_tensor(out=out, in0=in0, in1=in1, op=op), r, w)

    def STT(self, eng, out, in0, scalar, in1, op0, op1, r, w):
        self.S.op(eng, lambda e: e.scalar_tensor_tensor(out=out, in0=in0, scalar=scalar, in1=in1, op0=op0, op1=op1), r, w)

    def CP(self, eng, out, in_, r, w):
        if eng == "act":
            self.S.op("act", lambda e: e.copy(out=out, in_=in_), r, w)
        else:
            self.S.op(eng, lambda e: e.tensor_copy(out=out, in_=in_), r, w)

    def RED(self, out, in_, op, r, w, axis=AX.X):
        self.S.op("dve", lambda e: e.tensor_reduce(out=out, in_=in_, axis=axis, op=op), r, w)

    def MEMSET(self, eng, ap, val, w):
        self.S.op(eng, lambda e: e.memset(ap, val), (), w)

    def DMA(self, q, out, in_, r, w, **kw):
        self.S.dma(q, lambda e: e.dma_start(out=out, in_=in_, **kw), r, w)

    def GATHER(self, out, table, idx, r, w, bound=None):
        if bound is None:
            self.S.dma("pool", lambda e: e.indirect_dma_start(
                out=out, out_offset=None, in_=table, in_offset=bass.IndirectOffsetOnAxis(ap=idx, axis=0)), r, w)
        else:
            self.S.dma("pool", lambda e: e.indirect_dma_start(
                out=out, out_offset=None, in_=table, in_offset=bass.IndirectOffsetOnAxis(ap=idx, axis=0),
                bounds_check=self.bound_reg, oob_is_err=False), r, w)

    def SCATTER(self, table, idx, in_, r, w):
        self.S.dma("pool", lambda e: e.indirect_dma_start(
            out=table, out_offset=bass.IndirectOffsetOnAxis(ap=idx, axis=0), in_=in_, in_offset=None), r, w)

    def sb(self, es, name, shape, dt):
        self.uid = getattr(self, "uid", 0) + 1
        return es.enter_context(self.nc.sbuf_tensor("%s_u%d" % (name, self.uid), shape, dt))

    def ps(self, es, name, shape, dt):
        self.uid = getattr(self, "uid", 0) + 1
        return es.enter_context(self.nc.psum_tensor("%s_u%d" % (name, self.uid), shape, dt))

    def declare_dram(self):
        nc = self.nc

        def inp(name, shape, dt=F32):
            return nc.dram_tensor(name, shape, dt, kind="ExternalInput").ap()

        def internal(name, shape, dt):
            return nc.dram_tensor(name, shape, dt, kind="Internal").ap()
        d = {}
        d["x"] = inp("x", [SEQ, D])
        for j in range(2):
            d["cwin%d" % j] = inp("cwin%d" % j, [D, 3 * D])
            d["cwout%d" % j] = inp("cwout%d" % j, [D, D])
            d["awin%d" % j] = inp("awin%d" % j, [D, 2760])
            d["awout%d" % j] = inp("awout%d" % j, [2048, D])
        d["conv_k"] = inp("conv_k", [2, 3, D])
        d["kv_g"] = inp("kv_g", [2, 128])
        d["ki_g"] = inp("ki_g", [2, 64])
        d["ki_b"] = inp("ki_b", [2, 64])
        d["rel_bias"] = inp("rel_bias", [32, 16])
        d["ohs"] = inp("ohs", [32, VW])
        d["rw"] = inp("rw", [DEPTH, D, 72])
        d["rb"] = inp("rb", [DEPTH, 72])
        for l in range(DEPTH):
            d["w1r%d" % l] = inp("w1r%d" % l, [64 * 128, 2048])
            d["w3r%d" % l] = inp("w3r%d" % l, [64 * 128, 2048])
            d["w2r%d" % l] = inp("w2r%d" % l, [64 * 128, 2048])
        d["ln1_g"] = inp("ln1_g", [DEPTH, D])
        d["ln1_b"] = inp("ln1_b", [DEPTH, D])
        d["ln2_g"] = inp("ln2_g", [DEPTH, D])
        d["ln2_b"] = inp("ln2_b", [DEPTH, D])
        d["out"] = nc.dram_tensor("out", [SEQ, D], F32, kind="ExternalOutput").ap()
        d["xres"] = internal("xres_d", [SEQ, D], F32)
        d["xs"] = internal("xs_d", [NBLK * 128, D], BF16)
        d["ys"] = internal("ys_d", [NBLK * 128, D], F32)
        d["maskT"] = internal("maskT_d", [SEQ, SEQ], BF16)
        d["t5"] = internal("t5_d", [16 * 128 * (VW + 1)], F32)
        d["qT"] = internal("qT_d", [NH, 128, SEQ], BF16)
        self.d = d

    @staticmethod
    def bcast_rows(vec_ap, n):
        return bass.AP(tensor=vec_ap.tensor, offset=vec_ap.offset, ap=[[0, 128], [1, n]])

    def build(self):
        nc = self.nc
        self.declare_dram()
        with ExitStack() as es:
            self.S = Sched(nc, es)
            S = self.S
            self.xa = self.sb(es, "xa", [128, 16384], BF16)
            self.xT = self.xa[:, :].rearrange("p (k t) -> p k t", k=KC)
            self.xb = self.xa[:, :].rearrange("p (i d) -> p i d", i=NT)
            self.BxT = [Buf("xT%d" % i) for i in range(NT)]
            self.Bxb = [Buf("xb%d" % i) for i in range(NT)]
            self.lg = self.sb(es, "lg", [128, NT, 72], F32)
            self.Blg = Buf("lg")
            self.ident_f = self.sb(es, "ident_f", [128, 128], F32)
            self.ident_b = self.sb(es, "ident_b", [128, 128], BF16)
            self.ones_b = self.sb(es, "ones_b", [128, 128], BF16)
            self.tri_b = self.sb(es, "tri_b", [128, 128], BF16)
            self.ones_row = self.sb(es, "ones_row", [1, 128], F32)
            self.eps_ln = self.sb(es, "eps_ln", [128, 1], F32)
            self.eps_rms = self.sb(es, "eps_rms", [128, 1], F32)
            self.pidx = self.sb(es, "pidx", [128, 1], F32)
            self.bvals = self.sb(es, "bvals", [128, NBLK], F32)
            self.BtT = self.sb(es, "BtT", [128, NH, 2, 128], BF16)
            self.Bconst = Buf("const")
            self.phase_init()
            self.phase_load_x()
            import os
            skip0 = int(os.environ.get("SKIP0", "0"))
            for l in range(self.n_layers):
                if l < skip0:
                    continue
                last_mixer = self.stop_after_mixer and l == self.n_layers - 1
                if l % 2 == 0:
                    self.phase_conv(l, to_out=last_mixer)
                else:
                    self.phase_attn(l, to_out=last_mixer)
                if last_mixer:
                    break
                self.phase_moe(l, to_out=(l == self.n_layers - 1))
        return nc

    def phase_init(self):
        S = self.S
        Bc = self.Bconst
        with ExitStack() as es:
            tmpf = self.sb(es, "init_tmpf", [128, 128], F32)
            tmpi = self.sb(es, "init_tmpi", [128, NBLK], I32)
            Bt = Buf()
            Bi = Buf()
            self.MEMSET("pool", self.ident_f[:], 1.0, [Bc])
            S.op("pool", lambda e: e.affine_select(out=self.ident_f[:], in_=self.ident_f[:], pattern=[[-1, 128]],
                                                   compare_op=ALU.is_equal, fill=0.0, base=0, channel_multiplier=1),
                 [Bc], [Bc])
            self.CP("dve", self.ident_b[:], self.ident_f[:], [Bc], [Bc])
            self.MEMSET("dve", self.ones_b[:], 1.0, [Bc])
            self.MEMSET("pool", tmpf[:], 1.0, [Bt])
            S.op("pool", lambda e: e.affine_select(out=tmpf[:], in_=tmpf[:], pattern=[[1, 128]],
                                                   compare_op=ALU.is_ge, fill=0.0, base=-1, channel_multiplier=-1),
                 [Bt], [Bt])
            self.CP("dve", self.tri_b[:], tmpf[:], [Bt], [Bc])
            self.MEMSET("dve", self.ones_row[:], 1.0, [Bc])
            self.MEMSET("dve", self.eps_ln[:], LN_EPS, [Bc])
            self.MEMSET("dve", self.eps_rms[:], RMS_EPS, [Bc])
            S.op("pool", lambda e: e.iota(tmpi[:, 0:1], pattern=[[0, 1]], base=0, channel_multiplier=1), (), [Bi])
            self.CP("dve", self.pidx[:], tmpi[:, 0:1], [Bi], [Bc])
            S.op("pool", lambda e: e.iota(tmpi[:, :], pattern=[[128, NBLK]], base=0, channel_multiplier=0), [Bi], [Bi])
            self.CP("dve", self.bvals[:], tmpi[:, :], [Bi], [Bc])
            if self.n_layers >= 2:
                self.init_t5(es)
            S.flush()

    def init_t5(self, es):
        S = self.S
        d = self.d
        rel = self.sb(es, "t5_rel", [32, 16], F32)
        ohs = self.sb(es, "t5_ohs", [32, VW], F32)
        rhs = self.sb(es, "t5_rhs", [32, NH * VW], F32)
        ones32 = self.sb(es, "t5_ones", [32, 128], F32)
        vec = self.sb(es, "t5_vec", [128, NH, VW], F32)
        btf = self.sb(es, "t5_btf", [128, NH, 2, 128], F32)
        Br, Bo, Brhs, B1, Bv = Buf(), Buf(), Buf(), Buf(), Buf()
        pp = [self.ps(es, "t5_ps%d" % i, [128, 512], F32) for i in range(2)]
        Bpp = [Buf(), Buf()]
        self.DMA("sp", rel[:], d["rel_bias"], (), [Br])
        self.DMA("sp", ohs[:], d["ohs"], (), [Bo])
        self.MEMSET("dve", ones32[:], 1.0, [B1])
        for h in range(NH):
            self.TS("dve", rhs[:, h * VW:(h + 1) * VW], ohs[:], rel[:, h:h + 1], ALU.mult, [Br, Bo], [Brhs])
        tot = NH * VW
        vflat = vec[:, :, :].rearrange("p h v -> p (h v)")
        c = 0
        k = 0
        while c < tot:
            n = min(512, tot - c)
            self.MM(pp[k % 2][:, 0:n], ones32[:, :], rhs[:, c:c + n], True, True, [B1, Brhs], [Bpp[k % 2]])
            self.CP("act" if k % 2 else "dve", vflat[:, c:c + n], pp[k % 2][:, 0:n], [Bpp[k % 2]], [Bv])
            c += n
            k += 1
        t5 = d["t5"]
        dst = bass.AP(tensor=t5.tensor, offset=0, ap=[[VW + 1, 128], [128 * (VW + 1), NH], [1, VW]])
        Bd = Buf()
        self.DMA("sp", dst, vec[:, :, :], [Bv], [Bd])
        src = bass.AP(tensor=t5.tensor, offset=127, ap=[[VW, 128], [128 * (VW + 1), NH], [128, 2], [1, 128]])
        Bbt = Buf()
        self.DMA("sp", btf[:, :, :, :], src, [Bd], [Bbt])
        self.CP("dve", self.BtT[:, :, :, :], btf[:, :, :, :], [Bbt], [self.Bconst])

    def to_xT(self, src, Bsrc, i, pts, Bpts):
        for half in range(2):
            pt, Bp = pts[half], Bpts[half]
            for q in range(4):
                kc = half * 4 + q
                self.TR(pt[:, q * 128:(q + 1) * 128], src[:, kc * 128:(kc + 1) * 128], self.ident_f[:],
                        [Bsrc, self.Bconst], [Bp])
            dst = self.xT[:, half * 4:(half + 1) * 4, i * 128:(i + 1) * 128]
            self.CP("act" if half else "dve", dst, pt[:, :].rearrange("p (q t) -> p q t", q=4), [Bp],
                    [self.BxT[i]] + self.Bxb)

    def phase_load_x(self):
        S = self.S
        d = self.d
        with ExitStack() as es:
            xt = [self.sb(es, "lx_xt%d" % k, [128, D], F32) for k in range(2)]
            Bx = [Buf(), Buf()]
            pts = [self.ps(es, "lx_pt%d" % k, [128, 512], F32) for k in range(4)]
            Bp = [Buf() for _ in range(4)]
            Bxr = Buf()
            self.DMA("sp", d["xres"], d["x"], (), [Bxr])
            for i in range(NT):
                k = i % 2
                self.DMA("sp", xt[k][:], d["x"][i * 128:(i + 1) * 128, :], (), [Bx[k]])
                self.to_xT(xt[k], Bx[k], i, pts[2 * k:2 * k + 2], Bp[2 * k:2 * k + 2])
            S.flush()

    def ln_setup(self, es, tag, g_ap, b_ap, lidx, router, nbuf=3):
        c = {}
        c["g"] = self.sb(es, tag + "_g", [128, D], F32)
        c["b"] = self.sb(es, tag + "_b", [128, D], F32)
        c["Bgb"] = Buf()
        self.DMA("sp", c["g"][:], self.bcast_rows(g_ap, D), (), [c["Bgb"]])
        self.DMA("sp", c["b"][:], self.bcast_rows(b_ap, D), (), [c["Bgb"]])
        for nm in ("xr", "v", "xo"):
            c[nm] = [self.sb(es, "%s_%s%d" % (tag, nm, k), [128, D], F32) for k in range(nbuf)]
            c["B" + nm] = [Buf() for _ in range(nbuf)]
        c["nbuf"] = nbuf
        c["st"] = [self.sb(es, "%s_st%d" % (tag, k), [128, 16], F32) for k in range(nbuf)]
        c["Bst"] = [Buf() for _ in range(nbuf)]
        c["router"] = router
        if router:
            c["rw"] = self.sb(es, tag + "_rw", [128, KC, 72], F32)
            c["rb"] = self.sb(es, tag + "_rbias", [1, 72], F32)
            c["Brw"] = Buf()
            self.DMA("sp", c["rw"][:, :, :], self.d["rw"][lidx].rearrange("(k p) c -> p k c", p=128), (), [c["Brw"]])
            self.DMA("sp", c["rb"][:, :], self.d["rb"][lidx:lidx + 1, :], (), [c["Brw"]])
            c["x32"] = [self.sb(es, "%s_x32%d" % (tag, k), [128, KC, 128], F32) for k in range(2)]
            c["Bx32"] = [Buf(), Buf()]
        return c

    def _ln_bufs(self, c, i):
        k3 = i % c["nbuf"]
        return (c["xr"][k3], c["v"][k3], c["xo"][k3], c["st"][k3], c["Bxr"][k3], c["Bv"][k3], c["Bxo"][k3], c["Bst"][k3])

    def ln_A(self, c, i, y_halves, y_bufs):
        d = self.d
        xr, v, xo, st, Bxr, Bv, Bxo, Bst = self._ln_bufs(c, i)
        self.DMA("sp", xr[:], d["xres"][i * 128:(i + 1) * 128, :], (), [Bxr])
        for h in range(2):
            self.STT("dve", v[:, h * 512:(h + 1) * 512], xr[:, h * 512:(h + 1) * 512], ALPHA, y_halves[h],
                     ALU.mult, ALU.add, [Bxr, y_bufs[h]], [Bv])
        for h in range(2):
            self.S.op("dve", (lambda hh, st=st, v=v: (lambda e: e.bn_stats(out=st[:, hh * 6:(hh + 1) * 6],
                                                                          in_=v[:, hh * 512:(hh + 1) * 512])))(h), [Bv], [Bst])
        self.S.op("dve", (lambda st: (lambda e: e.bn_aggr(out=st[:, 12:14], in_=st[:, 0:12])))(st), [Bst], [Bst])
        self.ACT(st[:, 14:15], st[:, 13:14], AF.Sqrt, [Bst, self.Bconst], [Bst], bias=self.eps_ln[:, 0:1], scale=1.0)
        self.S.op("dve", (lambda st: (lambda e: e.reciprocal(out=st[:, 14:15], in_=st[:, 14:15])))(st), [Bst], [Bst])
        self.STT("dve", st[:, 15:16], st[:, 12:13], -1.0, st[:, 14:15], ALU.mult, ALU.mult, [Bst], [Bst])

    def ln_B(self, c, i, dst_dram):
        xr, v, xo, st, Bxr, Bv, Bxo, Bst = self._ln_bufs(c, i)
        self.ACT(v[:, :], v[:, :], AF.Identity, [Bv, Bst], [Bv], scale=st[:, 14:15], bias=st[:, 15:16])
        self.TT("dve", v[:, :], v[:, :], c["g"][:, :], ALU.mult, [Bv, c["Bgb"]], [Bv])
        self.TT("dve", xo[:, :], v[:, :], c["b"][:, :], ALU.add, [Bv, c["Bgb"]], [Bxo])
        self.DMA("sp", dst_dram[i * 128:(i + 1) * 128, :], xo[:], [Bxo], ())
        if c["router"]:
            self.CP("act", self.xb[:, i, :], xo[:, :], [Bxo], [self.Bxb[i]] + self.BxT)

    def ln_C(self, c, i, pts, Bpts, prl, Bprl):
        xr, v, xo, st, Bxr, Bv, Bxo, Bst = self._ln_bufs(c, i)
        k = i % 2
        if not c["router"]:
            self.to_xT(xo, Bxo, i, pts, Bpts)
        else:
            x32, Bx32 = c["x32"][k], c["Bx32"][k]
            for half in range(2):
                pt, Bp = pts[half], Bpts[half]
                for q in range(4):
                    kc = half * 4 + q
                    self.TR(pt[:, q * 128:(q + 1) * 128], xo[:, kc * 128:(kc + 1) * 128], self.ident_f[:],
                            [Bxo, self.Bconst], [Bp])
                self.CP("act" if half else "dve", x32[:, half * 4:(half + 1) * 4, :],
                        pt[:, :].rearrange("p (q t) -> p q t", q=4), [Bp], [Bx32])

    def ln_D(self, c, i, prl, Bprl):
        if not c["router"]:
            return
        k = i % 2
        x32, Bx32 = c["x32"][k], c["Bx32"][k]
        for kc in range(KC):
            self.MM(prl[:, 0:72], x32[:, kc, :], c["rw"][:, kc, :], kc == 0, False, [Bx32, c["Brw"]], [Bprl])
        self.MM(prl[:, 0:72], self.ones_row[0:1, :], c["rb"][0:1, :], False, True, [self.Bconst, c["Brw"]], [Bprl])
        self.CP("act", self.lg[:, i, :], prl[:, 0:72], [Bprl], [self.Blg])

    def ln_run(self, c, tiles, yfn, dst, ptsfn):
        n = len(tiles)
        for s in range(n + 3):
            if s < n:
                yh, yb = yfn(tiles[s])
                self.ln_A(c, tiles[s], yh, yb)
            if 0 <= s - 1 < n:
                self.ln_B(c, tiles[s - 1], dst)
            if 0 <= s - 2 < n:
                pts, Bpts, prl, Bprl = ptsfn(tiles[s - 2])
                self.ln_C(c, tiles[s - 2], pts, Bpts, prl, Bprl)
            if 0 <= s - 3 < n:
                pts, Bpts, prl, Bprl = ptsfn(tiles[s - 3])
                self.ln_D(c, tiles[s - 3], prl, Bprl)

    def phase_conv(self, l, to_out):
        S = self.S
        d = self.d
        j = l // 2
        win = d["cwin%d" % j].rearrange("(k p) f -> p k f", p=128)
        with ExitStack() as es:
            c = self.ln_setup(es, "cv", d["ln1_g"][l], d["ln1_b"][l], l, router=not to_out)
            wb = [self.sb(es, "cv_w%d" % k, [128, 3, KC, 128], BF16) for k in range(2)]
            Bw = [Buf(), Buf()]
            wout = self.sb(es, "cv_wout", [128, KC, D], BF16)
            Bwout = Buf()
            zT = self.sb(es, "cv_zT", [128, KC, SEQ], BF16)
            Bz = [Buf() for _ in range(KC)]
            ub = self.sb(es, "cv_u", [128, SEQ + 2], F32)
            Bu = Buf()
            cs = [self.sb(es, "cv_cs%d" % k, [128, 512], F32) for k in range(2)]
            Bcs = [Buf(), Buf()]
            cvt = [self.sb(es, "cv_cv%d" % k, [128, 512], F32) for k in range(2)]
            Bcv = [Buf(), Buf()]
            ck = self.sb(es, "cv_ck", [128, 3, KC], F32)
            Bck = Buf()
            pp = [self.ps(es, "cv_ps%d" % k, [128, 512], F32) for k in range(8)]
            Bpp = [Buf() for _ in range(8)]
            self.DMA("sp", ck[:, :, :], d["conv_k"][j].rearrange("j (f p) -> p j f", p=128), (), [Bck],
                     allow_slow_non_contiguous=True)
            self.DMA("pool", wout[:, :, :], d["cwout%d" % j].rearrange("(k p) f -> p k f", p=128), (), [Bwout])
            self.MEMSET("dve", ub[:, 0:2], 0.0, [Bu])
            for f in range(KC):
                k = f % 2
                for g in range(3):
                    self.DMA("pool", wb[k][:, g, :, :], win[:, :, g * D + f * 128: g * D + (f + 1) * 128], (), [Bw[k]])
                for tc in range(4):
                    base = (tc % 2) * 3
                    pB, pC, pH = pp[base], pp[base + 1], pp[base + 2]
                    BpB, BpC, BpH = Bpp[base], Bpp[base + 1], Bpp[base + 2]
                    rT = [self.BxT[4 * tc + q] for q in range(4)]
                    for g, (po, Bpo) in enumerate(((pB, BpB), (pC, BpC), (pH, BpH))):
                        for kc in range(KC):
                            self.MM(po[:, :], wb[k][:, g, kc, :], self.xT[:, kc, tc * 512:(tc + 1) * 512],
                                    kc == 0, kc == KC - 1, [Bw[k]] + rT, [Bpo])
                    kk = tc % 2
                    self.CP("act", cs[kk][:, :], pC[:, :], [BpC], [Bcs[kk]])
                    self.TT("dve", ub[:, 2 + tc * 512: 2 + (tc + 1) * 512], pH[:, :], cs[kk][:, :], ALU.mult,
                            [BpH, Bcs[kk]], [Bu])
                    o = tc * 512
                    self.TS("dve", cvt[kk][:, :], ub[:, o:o + 512], ck[:, 0, f:f + 1], ALU.mult, [Bu, Bck], [Bcv[kk]])
                    self.STT("dve", cvt[kk][:, :], ub[:, o + 1:o + 513], ck[:, 1, f:f + 1], cvt[kk][:, :],
                             ALU.mult, ALU.add, [Bu, Bck, Bcv[kk]], [Bcv[kk]])
                    self.STT("dve", cvt[kk][:, :], ub[:, o + 2:o + 514], ck[:, 2, f:f + 1], cvt[kk][:, :],
                             ALU.mult, ALU.add, [Bu, Bck, Bcv[kk]], [Bcv[kk]])
                    self.TT("dve", zT[:, f, o:o + 512], pB[:, :], cvt[kk][:, :], ALU.mult, [BpB, Bcv[kk]], [Bz[f]])
            dst = d["out"] if to_out else d["xres"]
            py = [pp[6], pp[7]]
            Bpy = [Bpp[6], Bpp[7]]

            def yfn(i):
                for h in range(2):
                    for kc in range(KC):
                        self.MM(py[h][:, :], zT[:, kc, i * 128:(i + 1) * 128], wout[:, kc, h * 512:(h + 1) * 512],
                                kc == 0, kc == KC - 1, [Bz[kc], Bwout], [Bpy[h]])
                return [py[0][:, :], py[1][:, :]], Bpy

            def ptsfn(i):
                k = i % 2
                return [pp[0 + 2 * k], pp[1 + 2 * k]], [Bpp[0 + 2 * k], Bpp[1 + 2 * k]], pp[4 + k], Bpp[4 + k]
            self.ln_run(c, list(range(NT)), yfn, dst, ptsfn)
            S.flush()

    def phase_moe(self, l, to_out):
        S = self.S
        d = self.d
        with ExitStack() as es:
            cur_es = [es]
            sm = lambda name, shape, dt=F32: self.sb(cur_es[0], "mo_" + name, shape, dt)
            gates = sm("gates", [128, NT, 2]); desti = sm("desti", [128, NT, 2], I32); widx = sm("widx", [128, NBLK], I32)
            esA = ExitStack()
            cur_es[0] = esA
            lg = self.lg
            Blg = self.Blg
            lgg = lg[:, :, 0:8]
            lge = lg[:, :, 8:72].rearrange("p i (g e) -> p i g e", g=8)
            gmax = sm("gmax", [128, NT]); goh = sm("goh", [128, NT, 8]); gsh = sm("gsh", [128, NT, 8])
            gsum = sm("gsum", [128, NT]); ggate = sm("ggate", [128, NT])
            tmp4 = sm("tmp4", [128, NT, 8, 8]); sel = sm("sel", [128, NT, 8]); sel2 = sm("sel2", [128, NT, 8])
            v1 = sm("v1", [128, NT]); v2 = sm("v2", [128, NT]); oh1 = sm("oh1", [128, NT, 8]); oh2 = sm("oh2", [128, NT, 8])
            e2 = sm("e2", [128, NT]); p1 = sm("p1", [128, NT])
            A1 = sm("A1", [128, NT, 64]); A2 = sm("A2", [128, NT, 64]); Ab = sm("Ab", [128, NT, 64], BF16)
            B = Buf("route")
            bc8 = lambda ap: ap.unsqueeze(2).to_broadcast([128, NT, 8])
            self.RED(gmax[:, :], lgg, ALU.max, [Blg], [B])
            self.TT("dve", goh[:, :, :], lgg, bc8(gmax[:, :]), ALU.is_equal, [Blg, B], [B])
            self.TT("dve", gsh[:, :, :], lgg, bc8(gmax[:, :]), ALU.subtract, [Blg, B], [B])
            self.ACT(gsh[:, :, :], gsh[:, :, :], AF.Exp, [B], [B])
            self.RED(gsum[:, :], gsh[:, :, :], ALU.add, [B], [B])
            S.op("dve", lambda e: e.reciprocal(out=ggate[:, :], in_=gsum[:, :]), [B], [B])
            self.TT("dve", tmp4[:, :, :, :], lge, goh[:, :, :].unsqueeze(3).to_broadcast([128, NT, 8, 8]), ALU.mult,
                    [Blg, B], [B])
            self.RED(sel[:, :, :], tmp4[:, :, :, :].rearrange("p i g e -> p i e g"), ALU.add, [B], [B])
            self.RED(v1[:, :], sel[:, :, :], ALU.max, [B], [B])
            self.TT("dve", oh1[:, :, :], sel[:, :, :], bc8(v1[:, :]), ALU.is_equal, [B], [B])
            self.STT("dve", sel2[:, :, :], oh1[:, :, :], NEG, sel[:, :, :], ALU.mult, ALU.add, [B], [B])
            self.RED(v2[:, :], sel2[:, :, :], ALU.max, [B], [B])
            self.TT("dve", oh2[:, :, :], sel2[:, :, :], bc8(v2[:, :]), ALU.is_equal, [B], [B])
            self.TT("dve", e2[:, :], v2[:, :], v1[:, :], ALU.subtract, [B], [B])
            self.ACT(e2[:, :], e2[:, :], AF.Exp, [B], [B])
            self.TS("dve", p1[:, :], e2[:, :], 1.0, ALU.add, [B], [B])
            S.op("dve", lambda e: e.reciprocal(out=p1[:, :], in_=p1[:, :]), [B], [B])
            self.TT("dve", gates[:, :, 0], p1[:, :], ggate[:, :], ALU.mult, [B], [B])
            self.TT("dve", e2[:, :], e2[:, :], p1[:, :], ALU.mult, [B], [B])
            self.TT("dve", gates[:, :, 1], e2[:, :], ggate[:, :], ALU.mult, [B], [B])
            gb = goh[:, :, :].unsqueeze(3).to_broadcast([128, NT, 8, 8])
            for Ak, ohk in ((A1, oh1), (A2, oh2)):
                self.TT("dve", Ak[:, :, :].rearrange("p i (g e) -> p i g e", g=8), gb,
                        ohk[:, :, :].unsqueeze(2).to_broadcast([128, NT, 8, 8]), ALU.mult, [B], [B])
            self.TT("dve", Ab[:, :, :], A1[:, :, :], A2[:, :, :], ALU.add, [B], [B])
            pp = [self.ps(es, "mo_pp%d" % k, [128, 512], F32) for k in range(6)]
            Bpp = [Buf() for _ in range(6)]
            pcs, ppf = pp[0:2], pp[2:4]
            Bpcs, Bppf = Bpp[0:2], Bpp[2:4]
            Abf = Ab[:, :, :].rearrange("p i e -> p (i e)")
            for h in range(2):
                self.MM(pcs[h][:, :], self.ones_b[:, :], Abf[:, h * 512:(h + 1) * 512], True, True, [B, self.Bconst], [Bpcs[h]])
                self.MM(ppf[h][:, :], self.tri_b[:, :], Abf[:, h * 512:(h + 1) * 512], True, True, [B, self.Bconst], [Bppf[h]])
            cs = sm("cs", [128, NT, 64]); base = sm("base", [128, NT, 64]); carry = sm("carry", [128, NT, 64])
            csf = cs[:, :, :].rearrange("p i e -> p (i e)")
            bsf = base[:, :, :].rearrange("p i e -> p (i e)")
            for h in range(2):
                self.CP("dve", csf[:, h * 512:(h + 1) * 512], pcs[h][:, :], [Bpcs[h]], [B])
                self.CP("act", bsf[:, h * 512:(h + 1) * 512], ppf[h][:, :], [Bppf[h]], [B])
            self.MEMSET("dve", carry[:, 0, :], 0.0, [B])
            for i in range(1, NT):
                self.TT("dve", carry[:, i, :], carry[:, i - 1, :], cs[:, i - 1, :], ALU.add, [B], [B])
            cnt = sm("cnt", [128, 64]); cnti = sm("cnti", [128, 64], I32); padf = sm("padf", [128, 64])
            sc = [sm("scan%d" % k, [128, 64]) for k in range(2)]
            self.TT("dve", cnt[:, :], carry[:, NT - 1, :], cs[:, NT - 1, :], ALU.add, [B], [B])
            self.TS("dve", cnti[:, :], cnt[:, :], 127.0, ALU.add, [B], [B])
            self.TS("dve", cnti[:, :], cnti[:, :], 7, ALU.arith_shift_right, [B], [B])
            self.TS("dve", cnti[:, :], cnti[:, :], 7, ALU.logical_shift_left, [B], [B])
            self.CP("dve", padf[:, :], cnti[:, :], [B], [B])
            self.CP("dve", sc[0][:, :], padf[:, :], [B], [B])
            cur = 0
            s = 1
            while s < 64:
                a, b = sc[cur], sc[1 - cur]
                self.CP("dve", b[:, 0:s], a[:, 0:s], [B], [B])
                self.TT("dve", b[:, s:64], a[:, s:64], a[:, 0:64 - s], ALU.add, [B], [B])
                cur = 1 - cur
                s *= 2
            pend = sc[cur]
            pstart = sm("pstart", [128, 64])
            self.TT("dve", pstart[:, :], pend[:, :], padf[:, :], ALU.subtract, [B], [B])
            self.TT("dve", base[:, :, :], base[:, :, :], carry[:, :, :], ALU.add, [B], [B])
            self.TT("dve", base[:, :, :], base[:, :, :], pstart[:, :].unsqueeze(1).to_broadcast([128, NT, 64]), ALU.add,
                    [B], [B])
            destf = sm("destf", [128, NT, 2])
            for kk, Ak in enumerate((A1, A2)):
                self.TT("dve", cs[:, :, :], Ak[:, :, :], base[:, :, :], ALU.mult, [B], [B])
                self.RED(destf[:, :, kk], cs[:, :, :], ALU.add, [B], [B])
            Bdest = Buf("dest")
            self.CP("dve", desti[:, :, :], destf[:, :, :], [B], [Bdest])
            cmp = sm("cmp", [128, NBLK, 64], BF16); blke = sm("blke", [128, NBLK])
            self.TT("dve", cmp[:, :, :], pend[:, :].unsqueeze(1).to_broadcast([128, NBLK, 64]),
                    self.bvals[:, :].unsqueeze(2).to_broadcast([128, NBLK, 64]), ALU.is_le, [B, self.Bconst], [B])
            self.RED(blke[:, :], cmp[:, :, :], ALU.add, [B], [B])
            self.TS("dve", blke[:, :], blke[:, :], 128.0, ALU.mult, [B], [B])
            Bwidx = Buf("widx")
            self.TS("dve", widx[:, :], blke[:, :], self.pidx[:, 0:1], ALU.add, [B, self.Bconst], [Bwidx])
            Bxs = Buf("xs")
            for i in range(NT):
                for kk in range(2):
                    self.SCATTER(d["xs"], desti[:, i, kk:kk + 1], self.xb[:, i, :], [Bdest, self.Bxb[i]], [Bxs])
            S.flush()
            esA.close()
            esB = ExitStack()
            cur_es[0] = esB
            NB = 5
            xsb = [sm("xsb%d" % k, [128, D], BF16) for k in range(5)]; Bxsb = [Buf() for _ in range(5)]
            xsT = [sm("xsT%d" % k, [128, KC, 128], BF16) for k in range(2)]; BxsT = [Buf(), Buf()]
            w1b = [sm("w1b%d" % k, [128, KC, 256], BF16) for k in range(NB)]; Bw1 = [Buf() for _ in range(NB)]
            w3b = [sm("w3b%d" % k, [128, KC, 256], BF16) for k in range(NB)]; Bw3 = [Buf() for _ in range(NB)]
            w2b = [sm("w2b%d" % k, [128, 2, D], BF16) for k in range(NB)]; Bw2 = [Buf() for _ in range(NB)]
            sa = [sm("sa%d" % k, [128, 256]) for k in range(2)]; Bsa = [Buf(), Buf()]
            hT = [sm("hT%d" % k, [128, 2, 128], BF16) for k in range(2)]; BhT = [Buf(), Buf()]
            ysb = [sm("ysb%d" % k, [128, D]) for k in range(2)]; Bysb = [Buf(), Buf()]
            ptr = [self.ps(es, "mo_ptr%d" % k, [128, 1024], BF16) for k in range(2)]; Bptr = [Buf(), Buf()]
            ph = pp[0:2]; Bph = Bpp[0:2]
            pys = pp[2:4]; Bpys = Bpp[2:4]
            Bys = Buf("ys")
            w1t, w3t, w2t = d["w1r%d" % l], d["w3r%d" % l], d["w2r%d" % l]
            PF = 4
            S.streams["pool"].append(("raw", lambda e: setattr(self, "bound_reg", e.to_reg(64 * 128 - 1))))

            def issue_loads(b):
                kx = b % 5
                kw = b % NB
                self.DMA("act", xsb[kx][:, :], d["xs"][b * 128:(b + 1) * 128, :], [Bxs], [Bxsb[kx]])
                self.GATHER(w1b[kw][:, :, :].rearrange("p k f -> p (k f)"), w1t, widx[:, b:b + 1], [Bwidx], [Bw1[kw]], bound=64 * 128 - 1)
                self.GATHER(w3b[kw][:, :, :].rearrange("p k f -> p (k f)"), w3t, widx[:, b:b + 1], [Bwidx], [Bw3[kw]], bound=64 * 128 - 1)
                self.GATHER(w2b[kw][:, :, :].rearrange("p k f -> p (k f)"), w2t, widx[:, b:b + 1], [Bwidx], [Bw2[kw]], bound=64 * 128 - 1)
            for b in range(PF):
                issue_loads(b)
            for b in range(NBLK):
                k = b % 2
                kw = b % NB
                kx = b % 5
                if b + PF < NBLK:
                    issue_loads(b + PF)
                for kc in range(KC):
                    self.TR(ptr[k][:, kc * 128:(kc + 1) * 128], xsb[kx][:, kc * 128:(kc + 1) * 128], self.ident_b[:],
                            [Bxsb[kx], self.Bconst], [Bptr[k]])
                self.CP("dve", xsT[k][:, :, :].rearrange("p k r -> p (k r)"), ptr[k][:, :], [Bptr[k]], [BxsT[k]])
                for gi, (wt, Bwt) in enumerate(((w1b[kw], Bw1[kw]), (w3b[kw], Bw3[kw]))):
                    for fc in range(2):
                        col = (gi * 2 + fc) * 128
                        for kc in range(KC):
                            self.MM(ph[k][:, col:col + 128], wt[:, kc, fc * 128:(fc + 1) * 128], xsT[k][:, kc, :],
                                    kc == 0, kc == KC - 1, [Bwt, BxsT[k]], [Bph[k]])
                self.ACT(sa[k][:, :], ph[k][:, 0:256], AF.Silu, [Bph[k]], [Bsa[k]])
                self.TT("dve", hT[k][:, :, :].rearrange("p c r -> p (c r)"), sa[k][:, :], ph[k][:, 256:512], ALU.mult,
                        [Bsa[k], Bph[k]], [BhT[k]])
                for h in range(2):
                    py = pys[h]
                    for fc in range(2):
                        self.MM(py[:, :], hT[k][:, fc, :], w2b[kw][:, fc, h * 512:(h + 1) * 512], fc == 0, fc == 1,
                                [BhT[k], Bw2[kw]], [Bpys[h]])
                    self.CP("act" if h == 0 else "dve", ysb[k][:, h * 512:(h + 1) * 512], py[:, :],
                            [Bpys[h]], [Bysb[k]])
                self.DMA("sp", d["ys"][b * 128:(b + 1) * 128, :], ysb[k][:, :], [Bysb[k]], [Bys])
            esC = ExitStack()
            cur_es[0] = esC
            c = self.ln_setup(esC, "m2", d["ln2_g"][l], d["ln2_b"][l], l, router=False)
            y0 = [sm("y0_%d" % k, [128, D]) for k in range(2)]; By0 = [Buf(), Buf()]
            y1 = [sm("y1_%d" % k, [128, D]) for k in range(2)]; By1 = [Buf(), Buf()]
            dst = d["out"] if to_out else d["xres"]
            def issue_gather(i):
                k = i % 2
                self.GATHER(y0[k][:, :], d["ys"], desti[:, i, 0:1], [Bdest, Bys], [By0[k]])
                self.GATHER(y1[k][:, :], d["ys"], desti[:, i, 1:2], [Bdest, Bys], [By1[k]])
            issue_gather(0)

            def yfn(i):
                k = i % 2
                self.TS("dve", y0[k][:, :], y0[k][:, :], gates[:, i, 0:1], ALU.mult, [By0[k], B], [By0[k]])
                self.STT("dve", y0[k][:, :], y1[k][:, :], gates[:, i, 1:2], y0[k][:, :], ALU.mult, ALU.add,
                         [By1[k], By0[k], B], [By0[k]])
                if i + 1 < NT:
                    issue_gather(i + 1)
                return [y0[k][:, 0:512], y0[k][:, 512:1024]], [By0[k], By0[k]]

            def ptsfn(i):
                k = i % 2
                return [pp[2 * k], pp[2 * k + 1]], [Bpp[2 * k], Bpp[2 * k + 1]], None, None
            self.ln_run(c, list(range(NT)), yfn, dst, ptsfn)
            S.flush()
            esC.close()
            esB.close()

    def phase_attn(self, l, to_out):
        S = self.S
        d = self.d
        j = l // 2
        awin = d["awin%d" % j].rearrange("(k p) f -> p k f", p=128)
        import os
        att_stop = int(os.environ.get("ATT_STOP", "9"))
        if att_stop <= 0:
            return
        qd = d["qT"]
        with ExitStack() as es0:
            sm0 = lambda name, shape, dt=F32: self.sb(es0, "at_" + name, shape, dt)
            ckv_tm = sm0("ckv_tm", [128, NT, 128], BF16); Bckv = Buf("ckv_tm")
            ckvT = sm0("ckvT", [128, SEQ], BF16); BckvT = Buf("ckvT")
            with ExitStack() as es:
                sm = lambda name, shape, dt=F32: self.sb(es, "a1_" + name, shape, dt)
                pp = [self.ps(es, "a1_pp%d" % k, [128, 512], F32) for k in range(6)]
                Bpp = [Buf() for _ in range(6)]
                ptb = [self.ps(es, "a1_ptb%d" % k, [128, 1024], BF16) for k in range(2)]
                Bptb = [Buf(), Buf()]
                kiT = sm("kiT", [128, SEQ], BF16); BkiT = Buf("kiT")
                qiT = sm("qiT", [64, NIH, SEQ], BF16); BqiT = [Buf() for _ in range(NIH)]
                wi = sm("wi", [128, NT, 8]); Bwi = Buf("wi")
                wckv = sm("wckv", [128, KC, 128], BF16); wki = sm("wki", [128, KC, 72], BF16); Bwa = Buf()
                kvg = sm("kvg", [128, 128]); kig = sm("kig", [128, 64]); kib = sm("kib", [128, 64]); Bg = Buf()
                self.DMA("pool", wckv[:, :, :], awin[:, :, 2048:2176], (), [Bwa])
                self.DMA("pool", wki[:, :, :], awin[:, :, 2688:2760], (), [Bwa])
                self.DMA("sp", kvg[:, :], self.bcast_rows(d["kv_g"][j], 128), (), [Bg])
                self.DMA("sp", kig[:, :], self.bcast_rows(d["ki_g"][j], 64), (), [Bg])
                self.DMA("sp", kib[:, :], self.bcast_rows(d["ki_b"][j], 64), (), [Bg])
                st1 = [sm("st%d" % k, [128, 24]) for k in range(2)]; Bst1 = [Buf(), Buf()]
                junk = [sm("junk%d" % k, [128, 128]) for k in range(2)]
                kin = [sm("kin%d" % k, [128, 128], BF16) for k in range(2)]; Bkin = [Buf(), Buf()]
                ktmp = [sm("ktmp%d" % k, [128, 64]) for k in range(2)]
                bis = int(os.environ.get("BIS", "9"))
                def ck_A(i):
                    k = i % 2
                    pa, Bpa = pp[k], Bpp[k]
                    for kc in range(KC):
                        self.MM(pa[:, 0:128], self.xT[:, kc, i * 128:(i + 1) * 128], wckv[:, kc, :], kc == 0, kc == KC - 1,
                                [self.BxT[i], Bwa], [Bpa])
                    for kc in range(KC):
                        self.MM(pa[:, 128:200], self.xT[:, kc, i * 128:(i + 1) * 128], wki[:, kc, :], kc == 0, kc == KC - 1,
                                [self.BxT[i], Bwa], [Bpa])

                def ck_B(i):
                    k = i % 2
                    pa, Bpa = pp[k], Bpp[k]
                    st, Bs = st1[k], Bst1[k]
                    self.ACT(junk[k][:, :], pa[:, 0:128], AF.Square, [Bpa], [Bs], accum=st[:, 0:1])
                    self.ACT(st[:, 1:2], st[:, 0:1], AF.Sqrt, [Bs, self.Bconst], [Bs], scale=1.0 / 128.0, bias=self.eps_rms[:, 0:1])
                    S.op("dve", (lambda s_: (lambda e: e.reciprocal(out=s_[:, 1:2], in_=s_[:, 1:2])))(st), [Bs], [Bs])
                    self.STT("dve", ckv_tm[:, i, :], pa[:, 0:128], st[:, 1:2], kvg[:, :], ALU.mult, ALU.mult,
                             [Bpa, Bs, Bg], [Bckv])
                    S.op("dve", (lambda s_, p_: (lambda e: e.bn_stats(out=s_[:, 8:14], in_=p_[:, 128:192])))(st, pa), [Bpa], [Bs])
                    S.op("dve", (lambda s_: (lambda e: e.bn_aggr(out=s_[:, 14:16], in_=s_[:, 8:14])))(st), [Bs], [Bs])
                    self.ACT(st[:, 16:17], st[:, 15:16], AF.Sqrt, [Bs, self.Bconst], [Bs], scale=1.0, bias=self.eps_ln[:, 0:1])
                    S.op("dve", (lambda s_: (lambda e: e.reciprocal(out=s_[:, 16:17], in_=s_[:, 16:17])))(st), [Bs], [Bs])
                    self.TS("dve", ktmp[k][:, :], pa[:, 128:192], st[:, 14:15], ALU.subtract, [Bpa, Bs], [Bkin[k]],
                            s2=st[:, 16:17], op1=ALU.mult)
                    self.TT("dve", ktmp[k][:, :], ktmp[k][:, :], kig[:, :], ALU.mult, [Bkin[k], Bg], [Bkin[k]])
                    self.TT("dve", kin[k][:, 0:64], ktmp[k][:, :], kib[:, :], ALU.add, [Bkin[k], Bg], [Bkin[k]])
                    self.TT("dve", kin[k][:, 64:128], ktmp[k][:, :], kib[:, :], ALU.add, [Bkin[k], Bg], [Bkin[k]])
                    self.CP("act", wi[:, i, :], pa[:, 192:200], [Bpa], [Bwi])

                def ck_C(i):
                    k = i % 2
                    self.TR(ptb[k][:, 0:128], ckv_tm[:, i, :], self.ident_b[:], [Bckv, self.Bconst], [Bptb[k]])
                    self.TR(ptb[k][:, 128:256], kin[k][:, :], self.ident_b[:], [Bkin[k], self.Bconst], [Bptb[k]])
                    self.CP("dve", ckvT[:, i * 128:(i + 1) * 128], ptb[k][:, 0:128], [Bptb[k]], [BckvT])
                    self.CP("dve", kiT[:, i * 128:(i + 1) * 128], ptb[k][:, 128:256], [Bptb[k]], [BkiT])
                for s in range(NT + 2):
                    if s < NT:
                        ck_A(s)
                    if 0 <= s - 1 < NT:
                        ck_B(s - 1)
                    if 0 <= s - 2 < NT:
                        ck_C(s - 2)
                if att_stop <= 1:
                    S.flush()
                    return
                wq = [sm("wq%d" % k, [128, KC, 128], BF16) for k in range(2)]; Bwq = [Buf(), Buf()]
                qs = [sm("qs%d" % k, [128, 512], BF16) for k in range(4)]; Bqs = [Buf() for _ in range(4)]
                Bqd = Buf("qd")
                n = 0
                for h in range(NH):
                    k = h % 2
                    self.DMA("pool", wq[k][:, :, :], awin[:, :, h * 128:(h + 1) * 128], (), [Bwq[k]])
                    for tc in range(4):
                        po, Bpo = pp[2 + n % 4], Bpp[2 + n % 4]
                        for kc in range(KC):
                            self.MM(po[:, :], wq[k][:, kc, :], self.xT[:, kc, tc * 512:(tc + 1) * 512], kc == 0, kc == KC - 1,
                                    [Bwq[k]] + self.BxT[4 * tc:4 * tc + 4], [Bpo])
                        if n % 2:
                            self.ACT(qs[n % 4][:, :], po[:, :], AF.Copy, [Bpo], [Bqs[n % 4]], scale=128.0 ** -0.5)
                        else:
                            self.TS("dve", qs[n % 4][:, :], po[:, :], 128.0 ** -0.5, ALU.mult, [Bpo], [Bqs[n % 4]])
                        self.DMA("sp", qd[h, :, tc * 512:(tc + 1) * 512], qs[n % 4][:, :], [Bqs[n % 4]], [Bqd])
                        n += 1
                wqi = [sm("wqi%d" % k, [128, KC, 64], BF16) for k in range(2)]; Bwqi = [Buf(), Buf()]
                for h in range(NIH):
                    k = h % 2
                    self.DMA("pool", wqi[k][:, :, :], awin[:, :, 2176 + h * 64:2176 + (h + 1) * 64], (), [Bwqi[k]])
                    for tc in range(4):
                        po, Bpo = pp[2 + n % 4], Bpp[2 + n % 4]
                        for kc in range(KC):
                            self.MM(po[0:64, :], wqi[k][:, kc, :], self.xT[:, kc, tc * 512:(tc + 1) * 512], kc == 0, kc == KC - 1,
                                    [Bwqi[k]] + self.BxT[4 * tc:4 * tc + 4], [Bpo])
                        self.CP("act" if n % 2 else "dve", qiT[:, h, tc * 512:(tc + 1) * 512], po[0:64, :], [Bpo], [BqiT[h]])
                        n += 1
                if att_stop <= 2:
                    S.flush()
                    return
                U32 = mybir.dt.uint32
                NIT = 18
                sc4s = [sm("sc4_%d" % k, [128, 4, SEQ]) for k in range(2)]
                Bscs = [[Buf() for _ in range(4)] for _ in range(2)]
                junkb = sm("junkb", [128, SEQ], BF16)
                junka = junkb
                lhs_ = [sm("lohi%d" % k, [128, 5, 4]) for k in range(2)]; Blhs = [Buf("lohi0"), Buf("lohi1")]
                Bcas = [Buf("cnta0"), Buf("cnta1")]; Bcds = [Buf("cntd0"), Buf("cntd1")]; Btvs = [Buf("tv0"), Buf("tv1")]
                tvs = [sm("tv%d" % k, [128, 4]) for k in range(2)]
                pm = sm("predm", [128, 2, 4], U32)
                NRL = 12
                rl = [sm("rl%d" % k, [128, 512]) for k in range(NRL)]; Brl = [Buf() for _ in range(NRL)]
                maskb = [sm("maskb%d" % k, [128, SEQ], BF16) for k in range(1)] * 2; Bmk = [Buf()] * 2
                mT = [sm("mT%d" % k, [128, NT, 128], BF16) for k in range(1)] * 2; BmT = [Buf()] * 2
                Bmd = Buf("maskT_d")
                self.Bmd = Bmd
                cnt_ps = [0]

                def score_steps(g):
                    sc4, Bsc, lh, Blh = sc4s[g % 2], Bscs[g % 2], lhs_[g % 2], Blhs[g % 2]
                    steps = []
                    for q in range(4):
                        qb = 4 * g + q
                        L = (qb + 1) * 128
                        for c0 in range(0, L, 512):
                            nn = min(512, L - c0)
                            for h in range(NIH):
                                def mk_step(q=q, qb=qb, c0=c0, nn=nn, h=h):
                                    stt = {}

                                    def p1():
                                        i_ = cnt_ps[0]
                                        cnt_ps[0] += 1
                                        pS, BpS = pp[i_ % 2], Bpp[i_ % 2]
                                        r_, Br_ = rl[i_ % NRL], Brl[i_ % NRL]
                                        stt["r"] = (r_, Br_)
                                        self.MM(pS[:, 0:nn], qiT[:, h, qb * 128:(qb + 1) * 128], kiT[0:64, c0:c0 + nn], True, True,
                                                [BqiT[h], BkiT], [BpS])
                                        self.ACT(r_[:, 0:nn], pS[:, 0:nn], AF.Relu, [BpS], [Br_])

                                    def p2():
                                        r_, Br_ = stt["r"]
                                        sc_ = sc4[:, q, :]
                                        Bs_ = Bsc[q]
                                        if h == 0:
                                            self.TS("dve", sc_[:, c0:c0 + nn], r_[:, 0:nn], wi[:, qb, 0:1], ALU.mult, [Br_, Bwi], [Bs_])
                                        else:
                                            self.STT("dve", sc_[:, c0:c0 + nn], r_[:, 0:nn], wi[:, qb, h:h + 1], sc_[:, c0:c0 + nn],
                                                     ALU.mult, ALU.add, [Br_, Bwi, Bs_], [Bs_])
                                    return p1, p2
                                steps.append(mk_step())

                        def post(q=q, qb=qb, L=L):
                            sc_ = sc4[:, q, :]
                            Bs_ = Bsc[q]
                            if qb >= 2:
                                self.RED(lh[:, 0, q:q + 1], sc_[:, 0:L], ALU.min, [Bs_], [Blh])
                            dg = sc_[:, qb * 128:(qb + 1) * 128]
                            S.op("pool", (lambda a: (lambda e: e.affine_select(out=a, in_=a, pattern=[[-1, 128]], compare_op=ALU.is_ge,
                                                                              fill=NEG, base=0, channel_multiplier=1)))(dg), [Bs_], [Bs_])
                            if qb >= 2:
                                self.RED(lh[:, 1, q:q + 1], sc_[:, 0:L], ALU.max, [Bs_], [Blh])
                                Lq = L
                                act_q = (q >= (2 if g == 3 else 0)) if g > 0 else (q == 3)
                                self.MEMSET("pool", tvs[g % 2][:, q:q + 1], float(Lq - 2 * TOPK) if act_q else float(Lq - TOPK), [Btvs[g % 2]])
                        steps.append((lambda: None, post))
                    return steps

                def bisect_first(g):
                    sc4, Bsc, lh, Blh, tv = sc4s[g % 2], Bscs[g % 2], lhs_[g % 2], Blhs[g % 2], tvs[g % 2]
                    Bca, Bcd = Bcas[g % 2], Bcds[g % 2]
                    qs_need = [q for q in range(4) if 4 * g + q >= 2]
                    q0 = qs_need[0]
                    act_qs = [q for q in qs_need if ((q >= (2 if g == 3 else 0)) if g > 0 else (q == 3))]
                    self.TT("dve", lh[:, 2, q0:4], lh[:, 0, q0:4], lh[:, 1, q0:4], ALU.add, [Blh], [Blh])
                    self.TS("dve", lh[:, 2, q0:4], lh[:, 2, q0:4], 0.5, ALU.mult, [Blh], [Blh])
                    for q in qs_need:
                        L = (4 * g + q + 1) * 128
                        if q in act_qs:
                            self.ACT(junka[:, 0:L], sc4[:, q, 0:L], AF.Sign, [Bsc[q], Blh], [Bca], bias=lh[:, 2, q:q + 1],
                                     scale=-1.0, accum=lh[:, 3, q:q + 1])
                    for q in qs_need:
                        L = (4 * g + q + 1) * 128
                        if q not in act_qs:
                            self.TS("dve", junkb[:, 0:L], sc4[:, q, 0:L], lh[:, 2, q:q + 1], ALU.is_lt, [Bsc[q], Blh], [Bcd],
                                    op1=ALU.add, accum=lh[:, 3, q:q + 1])

                def bisect_second(g):
                    lh, Blh, tv = lhs_[g % 2], Blhs[g % 2], tvs[g % 2]
                    Bca, Bcd, Btv = Bcas[g % 2], Bcds[g % 2], Btvs[g % 2]
                    qs_need = [q for q in range(4) if 4 * g + q >= 2]
                    q0 = qs_need[0]
                    self.TT("dve", pm[:, 0, q0:4], lh[:, 3, q0:4], tv[:, q0:4], ALU.is_le, [Bca, Bcd, Btv], [Blh])
                    self.TT("dve", pm[:, 1, q0:4], lh[:, 3, q0:4], tv[:, q0:4], ALU.is_gt, [Bca, Bcd, Btv], [Blh])
                    S.op("dve", (lambda a, l_: (lambda e: e.copy_predicated(out=l_[:, 0, a:4], mask=pm[:, 0, a:4], data=l_[:, 2, a:4])))(q0, lh),
                         [Blh], [Blh])
                    S.op("dve", (lambda a, l_: (lambda e: e.copy_predicated(out=l_[:, 1, a:4], mask=pm[:, 1, a:4], data=l_[:, 2, a:4])))(q0, lh),
                         [Blh], [Blh])

                def emit_masks(g):
                    sc4, Bsc, lh, Blh = sc4s[g % 2], Bscs[g % 2], lhs_[g % 2], Blhs[g % 2]
                    for q in range(4):
                        qb = 4 * g + q
                        k = qb % 2
                        L = (qb + 1) * 128
                        if qb >= 2:
                            self.TS("dve", maskb[k][:, 0:L], sc4[:, q, 0:L], lh[:, 0, q:q + 1], ALU.is_lt, [Bsc[q], Blh], [Bmk[k]],
                                    s2=MASK_NEG, op1=ALU.mult)
                        else:
                            self.TS("dve", maskb[k][:, 0:L], sc4[:, q, 0:L], -1.0e29, ALU.is_lt, [Bsc[q]], [Bmk[k]],
                                    s2=MASK_NEG, op1=ALU.mult)
                        for g0 in range(0, qb + 1, 8):
                            gn = min(8, qb + 1 - g0)
                            pt, Bpt = ptb[(g0 // 8) % 2], Bptb[(g0 // 8) % 2]
                            for qq in range(gn):
                                st_ = g0 + qq
                                self.TR(pt[:, qq * 128:(qq + 1) * 128], maskb[k][:, st_ * 128:(st_ + 1) * 128], self.ident_b[:],
                                        [Bmk[k], self.Bconst], [Bpt])
                            self.CP("dve", mT[k][:, g0:g0 + gn, :].rearrange("p s t -> p (s t)"), pt[:, 0:gn * 128], [Bpt], [BmT[k]])
                        self.DMA("sp", d["maskT"][0:L, qb * 128:(qb + 1) * 128].rearrange("(s p) t -> p s t", p=128),
                                 mT[k][:, 0:qb + 1, :], [BmT[k]], [Bmd])

                for p1, p2 in score_steps(0):
                    p1()
                    p2()
                for g in range(4):
                    nxt = score_steps(g + 1) if g < 3 else []
                    per = (len(nxt) + NIT - 1) // NIT if nxt else 0
                    batches = [nxt[it * per:(it + 1) * per] for it in range(NIT)]
                    for p1, _ in batches[0]:
                        p1()
                    for it in range(NIT):
                        bisect_first(g)
                        nb_ = batches[it + 1] if it + 1 < NIT else []
                        hb_ = len(nb_) // 2
                        for p1, _ in nb_[:hb_]:
                            p1()
                        for _, p2 in batches[it]:
                            p2()
                        for p1, _ in nb_[hb_:]:
                            p1()
                        bisect_second(g)
                    emit_masks(g)
                S.flush()
            if att_stop <= 3:
                return
            with ExitStack() as es:
                sm = lambda name, shape, dt=F32: self.sb(es, "a3_" + name, shape, dt)
                pp = [self.ps(es, "a3_pp%d" % k, [128, 512], F32) for k in range(8)]
                Bpp = [Buf() for _ in range(8)]
                c = self.ln_setup(es, "a3", d["ln1_g"][l], d["ln1_b"][l], l, router=not to_out, nbuf=2)
                wout = sm("wout", [128, NH, D], BF16); Bwout = Buf()
                self.DMA("pool", wout[:, :, :], d["awout%d" % j].rearrange("(h c) f -> c h f", c=128), (), [Bwout])
                qtc = [sm("qtc%d" % k, [128, NH, 512], BF16) for k in range(2)]; Bqtc = [Buf(), Buf()]
                mk = sm("mk", [128, NT, 512], BF16); Bmk3 = Buf()
                oT = sm("oT", [128, NH, 512], BF16); BoT = [Buf() for _ in range(NH)]
                pT = [sm("pT%d" % k, [128, 512], BF16) for k in range(3)]; BpT = [Buf() for _ in range(3)]
                rec = sm("rec", [128, 512]); Brec = Buf()
                dst = d["out"] if to_out else d["xres"]
                npt = 0
                npl = 0
                for tc in range(4):
                    kq = tc % 2
                    nst = 4 * (tc + 1)
                    self.DMA("sp", qtc[kq][:, :, :], qd[:, :, tc * 512:(tc + 1) * 512].rearrange("h c t -> c h t"),
                             [Bqd], [Bqtc[kq]])
                    self.DMA("sp", mk[:, 0:nst, :],
                             d["maskT"][0:nst * 128, tc * 512:(tc + 1) * 512].rearrange("(s p) t -> p s t", p=128),
                             [Bmd], [Bmk3])
                    for h in range(NH):
                        po, Bpo = pp[2 + h % 2], Bpp[2 + h % 2]
                        pd, Bpd = pp[4 + h % 2], Bpp[4 + h % 2]

                        def emit_qk(st_, h=h):
                            nonlocal npl
                            c0 = max(0, st_ - 4 * tc) * 128
                            pl, Bpl = pp[npl % 2], Bpp[npl % 2]
                            npl += 1
                            extra = []
                            for dl in (0, 1):
                                tt = st_ + dl
                                if 4 * tc <= tt <= 4 * tc + 3:
                                    extra.append((dl, (tt - 4 * tc) * 128))
                            self.MM(pl[:, c0:512], ckvT[:, st_ * 128:(st_ + 1) * 128], qtc[kq][:, h, c0:512], True, False,
                                    [BckvT, Bqtc[kq]], [Bpl])
                            for ei, (dl, cc) in enumerate(extra):
                                self.MM(pl[:, cc:cc + 128], self.ident_b[:, :], self.BtT[:, h, dl, :], False, False,
                                        [self.Bconst], [Bpl])
                            self.MM(pl[:, c0:512], self.ident_b[:, :], mk[:, st_, c0:512], False, True, [self.Bconst, Bmk3], [Bpl])
                            return pl, Bpl, c0
                        nxt = emit_qk(0)
                        for st_ in range(nst):
                            pl, Bpl, c0 = nxt
                            if st_ + 1 < nst:
                                nxt = emit_qk(st_ + 1)
                            p_, Bp_ = pT[npt % 3], BpT[npt % 3]
                            npt += 1
                            self.ACT(p_[:, c0:512], pl[:, c0:512], AF.Exp, [Bpl], [Bp_])
                            self.MM(po[:, c0:512], ckv_tm[:, st_, :], p_[:, c0:512], st_ == 0, st_ == nst - 1, [Bckv, Bp_], [Bpo])
                            self.MM(pd[:, c0:512], self.ones_b[:, :], p_[:, c0:512], st_ == 0, st_ == nst - 1, [self.Bconst, Bp_], [Bpd])
                        S.op("dve", (lambda pd_: (lambda e: e.reciprocal(out=rec[:, :], in_=pd_[:, :])))(pd), [Bpd], [Brec])
                        self.TT("dve", oT[:, h, :], po[:, :], rec[:, :], ALU.mult, [Bpo, Brec], [BoT[h]])
                    py = [pp[6], pp[7]]
                    Bpy = [Bpp[6], Bpp[7]]

                    def yfn(i, tc=tc):
                        q = i - 4 * tc
                        for hf in range(2):
                            for h in range(NH):
                                self.MM(py[hf][:, :], oT[:, h, q * 128:(q + 1) * 128], wout[:, h, hf * 512:(hf + 1) * 512],
                                        h == 0, h == NH - 1, [BoT[h], Bwout], [Bpy[hf]])
                        return [py[0][:, :], py[1][:, :]], Bpy

                    def ptsfn(i):
                        return [pp[0], pp[1]], [Bpp[0], Bpp[1]], pp[2], Bpp[2]
                    self.ln_run(c, [4 * tc + q for q in range(4)], yfn, dst, ptsfn)
                S.flush()


def _t5_onehot():
    n_buckets, max_distance = 32, 128
    max_exact = n_buckets // 2
    oh = np.zeros((32, VW), np.float32)
    for jj in range(VW):
        dd = jj - 127
        if dd < 0:
            continue
        d_f = np.float32(max(dd, 1))
        large = max_exact + int(np.float32(np.log(d_f / np.float32(max_exact), dtype=np.float32)
                                           / np.float32(np.log(max_distance / max_exact))
                                           * np.float32(n_buckets - max_exact)))
        large = min(large, n_buckets - 1)
        bkt = dd if dd < max_exact else large
        oh[bkt, jj] += 1.0
        oh[n_buckets - 1, jj] -= 1.0
    return oh


def _prep_shared(inputs):
    f = lambda a: np.ascontiguousarray(np.asarray(a, dtype=np.float32))
    m = {}
    for j in range(2):
        m["cwin%d" % j] = f(inputs["conv_w_in"][j])
        m["cwout%d" % j] = f(inputs["conv_w_out"][j])
        m["awin%d" % j] = f(inputs["attn_w_in"][j])
        m["awout%d" % j] = f(inputs["attn_w_out"][j])
    m["conv_k"] = f(inputs["conv_k"])
    m["kv_g"] = f(inputs["kv_norm_g"])
    m["ki_g"] = f(inputs["kidx_ln_g"])
    m["ki_b"] = f(inputs["kidx_ln_b"])
    m["rel_bias"] = f(inputs["rel_bias"])
    m["ohs"] = _t5_onehot()
    m["rw"] = f(np.concatenate([np.asarray(inputs["router_wg"]), np.asarray(inputs["router_we"])], axis=2))
    m["rb"] = f(np.concatenate([np.asarray(inputs["router_bg"]), np.asarray(inputs["router_be"])], axis=1))
    for l in range(DEPTH):
        w1 = np.asarray(inputs["exp_w1"][l], dtype=np.float32).reshape(64, KC, 128, 256)
        m["w1r%d" % l] = np.ascontiguousarray(w1.transpose(0, 2, 1, 3)).reshape(64 * 128, 2048)
        w3 = np.asarray(inputs["exp_w3"][l], dtype=np.float32).reshape(64, KC, 128, 256)
        m["w3r%d" % l] = np.ascontiguousarray(w3.transpose(0, 2, 1, 3)).reshape(64 * 128, 2048)
        w2 = np.asarray(inputs["exp_w2"][l], dtype=np.float32).reshape(64, 2, 128, D)
        m["w2r%d" % l] = np.ascontiguousarray(w2.transpose(0, 2, 1, 3)).reshape(64 * 128, 2048)
    for nm in ("ln1_g", "ln1_b", "ln2_g", "ln2_b"):
        m[nm] = f(inputs[nm])
    return m


_PROG_CACHE = {}


def _get_prog(n_layers=DEPTH, stop_after_mixer=False):
    key = (n_layers, stop_after_mixer)
    if key not in _PROG_CACHE:
        _PROG_CACHE[key] = Prog(n_layers, stop_after_mixer).build()
    return _PROG_CACHE[key]


def kernel(**inputs):
    x = np.asarray(inputs["x"], dtype=np.float32)
    n = x.shape[0]
    shared = _prep_shared(inputs)
    nc = _get_prog()
    in_maps = []
    for b in range(n):
        m = dict(shared)
        m["x"] = np.ascontiguousarray(x[b])
        in_maps.append(m)
    res = run_bass_kernel_spmd(nc, in_maps, core_ids=list(range(n)))
    return np.stack([np.asarray(r["out"]) for r in res.results], axis=0).astype(np.float32)
```
